# Optimizing a Trainium2 kernel written in Bass

```python
import jax
import jax.numpy as jnp
from jax import lax
import numpy as np

D_MODEL = 2048
BATCH = 2
SEQ = 8192
DEPTH = 4

MIX_WIDTH = D_MODEL
RET_WIDTH = MIX_WIDTH // 2
RET_HEADS = 8
RET_HEAD_DIM = RET_WIDTH // RET_HEADS
RET_CHUNK = 128
RET_GN_EPS = 1e-5
ROPE_BASE = 10000.0
RWKV_WIDTH = MIX_WIDTH // 2
RWKV_HEAD_DIM = 64
RWKV_HEADS = RWKV_WIDTH // RWKV_HEAD_DIM
LORA_W = 64
LORA_A = 64
LORA_G = 128
RWKV_LN_EPS = 64e-5
D_FF = 4 * D_MODEL
NORM_EPS = 1e-6

RET_COLS = (RET_WIDTH, RET_WIDTH, RET_WIDTH, RET_WIDTH)
RWKV_COLS = (RWKV_WIDTH, RWKV_WIDTH, RWKV_WIDTH, LORA_W, LORA_A, LORA_G)
RWKV_SHIFT_WIDTH = sum(RWKV_COLS)
IN_WIDTH = sum(RET_COLS) + RWKV_SHIFT_WIDTH + 2 * D_MODEL

kernel_name = "retention_rwkv7_gated_hybrid"


def _split_cols(z, widths):
    offs = np.cumsum([0] + list(widths))
    return [z[..., int(offs[i]):int(offs[i + 1])] for i in range(len(widths))]


def rms_norm(x, g):
    xf = x.astype(jnp.float32)
    y = xf * lax.rsqrt(jnp.mean(xf * xf, axis=-1, keepdims=True) + NORM_EPS)
    return (y * g.astype(jnp.float32)).astype(x.dtype)


def head_norm(y, g, b, eps):
    H, d = y.shape[-2:]
    mean = jnp.mean(y, axis=-1, keepdims=True)
    var = jnp.mean(jnp.square(y - mean), axis=-1, keepdims=True)
    yn = (y - mean) * lax.rsqrt(var + eps)
    return yn * g.reshape(H, d).astype(jnp.float32) + b.reshape(H, d).astype(jnp.float32)


def rotary(t, positions):
    half = t.shape[-1] // 2
    inv_freq = ROPE_BASE ** (-jnp.arange(half, dtype=jnp.float32) / half)
    ang = positions.astype(jnp.float32)[..., None] * inv_freq
    cos = jnp.cos(ang)[:, :, None, :]
    sin = jnp.sin(ang)[:, :, None, :]
    t1, t2 = t[..., :half], t[..., half:]
    return jnp.concatenate([t1 * cos - t2 * sin, t2 * cos + t1 * sin], axis=-1)


def retention(q, k, v):
    B, S, H, d = q.shape
    C = RET_CHUNK
    N = S // C
    log_gamma = jnp.log1p(-(2.0 ** (-5.0 - jnp.arange(H, dtype=jnp.float32))))
    idx = jnp.arange(C, dtype=jnp.float32)
    diff = idx[:, None] - idx[None, :]
    inner_decay = jnp.where(diff[None] >= 0,
                            jnp.exp(jnp.maximum(diff, 0.0)[None] * log_gamma[:, None, None]), 0.0)
    query_decay = jnp.exp((idx + 1.0)[None, :] * log_gamma[:, None])[None, :, :, None]
    key_decay = jnp.exp((C - 1.0 - idx)[None, :] * log_gamma[:, None])[None, :, :, None]
    chunk_decay = jnp.exp(C * log_gamma)[None, :, None, None]

    def to_chunks(t):
        return t.reshape(B, N, C, H, d).transpose(1, 0, 3, 2, 4)

    def step(R, qkv):
        qn, kn, vn = qkv
        scores = jnp.einsum('bhcd,bhmd->bhcm', qn, kn) * inner_decay
        inner = jnp.einsum('bhcm,bhmd->bhcd', scores, vn)
        cross = jnp.einsum('bhcd,bhde->bhce', qn, R) * query_decay
        R_new = chunk_decay * R + jnp.einsum('bhcd,bhce->bhde', kn * key_decay, vn)
        return R_new, inner + cross

    R0 = jnp.zeros((B, H, d, d), jnp.float32)
    _, out = lax.scan(step, R0, (to_chunks(q), to_chunks(k), to_chunks(v)))
    return out.transpose(1, 0, 3, 2, 4).reshape(B, S, H, d)


def retention_branch(z, positions, gn_g, gn_b):
    B, S, _ = z.shape
    q, k, v, gr = _split_cols(z.astype(jnp.float32), RET_COLS)
    shp = (B, S, RET_HEADS, RET_HEAD_DIM)
    q = rotary(q.reshape(shp), positions)
    k = rotary(k.reshape(shp), positions) * (RET_HEAD_DIM ** -0.5)
    y = retention(q, k, v.reshape(shp))
    y = head_norm(y, gn_g, gn_b, RET_GN_EPS).reshape(B, S, RET_WIDTH)
    return jax.nn.silu(gr) * y


def rwkv7_scan(r, decay, k, v, kk, a):
    B, S, H, d = r.shape

    def step(state, inp):
        r_t, w_t, k_t, v_t, kk_t, a_t = inp
        sa = jnp.einsum('bhvk,bhk->bhv', state, -kk_t)
        state = (state * w_t[:, :, None, :]
                 + sa[..., None] * (kk_t * a_t)[:, :, None, :]
                 + v_t[..., None] * k_t[:, :, None, :])
        y = jnp.einsum('bhvk,bhk->bhv', state, r_t)
        return state, y

    seq_first = lambda t: jnp.moveaxis(t, 1, 0)
    s0 = jnp.zeros((B, H, d, d), jnp.float32)
    _, y = lax.scan(step, s0, tuple(seq_first(t) for t in (r, decay, k, v, kk, a)))
    return jnp.moveaxis(y, 0, 1)


def rwkv7_branch(z, mu, w0, w_up, a0, a_up, g_up, k_k, k_a, r_k, ln_g, ln_b):
    B, S, _ = z.shape
    f32 = jnp.float32
    z = z.astype(f32)
    z_prev = jnp.pad(z, ((0, 0), (1, 0), (0, 0)))[:, :-1]
    z = z + mu.astype(f32) * (z_prev - z)
    r, k, v, zw, za, zg = _split_cols(z, RWKV_COLS)
    w = -jax.nn.softplus(-(w0.astype(f32) + jnp.tanh(zw) @ w_up.astype(f32))) - 0.5
    decay = jnp.exp(-jnp.exp(w))
    a = jax.nn.sigmoid(a0.astype(f32) + za @ a_up.astype(f32))
    g = jax.nn.sigmoid(zg) @ g_up.astype(f32)
    heads = lambda t: t.reshape(B, S, RWKV_HEADS, RWKV_HEAD_DIM)
    kk = heads(k * k_k.astype(f32))
    kk = kk / jnp.maximum(jnp.sqrt(jnp.sum(kk * kk, axis=-1, keepdims=True)), 1e-12)
    k = k * (1.0 + (a - 1.0) * k_a.astype(f32))
    rh, kh, vh = heads(r), heads(k), heads(v)
    y = rwkv7_scan(rh, heads(decay), kh, vh, kk, heads(a))
    y = head_norm(y, ln_g, ln_b, RWKV_LN_EPS)
    bonus = jnp.sum(rh * kh * r_k.astype(f32).reshape(RWKV_HEADS, RWKV_HEAD_DIM), axis=-1, keepdims=True)
    y = y + bonus * vh
    return y.reshape(B, S, RWKV_WIDTH) * g


def setup_inputs(seed: int = 0) -> dict:
    key = jax.random.key(seed)
    ks = jax.random.split(key, 26)
    L, D = DEPTH, D_MODEL
    nrm = lambda k, shp, s: jax.random.normal(k, shp, jnp.float32) * s
    x = jax.random.normal(ks[0], (BATCH, SEQ, D), jnp.float32)
    offset = jax.random.randint(ks[1], (BATCH, 1), 0, 4096, dtype=jnp.int32)
    positions = offset + jnp.arange(SEQ, dtype=jnp.int32)[None, :]
    return {
        'x': x,
        'positions': positions,
        'norm1_g': 1.0 + nrm(ks[2], (L, D), 0.02),
        'w_in': nrm(ks[3], (L, D, IN_WIDTH), D ** -0.5),
        'ret_gn_g': 1.0 + nrm(ks[4], (L, RET_WIDTH), 0.02),
        'ret_gn_b': nrm(ks[5], (L, RET_WIDTH), 0.02),
        'rwkv_mu': jax.random.uniform(ks[6], (L, RWKV_SHIFT_WIDTH), jnp.float32),
        'rwkv_w0': jax.random.uniform(ks[7], (L, RWKV_WIDTH), jnp.float32, -6.0, 0.5),
        'rwkv_w_up': nrm(ks[8], (L, LORA_W, RWKV_WIDTH), 0.5 * LORA_W ** -0.5),
        'rwkv_a0': nrm(ks[9], (L, RWKV_WIDTH), 0.1),
        'rwkv_a_up': nrm(ks[10], (L, LORA_A, RWKV_WIDTH), LORA_A ** -0.5),
        'rwkv_g_up': nrm(ks[11], (L, LORA_G, RWKV_WIDTH), LORA_G ** -0.5),
        'rwkv_k_k': 0.85 + nrm(ks[12], (L, RWKV_WIDTH), 0.05),
        'rwkv_k_a': 1.0 + nrm(ks[13], (L, RWKV_WIDTH), 0.05),
        'rwkv_r_k': nrm(ks[14], (L, RWKV_WIDTH), 0.1),
        'rwkv_ln_g': 1.0 + nrm(ks[15], (L, RWKV_WIDTH), 0.02),
        'rwkv_ln_b': nrm(ks[16], (L, RWKV_WIDTH), 0.02),
        'w_branch_a': nrm(ks[17], (L, RET_WIDTH, D), RET_WIDTH ** -0.5),
        'w_branch_b': nrm(ks[18], (L, RWKV_WIDTH, D), RWKV_WIDTH ** -0.5),
        'w_out': nrm(ks[19], (L, D, D), D ** -0.5),
        'norm2_g': 1.0 + nrm(ks[20], (L, D), 0.02),
        'mlp_up': nrm(ks[21], (L, D, D_FF), D ** -0.5),
        'mlp_down': nrm(ks[22], (L, D_FF, D), D_FF ** -0.5),
        'final_g': 1.0 + nrm(ks[23], (D,), 0.02),
    }


def reference(x, positions, norm1_g, w_in, ret_gn_g, ret_gn_b, rwkv_mu, rwkv_w0, rwkv_w_up,
              rwkv_a0, rwkv_a_up, rwkv_g_up, rwkv_k_k, rwkv_k_a, rwkv_r_k, rwkv_ln_g, rwkv_ln_b,
              w_branch_a, w_branch_b, w_out, norm2_g, mlp_up, mlp_down, final_g):
    for l in range(DEPTH):
        h = rms_norm(x, norm1_g[l])
        z = h @ w_in[l]
        z_ret, z_rwkv, z_ga, z_gb = _split_cols(z, (sum(RET_COLS), RWKV_SHIFT_WIDTH, D_MODEL, D_MODEL))
        y_ret = retention_branch(z_ret, positions, ret_gn_g[l], ret_gn_b[l]).astype(x.dtype)
        y_rwkv = rwkv7_branch(z_rwkv, rwkv_mu[l], rwkv_w0[l], rwkv_w_up[l], rwkv_a0[l], rwkv_a_up[l],
                              rwkv_g_up[l], rwkv_k_k[l], rwkv_k_a[l], rwkv_r_k[l],
                              rwkv_ln_g[l], rwkv_ln_b[l]).astype(x.dtype)
        merged = (jax.nn.sigmoid(z_ga) * (y_ret @ w_branch_a[l])
                  + jax.nn.sigmoid(z_gb) * (y_rwkv @ w_branch_b[l]))
        x = x + merged @ w_out[l]
        h = rms_norm(x, norm2_g[l])
        x = x + jnp.square(jax.nn.relu(h @ mlp_up[l])) @ mlp_down[l]
    return rms_norm(x, final_g)
```

```python
import contextlib
import numpy as np
import concourse.bass as bass
import concourse.mybir as mybir

F32 = mybir.dt.float32
BF16 = mybir.dt.bfloat16
I32 = mybir.dt.int32
ALU = mybir.AluOpType
AF = mybir.ActivationFunctionType
AX = mybir.AxisListType

EPOCH = 30000
NDMA = 24


class Buf:
    __slots__ = ("name", "w", "rs")

    def __init__(self, name=""):
        self.name = name
        self.w = {}
        self.rs = {}


def _put(d, ev):
    k = id(ev[0])
    if k not in d or d[k][1] < ev[1]:
        d[k] = ev


class Prog:
    ENGS = ("pe", "act", "dve", "pool", "sp")

    def __init__(self, nc):
        self.nc = nc
        self.stack = contextlib.ExitStack()
        self.semstack = contextlib.ExitStack()
        self.eobj = {"pe": nc.tensor, "act": nc.scalar, "dve": nc.vector,
                     "pool": nc.gpsimd, "sp": nc.sync}
        self.lists = {e: [] for e in self.ENGS}
        self.cnt = {e: 0 for e in self.ENGS}
        self.cursem = {}
        self.seen = {e: {} for e in self.ENGS}
        self.nsem = 0
        for e in ("pe", "act", "dve", "pool"):
            self.cursem[e] = self.new_sem(e)
        self.dsem = [self.new_sem("dma%d" % i) for i in range(NDMA)]
        self.dcum = [0] * NDMA
        self.dnext = 0
        self.nbuf = 0
        self.ccsem = self.new_sem("cc")
        self.cccum = 0

    def new_sem(self, name):
        self.nsem += 1
        return self.semstack.enter_context(self.nc.semaphore("s_%s_%d" % (name, self.nsem)))

    def sbuf(self, name, shape, dtype):
        self.nalloc = getattr(self, "nalloc", 0) + 1
        return self.stack.enter_context(self.nc.sbuf_tensor("sb_%s_%d" % (name, self.nalloc), list(shape), dtype))

    def psum(self, name, shape, dtype):
        self.nalloc = getattr(self, "nalloc", 0) + 1
        return self.stack.enter_context(self.nc.psum_tensor("ps_%s_%d" % (name, self.nalloc), list(shape), dtype))

    def buf(self, name=""):
        self.nbuf += 1
        return Buf(name or ("b%d" % self.nbuf))

    def _deps(self, reads, writes, pwrites=(), selfsem=None):
        evs = []
        for b in reads:
            if b is not None:
                evs.extend(b.w.values())
        for b in writes:
            if b is not None:
                evs.extend(b.w.values())
                evs.extend(b.rs.values())
        for b in pwrites:
            if b is not None:
                evs.extend(e for e in b.w.values() if e[0] is not selfsem)
                evs.extend(b.rs.values())
        return evs

    def _waits(self, eng, evs):
        seen = self.seen[eng]
        need = {}
        for (s, v) in evs:
            if seen.get(id(s), (None, 0))[1] >= v:
                continue
            if id(s) not in need or need[id(s)][1] < v:
                need[id(s)] = (s, v)
        for k, (s, v) in need.items():
            seen[k] = (s, v)
        return list(need.values())

    def _record(self, ev, reads, writes, pwrites=()):
        for b in reads:
            if b is not None:
                _put(b.rs, ev)
        for b in writes:
            if b is not None:
                b.w = {id(ev[0]): ev}
                b.rs = {}
        for b in pwrites:
            if b is not None:
                _put(b.w, ev)

    def op(self, eng, fn, reads=(), writes=(), pwrites=()):
        if self.cnt[eng] >= EPOCH:
            self.cursem[eng] = self.new_sem(eng)
            self.cnt[eng] = 0
        evs = self._deps(reads, writes, pwrites, self.cursem[eng])
        waits = self._waits(eng, evs)
        self.cnt[eng] += 1
        ev = (self.cursem[eng], self.cnt[eng])
        self.lists[eng].append((waits, fn, ev[0], 1))
        self._record(ev, reads, writes, pwrites)
        return ev

    def dma(self, out_ap, in_ap, reads=(), writes=(), pwrites=(), q="sp", **kw):
        i = self.dnext
        self.dnext = (self.dnext + 1) % NDMA
        s = self.dsem[i]
        evs = self._deps(reads, writes)
        for b in pwrites:
            evs.extend(b.rs.values())
        if self.dcum[i] > 0:
            evs.append((s, self.dcum[i]))
        waits = self._waits(q, evs)
        self.dcum[i] += 16
        ev = (s, self.dcum[i])
        fn = (lambda e, o=out_ap, a=in_ap, k=kw: e.dma_start(out=o, in_=a, **k))
        self.lists[q].append((waits, fn, s, 16))
        self._record(ev, reads, writes, pwrites)
        return ev

    def cc(self, fn, reads=(), writes=()):
        s = self.ccsem
        evs = self._deps(reads, writes)
        if self.cccum > 0:
            evs.append((s, self.cccum))
        waits = self._waits("pool", evs)
        self.cccum += 1
        ev = (s, self.cccum)
        self.lists["pool"].append((waits, fn, s, 1))
        self._record(ev, reads, writes)
        return ev

    def barrier(self):
        evs = []
        for e in ("pe", "act", "dve", "pool"):
            if self.cnt[e] > 0:
                evs.append((self.cursem[e], self.cnt[e]))
        for i in range(NDMA):
            if self.dcum[i] > 0:
                evs.append((self.dsem[i], self.dcum[i]))
        if self.cccum > 0:
            evs.append((self.ccsem, self.cccum))
        for e in self.ENGS:
            waits = self._waits(e, list(evs))
            if waits:
                self.lists[e].append((waits, None, None, 0))

    @contextlib.contextmanager
    def scope(self):
        outer = self.stack
        self.stack = contextlib.ExitStack()
        try:
            yield
        finally:
            self.barrier()
            self.stack.close()
            self.stack = outer

    def wait_all(self, eng, bufs):
        evs = []
        for b in bufs:
            evs.extend(b.w.values())
            evs.extend(b.rs.values())
        waits = self._waits(eng, evs)
        self.lists[eng].append((waits, None, None, 0))

    def emit(self):
        nc = self.nc
        with nc.Block() as block:
            def run(engname):
                def body(e):
                    for waits, fn, sem, inc in self.lists[engname]:
                        for (s, v) in waits:
                            e.wait_ge(s, v)
                        if fn is not None:
                            fn(e).then_inc(sem, inc)
                return body
            block.tensor(run("pe"))
            block.scalar(run("act"))
            block.vector(run("dve"))
            block.gpsimd(run("pool"))
            block.sync(run("sp"))

    def close(self):
        self.stack.close()
        self.semstack.close()

D = 2048
DFF = 8192
KC = D // 128
TT = 512
NSUB = TT // 128
EPS = 1e-6


class Ctx:
    pass


def setup_common(P, C, consts):
    C.identb = P.sbuf("identb", [128, 128], BF16)
    C.identf = P.sbuf("identf", [128, 128], F32)
    C.bconst = P.buf("consts")
    P.dma(C.identf[:], consts["identf"], writes=[C.bconst])
    P.op("dve", lambda e: e.tensor_copy(out=C.identb[:], in_=C.identf[:]), reads=[C.bconst], pwrites=[C.bconst])
    C.epsb = P.sbuf("epsb", [128, 4], F32)
    P.op("dve", lambda e: e.memset(C.epsb[:, 0:1], EPS), pwrites=[C.bconst])
    C.acc = []
    for i in range(6):
        C.acc.append((P.psum("acc%d" % i, [128, 512], F32), P.buf("acc%d" % i)))
    C.ptr = []
    for i in range(2):
        C.ptr.append((P.psum("ptr%d" % i, [128, 1024], BF16), P.buf("ptr%d" % i)))


def alloc_wstream(P, C):
    C.wst = [(P.sbuf("wst%d" % i, [128, 4, 512], F32), P.buf("wst%d" % i)) for i in range(4)]
    C.wb = [(P.sbuf("wb%d" % i, [128, 16, 512], BF16), P.buf("wb%d" % i)) for i in range(2)]
    C.wst_i = 0
    C.wb_i = 0


def load_w_block(P, C, w_ap, nkc=16, ncol=512):
    wb, bwb = C.wb[C.wb_i % 2]
    C.wb_i += 1
    wv = w_ap.rearrange("(kc p) c -> p kc c", p=128)
    first = True
    for q in range(0, nkc, 4):
        n = min(4, nkc - q)
        st, bst = C.wst[C.wst_i % 4]
        C.wst_i += 1
        P.dma(st[:, 0:n, 0:ncol], wv[:, q:q + n, :], writes=[bst])
        eng = "pool" if (C.wst_i % 4) != 0 else "dve"
        kw = dict(writes=[bwb]) if first else dict(pwrites=[bwb])
        P.op(eng, lambda e, st=st, q=q, n=n: e.tensor_copy(out=wb[:, q:q + n, 0:ncol], in_=st[:, 0:n, 0:ncol]),
             reads=[bst], **kw)
        first = False
    return wb, bwb


def alloc_norm(P, C):
    C.xt = [(P.sbuf("xt%d" % i, [128, D], F32), P.buf("xt%d" % i)) for i in range(2)]
    C.junk = (P.sbuf("junk", [128, D], BF16), P.buf("junk"))
    C.hb = (P.sbuf("hb", [128, D], BF16), P.buf("hb"))
    C.ss = [(P.sbuf("ss%d" % i, [128, 2], F32), P.buf("ss%d" % i)) for i in range(2)]
    C.gsb = (P.sbuf("gsb", [128, D], F32), P.buf("gsb"))
    C.hT = (P.sbuf("hT", [128, KC, TT], BF16), P.buf("hT"))
    C.xt_i = 0


def norm_T(P, C, x_tile, bx):
    hT, bhT = C.hT
    gsb, bg = C.gsb
    junk, bjunk = C.junk
    hb, bhb = C.hb
    for s in range(NSUB):
        xt, bxt = C.xt[C.xt_i % 2]
        ss, bss = C.ss[C.xt_i % 2]
        C.xt_i += 1
        P.dma(xt[:], x_tile[s * 128:(s + 1) * 128, :], reads=[bx], writes=[bxt])
        P.op("act", lambda e, xt=xt, ss=ss: e.activation(out=junk[:], in_=xt[:], func=AF.Square, accum_out=ss[:, 0:1]),
             reads=[bxt], writes=[bjunk, bss])
        P.op("act", lambda e, ss=ss: e.activation(out=ss[:, 1:2], in_=ss[:, 0:1], func=AF.Sqrt, scale=1.0 / D, bias=C.epsb[:, 0:1]),
             reads=[bss, C.bconst], pwrites=[bss])
        P.op("dve", lambda e, ss=ss: e.reciprocal(out=ss[:, 1:2], in_=ss[:, 1:2]), reads=[bss], pwrites=[bss])
        P.op("dve", lambda e, xt=xt, ss=ss: e.scalar_tensor_tensor(out=hb[:], in0=xt[:], scalar=ss[:, 1:2], in1=gsb[:],
                                                                    op0=ALU.mult, op1=ALU.mult),
             reads=[bxt, bss, bg], writes=[bhb])
        for j in range(0, KC, 8):
            pt, bpt = C.ptr[(j // 8) % 2]
            for i in range(8):
                kw = dict(writes=[bpt]) if i == 0 else dict(pwrites=[bpt])
                P.op("pe", lambda e, pt=pt, i=i, j=j: e.transpose(out=pt[:, i * 128:(i + 1) * 128],
                                                                   in_=hb[:, (j + i) * 128:(j + i + 1) * 128],
                                                                   identity=C.identb[:]),
                     reads=[bhb, C.bconst], **kw)
            kw = dict(writes=[bhT]) if (s == 0 and j == 0) else dict(pwrites=[bhT])
            P.op("act", lambda e, pt=pt, j=j, s=s: e.activation(
                out=hT[:, j:j + 8, s * 128:(s + 1) * 128],
                in_=pt[:].rearrange("p (a b) -> p a b", a=8), func=AF.Copy),
                reads=[bpt], **kw)


def ffn_phase(P, C, xres, bxres, w_up, w_down, g2rep, ntiles):
    with P.scope():
        alloc_norm(P, C)
        alloc_wstream(P, C)
        uT = P.sbuf("uT", [128, DFF // 128, TT], BF16)
        buT = [P.buf("uT%d" % i) for i in range(DFF // 128)]
        tmp = [(P.sbuf("rtmp%d" % i, [128, 512], F32), P.buf("rtmp%d" % i)) for i in range(2)]
        xo = [(P.sbuf("xo%d" % i, [128, 512], F32), P.buf("xo%d" % i)) for i in range(2)]
        P.dma(C.gsb[0][:], g2rep, writes=[C.gsb[1]])
        ti = 0
        oi = 0
        for tt in range(ntiles):
            x_tile = xres[tt * TT:(tt + 1) * TT, :]
            bx = bxres[tt]
            norm_T(P, C, x_tile, bx)
            hT, bhT = C.hT
            for blk in range(DFF // 512):
                wb, bwb = load_w_block(P, C, w_up[:, blk * 512:(blk + 1) * 512])
                for j in range(4):
                    ps, bps = C.acc[j]
                    fc = blk * 4 + j
                    for kc in range(KC):
                        kw = dict(writes=[bps]) if kc == 0 else dict(pwrites=[bps])
                        P.op("pe", lambda e, ps=ps, wb=wb, kc=kc, j=j: e.matmul(
                            ps[:], lhsT=wb[:, kc, j * 128:(j + 1) * 128], rhs=hT[:, kc, :],
                            start=(kc == 0), stop=(kc == KC - 1)), reads=[bwb, bhT], **kw)
                    t, bt = tmp[ti % 2]
                    ti += 1
                    P.op("act", lambda e, t=t, ps=ps: e.activation(out=t[:], in_=ps[:], func=AF.Relu),
                         reads=[bps], writes=[bt])
                    P.op("pool", lambda e, t=t, fc=fc: e.tensor_tensor(out=uT[:, fc, :], in0=t[:], in1=t[:], op=ALU.mult),
                         reads=[bt], writes=[buT[fc]])
            for cb in range(D // 512):
                for fb in range(DFF // 2048):
                    wb, bwb = load_w_block(P, C, w_down[fb * 2048:(fb + 1) * 2048, cb * 512:(cb + 1) * 512])
                    for s in range(NSUB):
                        ps, bps = C.acc[s]
                        for fc in range(16):
                            first = (fb == 0 and fc == 0)
                            last = (fb == DFF // 2048 - 1 and fc == 15)
                            kw = dict(writes=[bps]) if first else dict(pwrites=[bps])
                            P.op("pe", lambda e, ps=ps, wb=wb, fc=fc, fb=fb, s=s, first=first, last=last: e.matmul(
                                ps[:], lhsT=uT[:, fb * 16 + fc, s * 128:(s + 1) * 128], rhs=wb[:, fc, :],
                                start=first, stop=last), reads=[bwb, buT[fb * 16 + fc]], **kw)
                for s in range(NSUB):
                    ps, bps = C.acc[s]
                    o, bo = xo[oi % 2]
                    oi += 1
                    rows = slice(tt * TT + s * 128, tt * TT + (s + 1) * 128)
                    P.dma(o[:], xres[rows, cb * 512:(cb + 1) * 512], reads=[bx], writes=[bo])
                    P.op("dve", lambda e, o=o, ps=ps: e.tensor_tensor(out=o[:], in0=o[:], in1=ps[:], op=ALU.add),
                         reads=[bps, bo], writes=[bo])
                    P.dma(xres[rows, cb * 512:(cb + 1) * 512], o[:], reads=[bo], pwrites=[bx])


def proj_phase(P, C, x, bx, w_in, g1rep, z_out, bz, ntiles, ncols):
    with P.scope():
        alloc_norm(P, C)
        alloc_wstream(P, C)
        zo = [(P.sbuf("zo%d" % i, [128, 512], F32), P.buf("zo%d" % i)) for i in range(4)]
        P.dma(C.gsb[0][:], g1rep, writes=[C.gsb[1]])
        oi = 0
        for tt in range(ntiles):
            norm_T(P, C, x[tt * TT:(tt + 1) * TT, :], bx)
            hT, bhT = C.hT
            for c0 in range(0, ncols, 512):
                nc_ = min(512, ncols - c0)
                wb, bwb = load_w_block(P, C, w_in[:, c0:c0 + nc_], ncol=nc_)
                for s in range(NSUB):
                    ps, bps = C.acc[s]
                    for kc in range(KC):
                        kw = dict(writes=[bps]) if kc == 0 else dict(pwrites=[bps])
                        P.op("pe", lambda e, ps=ps, wb=wb, kc=kc, s=s, nc_=nc_: e.matmul(
                            ps[:, 0:nc_], lhsT=hT[:, kc, s * 128:(s + 1) * 128], rhs=wb[:, kc, 0:nc_],
                            start=(kc == 0), stop=(kc == KC - 1)), reads=[bwb, bhT], **kw)
                    o, bo = zo[oi % 4]
                    oi += 1
                    if oi % 2:
                        P.op("act", lambda e, o=o, ps=ps, nc_=nc_: e.activation(out=o[:, 0:nc_], in_=ps[:, 0:nc_], func=AF.Copy),
                             reads=[bps], writes=[bo])
                    else:
                        P.op("dve", lambda e, o=o, ps=ps, nc_=nc_: e.tensor_copy(out=o[:, 0:nc_], in_=ps[:, 0:nc_]),
                             reads=[bps], writes=[bo])
                    rows = slice(tt * TT + s * 128, tt * TT + (s + 1) * 128)
                    P.dma(z_out[rows, c0:c0 + nc_], o[:, 0:nc_], reads=[bo], pwrites=[bz])


def merge_phase(P, C, xres, bxres, yretT, yrwT, gaT, gbT, w_a, w_b, w_out, ntiles):
    with P.scope():
        alloc_wstream(P, C)
        yst = [(P.sbuf("yst%d" % i, [128, 8, TT], F32), P.buf("yst%d" % i)) for i in range(2)]
        yb = [(P.sbuf("yb%d" % i, [128, 8, TT], BF16), P.buf("yb%d" % i)) for i in range(2)]
        gt = [(P.sbuf("gt%d" % i, [128, TT], F32), P.buf("gt%d" % i)) for i in range(4)]
        t12 = [(P.sbuf("t12_%d" % i, [128, TT], F32), P.buf("t12_%d" % i)) for i in range(4)]
        mT = P.sbuf("mT", [128, KC, TT], BF16)
        bmT = [P.buf("mT%d" % i) for i in range(KC)]
        xo = [(P.sbuf("xo%d" % i, [128, 512], F32), P.buf("xo%d" % i)) for i in range(2)]
        gi = 0
        oi = 0
        for tt in range(ntiles):
            tok = slice(tt * TT, (tt + 1) * TT)
            for i, src in enumerate((yretT, yrwT)):
                st, bst = yst[i]
                P.dma(st[:], src.rearrange("(kc p) t -> p kc t", p=128)[:, :, tok], writes=[bst])
                eng = "pool" if i == 0 else "dve"
                P.op(eng, lambda e, st=st, i=i: e.tensor_copy(out=yb[i][0][:], in_=st[:]), reads=[bst], writes=[yb[i][1]])
            for db in range(D // 512):
                wa, bwa = load_w_block(P, C, w_a[:, db * 512:(db + 1) * 512], nkc=8)
                wbb, bwbb = load_w_block(P, C, w_b[:, db * 512:(db + 1) * 512], nkc=8)
                for j in range(4):
                    dc = db * 4 + j
                    res = []
                    for i, (w_, bw_, gT) in enumerate(((wa, bwa, gaT), (wbb, bwbb, gbT))):
                        ps, bps = C.acc[(2 * j + i) % 6]
                        for kc in range(8):
                            kw = dict(writes=[bps]) if kc == 0 else dict(pwrites=[bps])
                            P.op("pe", lambda e, ps=ps, w_=w_, kc=kc, j=j, i=i: e.matmul(
                                ps[:], lhsT=w_[:, kc, j * 128:(j + 1) * 128], rhs=yb[i][0][:, kc, :],
                                start=(kc == 0), stop=(kc == 7)), reads=[bw_, yb[i][1]], **kw)
                        g, bg = gt[gi % 4]
                        t, bt = t12[gi % 4]
                        gi += 1
                        P.dma(g[:], gT[dc * 128:(dc + 1) * 128, tok], writes=[bg])
                        P.op("act", lambda e, g=g: e.activation(out=g[:], in_=g[:], func=AF.Sigmoid), reads=[bg], writes=[bg])
                        P.op("dve", lambda e, t=t, g=g, ps=ps: e.tensor_tensor(out=t[:], in0=g[:], in1=ps[:], op=ALU.mult),
                             reads=[bg, bps], writes=[bt])
                        res.append((t, bt))
                    P.op("pool", lambda e, dc=dc, a=res[0][0], b=res[1][0]: e.tensor_tensor(out=mT[:, dc, :], in0=a[:], in1=b[:], op=ALU.add),
                         reads=[res[0][1], res[1][1]], writes=[bmT[dc]])
            bx = bxres[tt]
            for cb in range(D // 512):
                wb, bwb = load_w_block(P, C, w_out[:, cb * 512:(cb + 1) * 512])
                for s in range(NSUB):
                    ps, bps = C.acc[s]
                    for kc in range(KC):
                        kw = dict(writes=[bps]) if kc == 0 else dict(pwrites=[bps])
                        P.op("pe", lambda e, ps=ps, wb=wb, kc=kc, s=s: e.matmul(
                            ps[:], lhsT=mT[:, kc, s * 128:(s + 1) * 128], rhs=wb[:, kc, :],
                            start=(kc == 0), stop=(kc == KC - 1)), reads=[bwb, bmT[kc]], **kw)
                    o, bo = xo[oi % 2]
                    oi += 1
                    rows = slice(tt * TT + s * 128, tt * TT + (s + 1) * 128)
                    P.dma(o[:], xres[rows, cb * 512:(cb + 1) * 512], reads=[bx], writes=[bo])
                    P.op("dve", lambda e, o=o, ps=ps: e.tensor_tensor(out=o[:], in0=o[:], in1=ps[:], op=ALU.add),
                         reads=[bps, bo], writes=[bo])
                    P.dma(xres[rows, cb * 512:(cb + 1) * 512], o[:], reads=[bo], pwrites=[bx])


def final_norm_phase(P, C, xres, bxres, grep, out, bout, ntiles):
    with P.scope():
        alloc_norm(P, C)
        gsb, bg = C.gsb
        P.dma(gsb[:], grep, writes=[bg])
        junk, bjunk = C.junk
        ob = [(P.sbuf("fo%d" % i, [128, D], F32), P.buf("fo%d" % i)) for i in range(2)]
        for r in range(ntiles * NSUB):
            xt, bxt = C.xt[r % 2]
            ss, bss = C.ss[r % 2]
            o, bo = ob[r % 2]
            rows = slice(r * 128, (r + 1) * 128)
            P.dma(xt[:], xres[rows, :], reads=[bxres[r // NSUB]], writes=[bxt])
            P.op("act", lambda e, xt=xt, ss=ss: e.activation(out=junk[:], in_=xt[:], func=AF.Square, accum_out=ss[:, 0:1]),
                 reads=[bxt], writes=[bjunk, bss])
            P.op("act", lambda e, ss=ss: e.activation(out=ss[:, 1:2], in_=ss[:, 0:1], func=AF.Sqrt, scale=1.0 / D, bias=C.epsb[:, 0:1]),
                 reads=[bss, C.bconst], pwrites=[bss])
            P.op("dve", lambda e, ss=ss: e.reciprocal(out=ss[:, 1:2], in_=ss[:, 1:2]), reads=[bss], pwrites=[bss])
            P.op("dve", lambda e, xt=xt, ss=ss, o=o: e.scalar_tensor_tensor(out=o[:], in0=xt[:], scalar=ss[:, 1:2], in1=gsb[:],
                                                                             op0=ALU.mult, op1=ALU.mult),
                 reads=[bxt, bss, bg], writes=[bo])
            P.dma(out[rows, :], o[:], reads=[bo], pwrites=[bout])


def build_C(N, final):
    nc = bass.Bass("TRN2", target_bir_lowering=False)
    dt = lambda n, s, k="ExternalInput": nc.dram_tensor(n, s, F32, kind=k).ap()
    x = dt("x", [N, D])
    yretT = dt("yretT", [1024, N]); yrwT = dt("yrwT", [1024, N])
    gaT = dt("gaT", [D, N]); gbT = dt("gbT", [D, N])
    w_a = dt("w_a", [1024, D]); w_b = dt("w_b", [1024, D]); w_out = dt("w_out", [D, D])
    w_up = dt("w_up", [D, DFF]); w_down = dt("w_down", [DFF, D])
    g2 = dt("g2", [128, D]); gf = dt("gf", [128, D]); identf = dt("identf", [128, 128])
    y = dt("y", [N, D], "ExternalOutput")
    xres = nc.dram_tensor("xres", [N, D], F32).ap()
    P = Prog(nc); C = Ctx()
    setup_common(P, C, {"identf": identf})
    nt = N // TT
    bxres = [P.buf() for _ in range(nt)]
    for tt in range(nt):
        P.dma(xres[tt * TT:(tt + 1) * TT, :], x[tt * TT:(tt + 1) * TT, :], writes=[bxres[tt]])
    merge_phase(P, C, xres, bxres, yretT, yrwT, gaT, gbT, w_a, w_b, w_out, nt)
    ffn_phase(P, C, xres, bxres, w_up, w_down, g2, nt)
    bout = P.buf()
    if final:
        final_norm_phase(P, C, xres, bxres, gf, y, bout, nt)
    else:
        for tt in range(nt):
            P.dma(y[tt * TT:(tt + 1) * TT, :], xres[tt * TT:(tt + 1) * TT, :], reads=[bxres[tt]], pwrites=[bout])
    P.wait_all("sp", [bout])
    P.emit(); P.close()
    return nc


def build_A(N, ncols=11520):
    nc = bass.Bass("TRN2", target_bir_lowering=False)
    dt = lambda n, s, k="ExternalInput": nc.dram_tensor(n, s, F32, kind=k).ap()
    x = dt("x", [N, D]); w = dt("w_in", [D, ncols]); g = dt("g1", [128, D]); identf = dt("identf", [128, 128])
    z = dt("z", [N, ncols], "ExternalOutput")
    P = Prog(nc); C = Ctx()
    setup_common(P, C, {"identf": identf})
    bz = P.buf()
    proj_phase(P, C, x, None, w, g, z, bz, N // TT, ncols)
    P.wait_all("sp", [bz])
    P.emit(); P.close()
    return nc


TWO_PI = 6.283185307179586
LN_EPS = 64e-5
GN_EPS = 1e-5


def build_B(S):
    nc = bass.Bass("TRN2", target_bir_lowering=False)
    dt = lambda n, s, k="ExternalInput", d=F32: nc.dram_tensor(n, s, d, kind=k).ap()
    ztm = dt("ztm", [S, 3, 256]); ztm_p = dt("ztm_p", [S, 3, 256])
    zvT = dt("zvT", [64, 4, S]); zvT_p = dt("zvT_p", [64, 4, S])
    zlT = dt("zlT", [128, 2, S]); zlT_p = dt("zlT_p", [128, 2, S])
    ptm = dt("ptm", [128, 10, 256]); mu_l = dt("mu_l", [128, 2]); mu_vT = dt("mu_vT", [64, 4])
    wa_up = dt("wa_up", [128, 256]); g_up = dt("g_up", [128, 256])
    sel = dt("sel", [128, 128, 64]); identf = dt("identf", [128, 128])
    rq = dt("rq", [S, 256]); rk = dt("rk", [S, 256]); rvT = dt("rvT", [128, 2, S]); rgr = dt("rgr", [S, 256])
    pos = dt("pos", [S, 1], d=I32); invf = dt("invf", [128, 64]); gam = dt("gam", [128, 2, 128])
    gn = dt("gn", [128, 2, 256]); selr = dt("selr", [128, 128, 128])
    y_rw = dt("y_rw", [S, 256], "ExternalOutput"); y_ret = dt("y_ret", [S, 256], "ExternalOutput")

    P = Prog(nc)
    sb = P.sbuf
    bc = P.buf("const")
    c_ptm = sb("ptm", [128, 10, 256], F32); c_mul = sb("mul", [128, 2], F32); c_muv = sb("muv", [64, 4], F32)
    c_wa = sb("waup", [128, 256], F32); c_gu = sb("gup", [128, 256], F32)
    c_sel = sb("sel", [128, 128, 64], F32); c_id = sb("id", [128, 128], F32)
    c_invf = sb("invf", [128, 64], F32); c_gam = sb("gam", [128, 2, 128], F32); c_gn = sb("gn", [128, 2, 256], F32)
    c_selr = sb("selr", [128, 128, 128], F32)
    c_omka = sb("omka", [128, 256], F32); c_bias = sb("cbias", [128, 4], F32)
    for t_, a_ in ((c_ptm, ptm), (c_mul, mu_l), (c_muv, mu_vT), (c_wa, wa_up), (c_gu, g_up), (c_sel, sel), (c_id, identf),
                   (c_invf, invf), (c_gam, gam), (c_gn, gn), (c_selr, selr)):
        P.dma(t_[:], a_, pwrites=[bc])
    P.op("dve", lambda e: e.tensor_scalar(out=c_omka[:], in0=c_ptm[:, 6, :], scalar1=-1.0, scalar2=1.0, op0=ALU.mult, op1=ALU.add),
         reads=[bc], pwrites=[bc])
    P.op("dve", lambda e: e.memset(c_bias[:, 0:1], -3.141592653589793), pwrites=[bc])
    P.op("dve", lambda e: e.memset(c_bias[:, 1:2], LN_EPS), pwrites=[bc])
    P.op("dve", lambda e: e.memset(c_bias[:, 2:3], GN_EPS), pwrites=[bc])

    Srw = sb("Srw", [64, 4, 64], F32); bS = P.buf("Srw")
    Rr = sb("Rr", [128, 2, 128], F32); bR = P.buf("Rr")
    P.op("dve", lambda e: e.memset(Srw[:], 0.0), writes=[bS])
    P.op("pool", lambda e: e.memset(Rr[:], 0.0), writes=[bR])

    psRow = [[(P.psum("prow%d_%d" % (i, j), [64, 512], F32), P.buf()) for j in range(3)] for i in range(2)]
    psMisc = (P.psum("pmisc", [128, 512], F32), P.buf("pmisc"))
    psRet = (P.psum("pret", [128, 512], F32), P.buf("pret"))

    def T(name, shape, n=1, dtype=F32):
        return [(sb(name + str(i), shape, dtype), P.buf(name + str(i))) for i in range(n)]
    zt = T("zt", [128, 3, 256], 2); zp = T("zp", [128, 3, 256], 2)
    vT = T("vT", [64, 4, 128], 2); vTp = T("vTp", [64, 4, 128], 2)
    lT = T("lT", [128, 2, 128], 2); lTp = T("lTp", [128, 2, 128], 2)
    rows = T("rows", [128, 5, 256], 2)
    yT = T("yT", [64, 4, 128], 2)
    w1 = T("w1", [128, 256], 6)
    sm = T("sm", [128, 16], 4)
    vtm = T("vtm", [128, 256], 2)
    gtm = T("gtm", [128, 256], 2)
    bon = T("bon", [128, 4], 2)
    tmpS = T("tmpS", [64, 4, 64], 2); saS = T("saS", [64, 4], 2)
    tmpP = T("tmpP", [64, 4, 64], 2)
    orw = T("orw", [128, 256], 2)
    st6 = T("st6", [128, 4, 6], 2); mv = T("mv", [128, 4, 2], 2)
    rqt = T("rqt", [128, 256], 2); rkt = T("rkt", [128, 256], 2); rgt = T("rgt", [128, 256], 2)
    rvt = T("rvt", [128, 2, 128], 2); post = T("post", [128, 2], 2, I32); posf = T("posf", [128, 2], 2)
    cs = T("cs", [128, 2, 64], 2)
    rrow = T("rrow", [128, 2, 256], 2)
    rrs = T("rrs", [128, 512], 2)
    ryT = T("ryT", [128, 2, 128], 2)
    tmpR = T("tmpR", [128, 2, 128], 2); tmpR2 = T("tmpR2", [128, 2, 128], 2)
    oret = T("oret", [128, 256], 2)
    w2 = T("w2", [128, 256], 4)
    kit = T("kit", [128, 64], 2, I32)

    NCH = S // 128
    for c in range(NCH):
        i2 = c % 2
        tok = slice(c * 128, (c + 1) * 128)
        z, bz = zt[i2]; zpp, bzp = zp[i2]
        P.dma(z[:], ztm[tok], writes=[bz]); P.dma(zpp[:], ztm_p[tok], writes=[bzp])
        v_, bv_ = vT[i2]; vp_, bvp_ = vTp[i2]
        P.dma(v_[:], zvT[:, :, tok], writes=[bv_]); P.dma(vp_[:], zvT_p[:, :, tok], writes=[bvp_])
        l_, bl_ = lT[i2]; lp_, blp_ = lTp[i2]
        P.dma(l_[:], zlT[:, :, tok], writes=[bl_]); P.dma(lp_[:], zlT_p[:, :, tok], writes=[blp_])
        P.op("dve", lambda e, z=z, zpp=zpp: e.tensor_tensor(out=zpp[:], in0=zpp[:], in1=z[:], op=ALU.subtract), reads=[bz, bzp], writes=[bzp])
        P.op("dve", lambda e, z=z, zpp=zpp: e.tensor_tensor(out=zpp[:], in0=zpp[:], in1=c_ptm[:, 0:3, :], op=ALU.mult), reads=[bzp, bc], writes=[bzp])
        P.op("dve", lambda e, z=z, zpp=zpp: e.tensor_tensor(out=z[:], in0=z[:], in1=zpp[:], op=ALU.add), reads=[bz, bzp], writes=[bz])
        P.op("pool", lambda e, v_=v_, vp_=vp_: e.tensor_tensor(out=vp_[:], in0=vp_[:], in1=v_[:], op=ALU.subtract), reads=[bv_, bvp_], writes=[bvp_])
        P.op("pool", lambda e, v_=v_, vp_=vp_: e.tensor_tensor(out=vp_[:], in0=vp_[:], in1=c_muv[:].unsqueeze(2).to_broadcast([64, 4, 128]), op=ALU.mult), reads=[bvp_, bc], writes=[bvp_])
        P.op("pool", lambda e, v_=v_, vp_=vp_: e.tensor_tensor(out=v_[:], in0=v_[:], in1=vp_[:], op=ALU.add), reads=[bv_, bvp_], writes=[bv_])
        P.op("pool", lambda e, l_=l_, lp_=lp_: e.tensor_tensor(out=lp_[:], in0=lp_[:], in1=l_[:], op=ALU.subtract), reads=[bl_, blp_], writes=[blp_])
        P.op("pool", lambda e, l_=l_, lp_=lp_: e.tensor_tensor(out=lp_[:], in0=lp_[:], in1=c_mul[:].unsqueeze(2).to_broadcast([128, 2, 128]), op=ALU.mult), reads=[blp_, bc], writes=[blp_])
        P.op("pool", lambda e, l_=l_, lp_=lp_: e.tensor_tensor(out=l_[:], in0=l_[:], in1=lp_[:], op=ALU.add), reads=[bl_, blp_], writes=[bl_])
        P.op("act", lambda e, l_=l_: e.activation(out=l_[0:64, 0, :], in_=l_[0:64, 0, :], func=AF.Tanh), reads=[bl_], writes=[bl_])
        P.op("act", lambda e, l_=l_: e.activation(out=l_[:, 1, :], in_=l_[:, 1, :], func=AF.Sigmoid), reads=[bl_], writes=[bl_])
        pm, bpm = psMisc
        rw_, brw_ = rows[i2]
        P.op("pe", lambda e, l_=l_: e.matmul(pm[:, 0:256], lhsT=l_[0:64, 0, :], rhs=c_wa[0:64, :], start=True, stop=True), reads=[bl_, bc], writes=[bpm])
        a1, ba1 = w1[0]
        P.op("dve", lambda e: e.tensor_tensor(out=a1[:], in0=pm[:, 0:256], in1=c_ptm[:, 3, :], op=ALU.add), reads=[bpm, bc], writes=[ba1])
        P.op("act", lambda e: e.activation(out=a1[:], in_=a1[:], func=AF.Sigmoid), reads=[ba1], writes=[ba1])
        P.op("act", lambda e, rw_=rw_: e.activation(out=rw_[:, 0, :], in_=a1[:], func=AF.Exp, scale=-0.6065306597126334), reads=[ba1], writes=[brw_])
        P.op("pe", lambda e, l_=l_: e.matmul(pm[:, 0:256], lhsT=l_[64:128, 0, :], rhs=c_wa[64:128, :], start=True, stop=True), reads=[bl_, bc, ba1], writes=[bpm])
        al, bal = w1[1]
        P.op("dve", lambda e: e.tensor_tensor(out=al[:], in0=pm[:, 0:256], in1=c_ptm[:, 4, :], op=ALU.add), reads=[bpm, bc], writes=[bal])
        P.op("act", lambda e: e.activation(out=al[:], in_=al[:], func=AF.Sigmoid), reads=[bal], writes=[bal])
        g_, bg_ = gtm[i2]
        P.op("pe", lambda e, l_=l_: e.matmul(pm[:, 0:256], lhsT=l_[:, 1, :], rhs=c_gu[:], start=True, stop=True), reads=[bl_, bc, bal], writes=[bpm])
        P.op("act", lambda e, g_=g_: e.activation(out=g_[:], in_=pm[:, 0:256], func=AF.Copy), reads=[bpm], writes=[bg_])
        kk, bkk = w1[2]; k2, bk2 = w1[3]
        s_, bs_ = sm[i2]
        P.op("dve", lambda e, z=z: e.tensor_tensor(out=kk[:], in0=z[:, 1, :], in1=c_ptm[:, 5, :], op=ALU.mult), reads=[bz, bc], writes=[bkk])
        P.op("dve", lambda e: e.tensor_tensor(out=k2[:], in0=kk[:], in1=kk[:], op=ALU.mult), reads=[bkk], writes=[bk2])
        P.op("dve", lambda e, s_=s_: e.tensor_reduce(out=s_[:, 0:4], in_=k2[:].rearrange("p (h k) -> p h k", h=4), axis=AX.X, op=ALU.add), reads=[bk2], writes=[bs_])
        P.op("act", lambda e, s_=s_: e.activation(out=s_[:, 0:4], in_=s_[:, 0:4], func=AF.Sqrt), reads=[bs_], writes=[bs_])
        P.op("dve", lambda e, s_=s_: e.tensor_scalar(out=s_[:, 0:4], in0=s_[:, 0:4], scalar1=1e-12, scalar2=None, op0=ALU.max), reads=[bs_], writes=[bs_])
        P.op("dve", lambda e, s_=s_: e.reciprocal(out=s_[:, 0:4], in_=s_[:, 0:4]), reads=[bs_], writes=[bs_])
        P.op("dve", lambda e, s_=s_: e.tensor_tensor(out=kk[:].rearrange("p (h k) -> p h k", h=4), in0=kk[:].rearrange("p (h k) -> p h k", h=4),
                                                     in1=s_[:, 0:4].unsqueeze(2).to_broadcast([128, 4, 64]), op=ALU.mult), reads=[bkk, bs_], writes=[bkk])
        P.op("dve", lambda e, rw_=rw_: e.tensor_scalar(out=rw_[:, 1, :], in0=kk[:], scalar1=-1.0, scalar2=None, op0=ALU.mult), reads=[bkk], pwrites=[brw_])
        P.op("dve", lambda e, rw_=rw_: e.tensor_tensor(out=rw_[:, 2, :], in0=kk[:], in1=al[:], op=ALU.mult), reads=[bkk, bal], pwrites=[brw_])
        P.op("dve", lambda e: e.tensor_tensor(out=k2[:], in0=al[:], in1=c_ptm[:, 6, :], op=ALU.mult), reads=[bal, bc], writes=[bk2])
        P.op("dve", lambda e: e.tensor_tensor(out=k2[:], in0=k2[:], in1=c_omka[:], op=ALU.add), reads=[bk2, bc], writes=[bk2])
        P.op("dve", lambda e, rw_=rw_, z=z: e.tensor_tensor(out=rw_[:, 3, :], in0=z[:, 1, :], in1=k2[:], op=ALU.mult), reads=[bz, bk2], pwrites=[brw_])
        P.op("dve", lambda e, rw_=rw_, z=z: e.tensor_copy(out=rw_[:, 4, :], in_=z[:, 0, :]), reads=[bz], pwrites=[brw_])
        bn_, bbn_ = bon[i2]
        P.op("dve", lambda e, z=z: e.tensor_tensor(out=k2[:], in0=z[:, 0, :], in1=c_ptm[:, 7, :], op=ALU.mult), reads=[bz, bc], writes=[bk2])
        P.op("dve", lambda e, rw_=rw_: e.tensor_tensor(out=k2[:], in0=k2[:], in1=rw_[:, 3, :], op=ALU.mult), reads=[bk2, brw_], writes=[bk2])
        P.op("dve", lambda e, bn_=bn_: e.tensor_reduce(out=bn_[:], in_=k2[:].rearrange("p (h k) -> p h k", h=4), axis=AX.X, op=ALU.add), reads=[bk2], writes=[bbn_])
        y_, by_ = yT[i2]
        for t in range(128):
            pr = psRow[t % 2]
            for j, (c0, n) in enumerate(((0, 512), (512, 512), (1024, 256))):
                P.op("pe", lambda e, pr=pr, j=j, c0=c0, n=n, rw_=rw_, t=t: e.matmul(
                    pr[j][0][:, 0:n], lhsT=c_sel[:, t, :], rhs=rw_[:].rearrange("p a b -> p (a b)")[:, c0:c0 + n], start=True, stop=True),
                    reads=[brw_, bc], writes=[pr[j][1]])
            row = lambda r: (pr[(r * 256) // 512][0][:, (r * 256) % 512:(r * 256) % 512 + 256].rearrange("p (h k) -> p h k", h=4), pr[(r * 256) // 512][1])
            wr, bwr = row(0); ar, bar = row(1); br_, bbr = row(2); kr, bkr = row(3); rr, brr = row(4)
            tS, btS = tmpS[t % 2]; sa, bsa = saS[t % 2]; tP, btP = tmpP[t % 2]
            P.op("dve", lambda e, tS=tS, ar=ar: e.tensor_tensor(out=tS[:], in0=Srw[:], in1=ar, op=ALU.mult), reads=[bS, bar], writes=[btS])
            P.op("dve", lambda e, tS=tS, sa=sa: e.tensor_reduce(out=sa[:], in_=tS[:], axis=AX.X, op=ALU.add), reads=[btS], writes=[bsa])
            P.op("dve", lambda e, wr=wr: e.tensor_tensor(out=Srw[:], in0=Srw[:], in1=wr, op=ALU.mult), reads=[bS, bwr], writes=[bS])
            P.op("dve", lambda e, tS=tS, sa=sa, br_=br_: e.tensor_tensor(out=tS[:], in0=br_, in1=sa[:].unsqueeze(2).to_broadcast([64, 4, 64]), op=ALU.mult),
                 reads=[bbr, bsa], writes=[btS])
            P.op("dve", lambda e, tS=tS: e.tensor_tensor(out=Srw[:], in0=Srw[:], in1=tS[:], op=ALU.add), reads=[bS, btS], writes=[bS])
            P.op("dve", lambda e, tP=tP, kr=kr, v_=v_, t=t: e.tensor_tensor(out=tP[:], in0=kr, in1=v_[:, :, t:t + 1].to_broadcast([64, 4, 64]), op=ALU.mult),
                 reads=[bkr, bv_], writes=[btP])
            P.op("dve", lambda e, tP=tP: e.tensor_tensor(out=Srw[:], in0=Srw[:], in1=tP[:], op=ALU.add), reads=[bS, btP], writes=[bS])
            P.op("dve", lambda e, tS=tS, rr=rr: e.tensor_tensor(out=tS[:], in0=Srw[:], in1=rr, op=ALU.mult), reads=[bS, brr], writes=[btS])
            kw = dict(writes=[by_]) if t == 0 else dict(pwrites=[by_])
            P.op("dve", lambda e, tS=tS, y_=y_, t=t: e.tensor_reduce(out=y_[:, :, t], in_=tS[:], axis=AX.X, op=ALU.add), reads=[btS], **kw)
        vt_, bvt_ = vtm[i2]
        o_, bo_ = orw[i2]
        for h in range(4):
            P.op("pe", lambda e, h=h, y_=y_: e.transpose(out=pm[:, h * 64:(h + 1) * 64], in_=y_[:, h, :], identity=c_id[0:64, 0:64]),
                 reads=[by_, bc] + ([bg_] if h == 0 else []), **(dict(writes=[bpm]) if h == 0 else dict(pwrites=[bpm])))
        s6, bs6 = st6[i2]; m_, bm_ = mv[i2]
        for h in range(4):
            P.op("dve", lambda e, h=h, s6=s6: e.bn_stats(out=s6[:, h, :], in_=pm[:, h * 64:(h + 1) * 64]), reads=[bpm], **(dict(writes=[bs6]) if h == 0 else dict(pwrites=[bs6])))
        for h in range(4):
            P.op("dve", lambda e, h=h, s6=s6, m_=m_: e.bn_aggr(out=m_[:, h, :], in_=s6[:, h, :]), reads=[bs6], **(dict(writes=[bm_]) if h == 0 else dict(pwrites=[bm_])))
        P.op("act", lambda e, m_=m_: e.activation(out=m_[:, :, 1], in_=m_[:, :, 1], func=AF.Sqrt, bias=c_bias[:, 1:2]), reads=[bm_, bc], writes=[bm_])
        P.op("dve", lambda e, m_=m_: e.reciprocal(out=m_[:, :, 1], in_=m_[:, :, 1]), reads=[bm_], writes=[bm_])
        for h in range(4):
            P.op("dve", lambda e, h=h, m_=m_, o_=o_: e.tensor_scalar(out=o_[:, h * 64:(h + 1) * 64], in0=pm[:, h * 64:(h + 1) * 64],
                                                                     scalar1=m_[:, h, 0:1], scalar2=m_[:, h, 1:2], op0=ALU.subtract, op1=ALU.mult),
                 reads=[bpm, bm_], **(dict(writes=[bo_]) if h == 0 else dict(pwrites=[bo_])))
        P.op("dve", lambda e, o_=o_: e.tensor_tensor(out=o_[:], in0=o_[:], in1=c_ptm[:, 8, :], op=ALU.mult), reads=[bo_, bc], writes=[bo_])
        P.op("dve", lambda e, o_=o_: e.tensor_tensor(out=o_[:], in0=o_[:], in1=c_ptm[:, 9, :], op=ALU.add), reads=[bo_, bc], writes=[bo_])
        P.op("dve", lambda e, z=z, bn_=bn_: e.tensor_tensor(out=k2[:].rearrange("p (h k) -> p h k", h=4), in0=z[:, 2, :].rearrange("p (h k) -> p h k", h=4),
                                                            in1=bn_[:].unsqueeze(2).to_broadcast([128, 4, 64]), op=ALU.mult), reads=[bz, bbn_], writes=[bk2])
        P.op("dve", lambda e, o_=o_: e.tensor_tensor(out=o_[:], in0=o_[:], in1=k2[:], op=ALU.add), reads=[bo_, bk2], writes=[bo_])
        P.op("dve", lambda e, o_=o_, g_=g_: e.tensor_tensor(out=o_[:], in0=o_[:], in1=g_[:], op=ALU.mult), reads=[bo_, bg_], writes=[bo_])
        P.dma(y_rw[tok, :], o_[:], reads=[bo_])

        q_, bq_ = rqt[i2]; k_, bk_ = rkt[i2]; gr_, bgr_ = rgt[i2]; rv_, brv_ = rvt[i2]
        pt_, bpt_ = post[i2]; pf_, bpf_ = posf[i2]; cs_, bcs_ = cs[i2]; rr_, brr_ = rrow[i2]
        P.dma(q_[:], rq[tok, :], writes=[bq_]); P.dma(k_[:], rk[tok, :], writes=[bk_]); P.dma(gr_[:], rgr[tok, :], writes=[bgr_])
        P.dma(rv_[:], rvT[:, :, tok], writes=[brv_]); P.dma(pt_[:, 0:1], pos[tok, :], writes=[bpt_])
        P.op("pool", lambda e, pt_=pt_, pf_=pf_: e.tensor_copy(out=pf_[:, 0:1], in_=pt_[:, 0:1]), reads=[bpt_], writes=[bpf_])
        ang, bang = w2[0]; ang2, bang2 = w2[1]
        P.op("dve", lambda e, pf_=pf_: e.tensor_scalar(out=ang[:, 0:64], in0=c_invf[:], scalar1=pf_[:, 0:1], scalar2=None, op0=ALU.mult), reads=[bpf_, bc], writes=[bang])
        P.op("dve", lambda e: e.tensor_scalar(out=ang2[:, 0:64], in0=ang[:, 0:64], scalar1=1.5707963267948966, scalar2=None, op0=ALU.add), reads=[bang], writes=[bang2])
        ki_, bki_ = kit[i2]
        for (a_, ba_, col) in ((ang2, bang2, 0), (ang, bang, 1)):
            P.op("dve", lambda e, a_=a_: e.tensor_scalar(out=a_[:, 64:128], in0=a_[:, 0:64], scalar1=1.0 / TWO_PI, scalar2=None, op0=ALU.mult), reads=[ba_], writes=[ba_])
            P.op("dve", lambda e, a_=a_, ki_=ki_: e.tensor_copy(out=ki_[:], in_=a_[:, 64:128]), reads=[ba_], writes=[bki_])
            P.op("dve", lambda e, a_=a_, ki_=ki_: e.tensor_copy(out=a_[:, 64:128], in_=ki_[:]), reads=[bki_], writes=[ba_])
            P.op("dve", lambda e, a_=a_: e.scalar_tensor_tensor(out=a_[:, 0:64], in0=a_[:, 64:128], scalar=-TWO_PI, in1=a_[:, 0:64], op0=ALU.mult, op1=ALU.add), reads=[ba_], writes=[ba_])
            P.op("dve", lambda e, a_=a_: e.tensor_scalar(out=a_[:, 64:128], in0=a_[:, 0:64], scalar1=3.141592653589793, scalar2=-TWO_PI, op0=ALU.is_gt, op1=ALU.mult), reads=[ba_], writes=[ba_])
            P.op("dve", lambda e, a_=a_: e.tensor_tensor(out=a_[:, 0:64], in0=a_[:, 0:64], in1=a_[:, 64:128], op=ALU.add), reads=[ba_], writes=[ba_])
            kw = dict(writes=[bcs_]) if col == 0 else dict(pwrites=[bcs_])
            P.op("act", lambda e, cs_=cs_, a_=a_, col=col: e.activation(out=cs_[:, col, :], in_=a_[:, 0:64], func=AF.Sin), reads=[ba_], **kw)
        ta, bta = w2[2]; tb, btb = w2[3]
        for xi, (x_, bx_) in enumerate(((k_, bk_), (q_, bq_))):
            xv = x_[:].rearrange("p (h two d) -> p h two d", h=2, two=2)
            ov = rr_[:, xi, :].rearrange("p (h two d) -> p h two d", h=2, two=2)
            tav = ta[:, 0:128].rearrange("p (h d) -> p h d", h=2); tbv = tb[:, 0:128].rearrange("p (h d) -> p h d", h=2)
            ncb = lambda cs_=cs_: cs_[:, 0:1, :].to_broadcast([128, 2, 64])
            nsb = lambda cs_=cs_: cs_[:, 1:2, :].to_broadcast([128, 2, 64])
            kw0 = dict(writes=[brr_]) if xi == 0 else dict(pwrites=[brr_])
            P.op("pool", lambda e, xv=xv, tav=tav, ncb=ncb: e.tensor_tensor(out=tav, in0=xv[:, :, 0, :], in1=ncb(), op=ALU.mult), reads=[bx_, bcs_], writes=[bta])
            P.op("pool", lambda e, xv=xv, tbv=tbv, nsb=nsb: e.tensor_tensor(out=tbv, in0=xv[:, :, 1, :], in1=nsb(), op=ALU.mult), reads=[bx_, bcs_], writes=[btb])
            P.op("pool", lambda e, ov=ov, tav=tav, tbv=tbv: e.tensor_tensor(out=ov[:, :, 0, :], in0=tav, in1=tbv, op=ALU.subtract), reads=[bta, btb], **kw0)
            P.op("pool", lambda e, xv=xv, tav=tav, ncb=ncb: e.tensor_tensor(out=tav, in0=xv[:, :, 1, :], in1=ncb(), op=ALU.mult), reads=[bx_, bcs_], writes=[bta])
            P.op("pool", lambda e, xv=xv, tbv=tbv, nsb=nsb: e.tensor_tensor(out=tbv, in0=xv[:, :, 0, :], in1=nsb(), op=ALU.mult), reads=[bx_, bcs_], writes=[btb])
            P.op("pool", lambda e, ov=ov, tav=tav, tbv=tbv: e.tensor_tensor(out=ov[:, :, 1, :], in0=tav, in1=tbv, op=ALU.add), reads=[bta, btb], pwrites=[brr_])
        P.op("pool", lambda e, rr_=rr_: e.tensor_scalar(out=rr_[:, 0, :], in0=rr_[:, 0, :], scalar1=128.0 ** -0.5, scalar2=None, op0=ALU.mult), reads=[brr_], pwrites=[brr_])
        pr_, bpr_ = psRet
        ry_, bry_ = ryT[i2]
        for t in range(128):
            P.op("pe", lambda e, rr_=rr_, t=t: e.matmul(pr_[:], lhsT=c_selr[:, t, :], rhs=rr_[:].rearrange("p a b -> p (a b)"), start=True, stop=True),
                 reads=[brr_, bc], writes=[bpr_])
            rs_, brs_ = rrs[t % 2]
            P.op("act", lambda e, rs_=rs_: e.activation(out=rs_[:], in_=pr_[:], func=AF.Copy), reads=[bpr_], writes=[brs_])
            t1, bt1 = tmpR[t % 2]; t2, bt2 = tmpR2[t % 2]
            krow = rs_[:, 0:256].rearrange("p (h d) -> p h d", h=2); qrow = rs_[:, 256:512].rearrange("p (h d) -> p h d", h=2)
            P.op("pool", lambda e: e.tensor_tensor(out=Rr[:], in0=Rr[:], in1=c_gam[:], op=ALU.mult), reads=[bR, bc], writes=[bR])
            P.op("pool", lambda e, t1=t1, krow=krow, rv_=rv_, t=t: e.tensor_tensor(out=t1[:], in0=krow, in1=rv_[:, :, t:t + 1].to_broadcast([128, 2, 128]), op=ALU.mult),
                 reads=[brs_, brv_], writes=[bt1])
            P.op("pool", lambda e, t1=t1: e.tensor_tensor(out=Rr[:], in0=Rr[:], in1=t1[:], op=ALU.add), reads=[bR, bt1], writes=[bR])
            P.op("pool", lambda e, t2=t2, qrow=qrow: e.tensor_tensor(out=t2[:], in0=Rr[:], in1=qrow, op=ALU.mult), reads=[bR, brs_], writes=[bt2])
            kw = dict(writes=[bry_]) if t == 0 else dict(pwrites=[bry_])
            P.op("dve", lambda e, t2=t2, ry_=ry_, t=t: e.tensor_reduce(out=ry_[:, :, t], in_=t2[:], axis=AX.X, op=ALU.add), reads=[bt2], **kw)
        for h in range(2):
            P.op("pe", lambda e, h=h, ry_=ry_: e.transpose(out=pm[:, h * 128:(h + 1) * 128], in_=ry_[:, h, :], identity=c_id[:]),
                 reads=[bry_, bc], **(dict(writes=[bpm]) if h == 0 else dict(pwrites=[bpm])))
        s6, bs6 = st6[i2]; m_, bm_ = mv[i2]
        orr, borr = oret[i2]
        for h in range(2):
            P.op("dve", lambda e, h=h, s6=s6: e.bn_stats(out=s6[:, h, :], in_=pm[:, h * 128:(h + 1) * 128]), reads=[bpm], **(dict(writes=[bs6]) if h == 0 else dict(pwrites=[bs6])))
        for h in range(2):
            P.op("dve", lambda e, h=h, s6=s6, m_=m_: e.bn_aggr(out=m_[:, h, :], in_=s6[:, h, :]), reads=[bs6], **(dict(writes=[bm_]) if h == 0 else dict(pwrites=[bm_])))
        P.op("act", lambda e, m_=m_: e.activation(out=m_[:, 0:2, 1], in_=m_[:, 0:2, 1], func=AF.Sqrt, bias=c_bias[:, 2:3]), reads=[bm_, bc], writes=[bm_])
        P.op("dve", lambda e, m_=m_: e.reciprocal(out=m_[:, 0:2, 1], in_=m_[:, 0:2, 1]), reads=[bm_], writes=[bm_])
        for h in range(2):
            P.op("dve", lambda e, h=h, m_=m_, orr=orr: e.tensor_scalar(out=orr[:, h * 128:(h + 1) * 128], in0=pm[:, h * 128:(h + 1) * 128],
                                                                       scalar1=m_[:, h, 0:1], scalar2=m_[:, h, 1:2], op0=ALU.subtract, op1=ALU.mult),
                 reads=[bpm, bm_], **(dict(writes=[borr]) if h == 0 else dict(pwrites=[borr])))
        P.op("dve", lambda e, orr=orr: e.tensor_tensor(out=orr[:], in0=orr[:], in1=c_gn[:, 0, :], op=ALU.mult), reads=[borr, bc], writes=[borr])
        P.op("dve", lambda e, orr=orr: e.tensor_tensor(out=orr[:], in0=orr[:], in1=c_gn[:, 1, :], op=ALU.add), reads=[borr, bc], writes=[borr])
        P.op("act", lambda e, gr_=gr_: e.activation(out=gr_[:], in_=gr_[:], func=AF.Silu), reads=[bgr_], writes=[bgr_])
        P.op("dve", lambda e, orr=orr, gr_=gr_: e.tensor_tensor(out=orr[:], in0=orr[:], in1=gr_[:], op=ALU.mult), reads=[borr, bgr_], writes=[borr])
        P.dma(y_ret[tok, :], orr[:], reads=[borr])

    P.barrier()
    P.emit(); P.close()
    return nc


def shift_prev(a, axis=0):
    out = np.zeros_like(a)
    sl_dst = [slice(None)] * a.ndim; sl_src = [slice(None)] * a.ndim
    sl_dst[axis] = slice(1, None); sl_src[axis] = slice(0, -1)
    out[tuple(sl_dst)] = a[tuple(sl_src)]
    return out


def rep128(v):
    return np.ascontiguousarray(np.broadcast_to(np.asarray(v, np.float32).reshape(1, -1), (128, v.size)))


def prep_B_rwkv(zr, zk, zv, zw, za, zg, p):
    S = zr.shape[0]
    d = {}
    ztm = np.stack([zr, zk, zv], 1).astype(np.float32)
    d["ztm"] = ztm; d["ztm_p"] = shift_prev(ztm, 0)
    zvT = np.ascontiguousarray(zv.reshape(S, 4, 64).transpose(2, 1, 0))
    d["zvT"] = zvT; d["zvT_p"] = shift_prev(zvT, 2)
    zl = np.stack([np.concatenate([zw, za], 1).T, zg.T], 1)
    zl = np.ascontiguousarray(zl.astype(np.float32))
    d["zlT"] = zl; d["zlT_p"] = shift_prev(zl, 2)
    ptm = np.stack([rep128(p[k]) for k in ("mu_r", "mu_k", "mu_v", "w0", "a0", "k_k", "k_a", "r_k", "ln_g", "ln_b")], 1)
    d["ptm"] = np.ascontiguousarray(ptm)
    d["mu_l"] = np.ascontiguousarray(np.stack([np.concatenate([p["mu_w"], p["mu_a"]]), p["mu_g"]], 1).astype(np.float32))
    d["mu_vT"] = np.ascontiguousarray(p["mu_v"].reshape(4, 64).T.astype(np.float32))
    d["wa_up"] = np.ascontiguousarray(np.concatenate([p["w_up"], p["a_up"]], 0).astype(np.float32))
    d["g_up"] = np.ascontiguousarray(p["g_up"].astype(np.float32))
    sel = np.zeros((128, 128, 64), np.float32)
    sel[np.arange(128), np.arange(128), :] = 1.0
    d["sel"] = sel
    d["identf"] = np.eye(128, dtype=np.float32)
    return d


def prep_B_ret(zq, zk, zv, zgr, pos, gn_g, gn_b, heads):
    S = zq.shape[0]
    d = {}
    d["rq"] = np.ascontiguousarray(zq, np.float32); d["rk"] = np.ascontiguousarray(zk, np.float32)
    d["rgr"] = np.ascontiguousarray(zgr, np.float32)
    d["rvT"] = np.ascontiguousarray(zv.reshape(S, 2, 128).transpose(2, 1, 0).astype(np.float32))
    d["pos"] = np.ascontiguousarray(pos.reshape(S, 1).astype(np.int32))
    inv = (10000.0 ** (-np.arange(64, dtype=np.float32) / np.float32(64))).astype(np.float32)
    d["invf"] = rep128(inv)
    gamma = 1.0 - 2.0 ** (-5.0 - np.asarray(heads, np.float64))
    d["gam"] = np.ascontiguousarray(np.broadcast_to(gamma.astype(np.float32).reshape(1, 2, 1), (128, 2, 128)))
    d["gn"] = np.ascontiguousarray(np.stack([rep128(gn_g), rep128(gn_b)], 1))
    selr = np.zeros((128, 128, 128), np.float32)
    selr[np.arange(128), np.arange(128), :] = 1.0
    d["selr"] = selr
    return d


from concourse.bass_utils import run_bass_kernel_spmd

NCORES = 8
NTOK = 2048
SEQ = 8192
_PROGS = {}


def _prog(name, fn):
    if name not in _PROGS:
        _PROGS[name] = fn()
    return _PROGS[name]


def kernel(**inp):
    f32 = lambda a: np.ascontiguousarray(np.asarray(a), dtype=np.float32)
    x = f32(inp["x"])
    positions = np.asarray(inp["positions"]).astype(np.int32)
    L = 4
    eye = np.eye(128, dtype=np.float32)
    xs = [np.ascontiguousarray(x[c // 4, (c % 4) * NTOK:(c % 4 + 1) * NTOK]) for c in range(NCORES)]
    w_in = inp["w_in"]; mu_all = np.asarray(inp["rwkv_mu"], np.float32)
    for l in range(L):
        ncA = _prog("A", lambda: build_A(NTOK))
        wl = f32(w_in[l]); g1 = rep128(np.asarray(inp["norm1_g"][l], np.float32))
        res = run_bass_kernel_spmd(ncA, [{"x": xs[c], "w_in": wl, "g1": g1, "identf": eye} for c in range(NCORES)],
                                   core_ids=list(range(NCORES)))
        z = [np.concatenate([res.results[4 * b + j]["z"] for j in range(4)], 0) for b in range(2)]
        del res, wl
        ncB = _prog("B", lambda: build_B(SEQ))
        mu = mu_all[l]
        insB = []
        for c in range(NCORES):
            b, hg = c // 4, c % 4
            zb = z[b]
            cs = slice(hg * 256, (hg + 1) * 256)
            R0 = 4096
            pv = lambda name: np.asarray(inp[name][l], np.float32)
            p = dict(mu_r=mu[0:1024][cs], mu_k=mu[1024:2048][cs], mu_v=mu[2048:3072][cs], mu_w=mu[3072:3136], mu_a=mu[3136:3200],
                     mu_g=mu[3200:3328], w0=pv("rwkv_w0")[cs], a0=pv("rwkv_a0")[cs], k_k=pv("rwkv_k_k")[cs], k_a=pv("rwkv_k_a")[cs],
                     r_k=pv("rwkv_r_k")[cs], ln_g=pv("rwkv_ln_g")[cs], ln_b=pv("rwkv_ln_b")[cs],
                     w_up=pv("rwkv_w_up")[:, cs], a_up=pv("rwkv_a_up")[:, cs], g_up=pv("rwkv_g_up")[:, cs])
            d = prep_B_rwkv(zb[:, R0 + hg * 256:R0 + (hg + 1) * 256], zb[:, R0 + 1024 + hg * 256:R0 + 1024 + (hg + 1) * 256],
                            zb[:, R0 + 2048 + hg * 256:R0 + 2048 + (hg + 1) * 256], zb[:, R0 + 3072:R0 + 3136],
                            zb[:, R0 + 3136:R0 + 3200], zb[:, R0 + 3200:R0 + 3328], p)
            d.update(prep_B_ret(zb[:, hg * 256:(hg + 1) * 256], zb[:, 1024 + hg * 256:1024 + (hg + 1) * 256],
                                zb[:, 2048 + hg * 256:2048 + (hg + 1) * 256], zb[:, 3072 + hg * 256:3072 + (hg + 1) * 256],
                                positions[b], pv("ret_gn_g")[cs], pv("ret_gn_b")[cs], [2 * hg, 2 * hg + 1]))
            insB.append(d)
        res = run_bass_kernel_spmd(ncB, insB, core_ids=list(range(NCORES)))
        del insB
        yret = [np.concatenate([res.results[4 * b + hg]["y_ret"] for hg in range(4)], 1) for b in range(2)]
        yrw = [np.concatenate([res.results[4 * b + hg]["y_rw"] for hg in range(4)], 1) for b in range(2)]
        del res
        final = (l == L - 1)
        ncC = _prog("Cf" if final else "C", lambda: build_C(NTOK, final))
        W = dict(w_a=f32(inp["w_branch_a"][l]), w_b=f32(inp["w_branch_b"][l]), w_out=f32(inp["w_out"][l]),
                 w_up=f32(inp["mlp_up"][l]), w_down=f32(inp["mlp_down"][l]),
                 g2=rep128(np.asarray(inp["norm2_g"][l], np.float32)), gf=rep128(np.asarray(inp["final_g"], np.float32)), identf=eye)
        insC = []
        for c in range(NCORES):
            b, j = c // 4, c % 4
            seg = slice(j * NTOK, (j + 1) * NTOK)
            d = dict(W)
            d.update(x=xs[c], yretT=np.ascontiguousarray(yret[b][seg].T), yrwT=np.ascontiguousarray(yrw[b][seg].T),
                     gaT=np.ascontiguousarray(z[b][seg, 7424:9472].T), gbT=np.ascontiguousarray(z[b][seg, 9472:11520].T))
            insC.append(d)
        res = run_bass_kernel_spmd(ncC, insC, core_ids=list(range(NCORES)))
        xs = [np.ascontiguousarray(res.results[c]["y"]) for c in range(NCORES)]
        del res, insC, W, z, yret, yrw
    out = np.stack([np.concatenate([xs[4 * b + j] for j in range(4)], 0) for b in range(2)], 0)
    return out.astype(np.float32)
```

```python
import contextlib
import numpy as np
import concourse.bass as bass
import concourse.mybir as mybir

F32 = mybir.dt.float32
BF16 = mybir.dt.bfloat16
I32 = mybir.dt.int32
ALU = mybir.AluOpType
AF = mybir.ActivationFunctionType
AX = mybir.AxisListType

EPOCH = 30000
NDMA = 24


class Buf:
    __slots__ = ("name", "w", "rs")

    def __init__(self, name=""):
        self.name = name
        self.w = {}
        self.rs = {}


def _put(d, ev):
    k = id(ev[0])
    if k not in d or d[k][1] < ev[1]:
        d[k] = ev


class Prog:
    ENGS = ("pe", "act", "dve", "pool", "sp")

    def __init__(self, nc):
        self.nc = nc
        self.stack = contextlib.ExitStack()
        self.semstack = contextlib.ExitStack()
        self.eobj = {"pe": nc.tensor, "act": nc.scalar, "dve": nc.vector,
                     "pool": nc.gpsimd, "sp": nc.sync}
        self.lists = {e: [] for e in self.ENGS}
        self.cnt = {e: 0 for e in self.ENGS}
        self.cursem = {}
        self.seen = {e: {} for e in self.ENGS}
        self.nsem = 0
        for e in ("pe", "act", "dve", "pool"):
            self.cursem[e] = self.new_sem(e)
        self.dsem = [self.new_sem("dma%d" % i) for i in range(NDMA)]
        self.dcum = [0] * NDMA
        self.dnext = 0
        self.nbuf = 0
        self.ccsem = self.new_sem("cc")
        self.cccum = 0

    def new_sem(self, name):
        self.nsem += 1
        return self.semstack.enter_context(self.nc.semaphore("s_%s_%d" % (name, self.nsem)))

    def sbuf(self, name, shape, dtype):
        self.nalloc = getattr(self, "nalloc", 0) + 1
        return self.stack.enter_context(self.nc.sbuf_tensor("sb_%s_%d" % (name, self.nalloc), list(shape), dtype))

    def psum(self, name, shape, dtype):
        self.nalloc = getattr(self, "nalloc", 0) + 1
        return self.stack.enter_context(self.nc.psum_tensor("ps_%s_%d" % (name, self.nalloc), list(shape), dtype))

    def buf(self, name=""):
        self.nbuf += 1
        return Buf(name or ("b%d" % self.nbuf))

    def _deps(self, reads, writes, pwrites=(), selfsem=None):
        evs = []
        for b in reads:
            if b is not None:
                evs.extend(b.w.values())
        for b in writes:
            if b is not None:
                evs.extend(b.w.values())
                evs.extend(b.rs.values())
        for b in pwrites:
            if b is not None:
                evs.extend(e for e in b.w.values() if e[0] is not selfsem)
                evs.extend(b.rs.values())
        return evs

    def _waits(self, eng, evs):
        seen = self.seen[eng]
        need = {}
        for (s, v) in evs:
            if seen.get(id(s), (None, 0))[1] >= v:
                continue
            if id(s) not in need or need[id(s)][1] < v:
                need[id(s)] = (s, v)
        for k, (s, v) in need.items():
            seen[k] = (s, v)
        return list(need.values())

    def _record(self, ev, reads, writes, pwrites=()):
        for b in reads:
            if b is not None:
                _put(b.rs, ev)
        for b in writes:
            if b is not None:
                b.w = {id(ev[0]): ev}
                b.rs = {}
        for b in pwrites:
            if b is not None:
                _put(b.w, ev)

    def op(self, eng, fn, reads=(), writes=(), pwrites=()):
        if self.cnt[eng] >= EPOCH:
            self.cursem[eng] = self.new_sem(eng)
            self.cnt[eng] = 0
        evs = self._deps(reads, writes, pwrites, self.cursem[eng])
        waits = self._waits(eng, evs)
        self.cnt[eng] += 1
        ev = (self.cursem[eng], self.cnt[eng])
        self.lists[eng].append((waits, fn, ev[0], 1))
        self._record(ev, reads, writes, pwrites)
        return ev

    def dma(self, out_ap, in_ap, reads=(), writes=(), pwrites=(), q="sp", **kw):
        i = self.dnext
        self.dnext = (self.dnext + 1) % NDMA
        s = self.dsem[i]
        evs = self._deps(reads, writes)
        for b in pwrites:
            evs.extend(b.rs.values())
        if self.dcum[i] > 0:
            evs.append((s, self.dcum[i]))
        waits = self._waits(q, evs)
        self.dcum[i] += 16
        ev = (s, self.dcum[i])
        fn = (lambda e, o=out_ap, a=in_ap, k=kw: e.dma_start(out=o, in_=a, **k))
        self.lists[q].append((waits, fn, s, 16))
        self._record(ev, reads, writes, pwrites)
        return ev

    def cc(self, fn, reads=(), writes=()):
        s = self.ccsem
        evs = self._deps(reads, writes)
        if self.cccum > 0:
            evs.append((s, self.cccum))
        waits = self._waits("pool", evs)
        self.cccum += 1
        ev = (s, self.cccum)
        self.lists["pool"].append((waits, fn, s, 1))
        self._record(ev, reads, writes)
        return ev

    def barrier(self):
        evs = []
        for e in ("pe", "act", "dve", "pool"):
            if self.cnt[e] > 0:
                evs.append((self.cursem[e], self.cnt[e]))
        for i in range(NDMA):
            if self.dcum[i] > 0:
                evs.append((self.dsem[i], self.dcum[i]))
        if self.cccum > 0:
            evs.append((self.ccsem, self.cccum))
        for e in self.ENGS:
            waits = self._waits(e, list(evs))
            if waits:
                self.lists[e].append((waits, None, None, 0))

    @contextlib.contextmanager
    def scope(self):
        outer = self.stack
        self.stack = contextlib.ExitStack()
        try:
            yield
        finally:
            self.barrier()
            self.stack.close()
            self.stack = outer

    def wait_all(self, eng, bufs):
        evs = []
        for b in bufs:
            evs.extend(b.w.values())
            evs.extend(b.rs.values())
        waits = self._waits(eng, evs)
        self.lists[eng].append((waits, None, None, 0))

    def emit(self):
        nc = self.nc
        with nc.Block() as block:
            def run(engname):
                def body(e):
                    for waits, fn, sem, inc in self.lists[engname]:
                        for (s, v) in waits:
                            e.wait_ge(s, v)
                        if fn is not None:
                            fn(e).then_inc(sem, inc)
                return body
            block.tensor(run("pe"))
            block.scalar(run("act"))
            block.vector(run("dve"))
            block.gpsimd(run("pool"))
            block.sync(run("sp"))

    def close(self):
        self.stack.close()
        self.semstack.close()

D = 2048
DFF = 8192
KC = D // 128
TT = 512
NSUB = TT // 128
EPS = 1e-6


class Ctx:
    pass


def setup_common(P, C, consts):
    C.identb = P.sbuf("identb", [128, 128], BF16)
    C.identf = P.sbuf("identf", [128, 128], F32)
    C.bconst = P.buf("consts")
    P.dma(C.identf[:], consts["identf"], writes=[C.bconst])
    P.op("dve", lambda e: e.tensor_copy(out=C.identb[:], in_=C.identf[:]), reads=[C.bconst], pwrites=[C.bconst])
    C.epsb = P.sbuf("epsb", [128, 4], F32)
    P.op("dve", lambda e: e.memset(C.epsb[:, 0:1], EPS), pwrites=[C.bconst])


def alloc_psum(P, C):
    C.acc = []
    for i in range(6):
        C.acc.append((P.psum("acc%d" % i, [128, 512], F32), P.buf("acc%d" % i)))
    C.ptr = []
    for i in range(2):
        C.ptr.append((P.psum("ptr%d" % i, [128, 1024], BF16), P.buf("ptr%d" % i)))


def alloc_wstream(P, C):
    C.wst = [(P.sbuf("wst%d" % i, [128, 4, 512], F32), P.buf("wst%d" % i)) for i in range(4)]
    C.wb = [(P.sbuf("wb%d" % i, [128, 16, 512], BF16), P.buf("wb%d" % i)) for i in range(2)]
    C.wst_i = 0
    C.wb_i = 0


def load_w_block(P, C, w_ap, nkc=16, ncol=512):
    wb, bwb = C.wb[C.wb_i % 2]
    C.wb_i += 1
    wv = w_ap.rearrange("(kc p) c -> p kc c", p=128)
    first = True
    for q in range(0, nkc, 4):
        n = min(4, nkc - q)
        st, bst = C.wst[C.wst_i % 4]
        C.wst_i += 1
        P.dma(st[:, 0:n, 0:ncol], wv[:, q:q + n, :], writes=[bst])
        eng = "pool" if (C.wst_i % 4) != 0 else "dve"
        kw = dict(writes=[bwb]) if first else dict(pwrites=[bwb])
        P.op(eng, lambda e, st=st, q=q, n=n: e.tensor_copy(out=wb[:, q:q + n, 0:ncol], in_=st[:, 0:n, 0:ncol]),
             reads=[bst], **kw)
        first = False
    return wb, bwb


def alloc_norm(P, C):
    C.xt = [(P.sbuf("xt%d" % i, [128, D], F32), P.buf("xt%d" % i)) for i in range(2)]
    C.junk = (P.sbuf("junk", [128, D], BF16), P.buf("junk"))
    C.hb = (P.sbuf("hb", [128, D], BF16), P.buf("hb"))
    C.ss = [(P.sbuf("ss%d" % i, [128, 2], F32), P.buf("ss%d" % i)) for i in range(2)]
    C.gsb = (P.sbuf("gsb", [128, D], F32), P.buf("gsb"))
    C.hT = (P.sbuf("hT", [128, KC, TT], BF16), P.buf("hT"))
    C.xt_i = 0


def norm_T(P, C, x_tile, bx, xload=None):
    hT, bhT = C.hT
    gsb, bg = C.gsb
    junk, bjunk = C.junk
    hb, bhb = C.hb
    for s in range(NSUB):
        xt, bxt = C.xt[C.xt_i % 2]
        ss, bss = C.ss[C.xt_i % 2]
        C.xt_i += 1
        if xload is None:
            P.dma(xt[:], x_tile[s * 128:(s + 1) * 128, :], reads=[bx], writes=[bxt])
        else:
            xload(s, xt, bxt)
        P.op("act", lambda e, xt=xt, ss=ss: e.activation(out=junk[:], in_=xt[:], func=AF.Square, accum_out=ss[:, 0:1]),
             reads=[bxt], writes=[bjunk, bss])
        P.op("act", lambda e, ss=ss: e.activation(out=ss[:, 1:2], in_=ss[:, 0:1], func=AF.Sqrt, scale=1.0 / D, bias=C.epsb[:, 0:1]),
             reads=[bss, C.bconst], pwrites=[bss])
        P.op("dve", lambda e, ss=ss: e.reciprocal(out=ss[:, 1:2], in_=ss[:, 1:2]), reads=[bss], pwrites=[bss])
        P.op("dve", lambda e, xt=xt, ss=ss: e.scalar_tensor_tensor(out=hb[:], in0=xt[:], scalar=ss[:, 1:2], in1=gsb[:],
                                                                    op0=ALU.mult, op1=ALU.mult),
             reads=[bxt, bss, bg], writes=[bhb])
        for j in range(0, KC, 8):
            pt, bpt = C.ptr[(j // 8) % 2]
            for i in range(8):
                kw = dict(writes=[bpt]) if i == 0 else dict(pwrites=[bpt])
                P.op("pe", lambda e, pt=pt, i=i, j=j: e.transpose(out=pt[:, i * 128:(i + 1) * 128],
                                                                   in_=hb[:, (j + i) * 128:(j + i + 1) * 128],
                                                                   identity=C.identb[:]),
                     reads=[bhb, C.bconst], **kw)
            kw = dict(writes=[bhT]) if (s == 0 and j == 0) else dict(pwrites=[bhT])
            P.op("act", lambda e, pt=pt, j=j, s=s: e.activation(
                out=hT[:, j:j + 8, s * 128:(s + 1) * 128],
                in_=pt[:].rearrange("p (a b) -> p a b", a=8), func=AF.Copy),
                reads=[bpt], **kw)


def ffn_phase(P, C, xres, bxres, w_up, w_down, g2rep, ntiles):
    with P.scope():
        alloc_psum(P, C)
        alloc_norm(P, C)
        alloc_wstream(P, C)
        uT = P.sbuf("uT", [128, DFF // 128, TT], BF16)
        buT = [P.buf("uT%d" % i) for i in range(DFF // 128)]
        tmp = [(P.sbuf("rtmp%d" % i, [128, 512], F32), P.buf("rtmp%d" % i)) for i in range(2)]
        xo = [(P.sbuf("xo%d" % i, [128, 512], F32), P.buf("xo%d" % i)) for i in range(2)]
        P.dma(C.gsb[0][:], g2rep, writes=[C.gsb[1]])
        ti = 0
        oi = 0
        for tt in range(ntiles):
            x_tile = xres[tt * TT:(tt + 1) * TT, :]
            bx = bxres[tt]
            norm_T(P, C, x_tile, bx)
            hT, bhT = C.hT
            for blk in range(DFF // 512):
                wb, bwb = load_w_block(P, C, w_up[:, blk * 512:(blk + 1) * 512])
                for j in range(4):
                    ps, bps = C.acc[j]
                    fc = blk * 4 + j
                    for kc in range(KC):
                        kw = dict(writes=[bps]) if kc == 0 else dict(pwrites=[bps])
                        P.op("pe", lambda e, ps=ps, wb=wb, kc=kc, j=j: e.matmul(
                            ps[:], lhsT=wb[:, kc, j * 128:(j + 1) * 128], rhs=hT[:, kc, :],
                            start=(kc == 0), stop=(kc == KC - 1)), reads=[bwb, bhT], **kw)
                    t, bt = tmp[ti % 2]
                    ti += 1
                    P.op("act", lambda e, t=t, ps=ps: e.activation(out=t[:], in_=ps[:], func=AF.Relu),
                         reads=[bps], writes=[bt])
                    P.op("pool", lambda e, t=t, fc=fc: e.tensor_tensor(out=uT[:, fc, :], in0=t[:], in1=t[:], op=ALU.mult),
                         reads=[bt], writes=[buT[fc]])
            for cb in range(D // 512):
                for fb in range(DFF // 2048):
                    wb, bwb = load_w_block(P, C, w_down[fb * 2048:(fb + 1) * 2048, cb * 512:(cb + 1) * 512])
                    for s in range(NSUB):
                        ps, bps = C.acc[s]
                        for fc in range(16):
                            first = (fb == 0 and fc == 0)
                            last = (fb == DFF // 2048 - 1 and fc == 15)
                            kw = dict(writes=[bps]) if first else dict(pwrites=[bps])
                            P.op("pe", lambda e, ps=ps, wb=wb, fc=fc, fb=fb, s=s, first=first, last=last: e.matmul(
                                ps[:], lhsT=uT[:, fb * 16 + fc, s * 128:(s + 1) * 128], rhs=wb[:, fc, :],
                                start=first, stop=last), reads=[bwb, buT[fb * 16 + fc]], **kw)
                for s in range(NSUB):
                    ps, bps = C.acc[s]
                    o, bo = xo[oi % 2]
                    oi += 1
                    rows = slice(tt * TT + s * 128, tt * TT + (s + 1) * 128)
                    P.dma(o[:], xres[rows, cb * 512:(cb + 1) * 512], reads=[bx], writes=[bo])
                    P.op("dve", lambda e, o=o, ps=ps: e.tensor_tensor(out=o[:], in0=o[:], in1=ps[:], op=ALU.add),
                         reads=[bps, bo], writes=[bo])
                    P.dma(xres[rows, cb * 512:(cb + 1) * 512], o[:], reads=[bo], pwrites=[bx])


def proj_phase(P, C, x, bx, w_in, g1rep, z_out, bz, ntiles, ncols):
    with P.scope():
        alloc_psum(P, C)
        alloc_norm(P, C)
        alloc_wstream(P, C)
        zo = [(P.sbuf("zo%d" % i, [128, 512], F32), P.buf("zo%d" % i)) for i in range(4)]
        P.dma(C.gsb[0][:], g1rep, writes=[C.gsb[1]])
        oi = 0
        for tt in range(ntiles):
            norm_T(P, C, x[tt * TT:(tt + 1) * TT, :], bx)
            hT, bhT = C.hT
            for c0 in range(0, ncols, 512):
                nc_ = min(512, ncols - c0)
                wb, bwb = load_w_block(P, C, w_in[:, c0:c0 + nc_], ncol=nc_)
                for s in range(NSUB):
                    ps, bps = C.acc[s]
                    for kc in range(KC):
                        kw = dict(writes=[bps]) if kc == 0 else dict(pwrites=[bps])
                        P.op("pe", lambda e, ps=ps, wb=wb, kc=kc, s=s, nc_=nc_: e.matmul(
                            ps[:, 0:nc_], lhsT=hT[:, kc, s * 128:(s + 1) * 128], rhs=wb[:, kc, 0:nc_],
                            start=(kc == 0), stop=(kc == KC - 1)), reads=[bwb, bhT], **kw)
                    o, bo = zo[oi % 4]
                    oi += 1
                    if oi % 2:
                        P.op("act", lambda e, o=o, ps=ps, nc_=nc_: e.activation(out=o[:, 0:nc_], in_=ps[:, 0:nc_], func=AF.Copy),
                             reads=[bps], writes=[bo])
                    else:
                        P.op("dve", lambda e, o=o, ps=ps, nc_=nc_: e.tensor_copy(out=o[:, 0:nc_], in_=ps[:, 0:nc_]),
                             reads=[bps], writes=[bo])
                    rows = slice(tt * TT + s * 128, tt * TT + (s + 1) * 128)
                    P.dma(z_out[rows, c0:c0 + nc_], o[:, 0:nc_], reads=[bo], pwrites=[bz])


def merge_phase(P, C, xres, bxres, yretT, yrwT, gaT, gbT, w_a, w_b, w_out, ntiles):
    with P.scope():
        alloc_psum(P, C)
        alloc_wstream(P, C)
        yst = [(P.sbuf("yst%d" % i, [128, 8, TT], F32), P.buf("yst%d" % i)) for i in range(2)]
        yb = [(P.sbuf("yb%d" % i, [128, 8, TT], BF16), P.buf("yb%d" % i)) for i in range(2)]
        gt = [(P.sbuf("gt%d" % i, [128, TT], F32), P.buf("gt%d" % i)) for i in range(4)]
        t12 = [(P.sbuf("t12_%d" % i, [128, TT], F32), P.buf("t12_%d" % i)) for i in range(4)]
        mT = P.sbuf("mT", [128, KC, TT], BF16)
        bmT = [P.buf("mT%d" % i) for i in range(KC)]
        xo = [(P.sbuf("xo%d" % i, [128, 512], F32), P.buf("xo%d" % i)) for i in range(2)]
        gi = 0
        oi = 0
        for tt in range(ntiles):
            tok = slice(tt * TT, (tt + 1) * TT)
            for i, src in enumerate((yretT, yrwT)):
                st, bst = yst[i]
                P.dma(st[:], src.rearrange("(kc p) t -> p kc t", p=128)[:, :, tok], writes=[bst])
                eng = "pool" if i == 0 else "dve"
                P.op(eng, lambda e, st=st, i=i: e.tensor_copy(out=yb[i][0][:], in_=st[:]), reads=[bst], writes=[yb[i][1]])
            for db in range(D // 512):
                wa, bwa = load_w_block(P, C, w_a[:, db * 512:(db + 1) * 512], nkc=8)
                wbb, bwbb = load_w_block(P, C, w_b[:, db * 512:(db + 1) * 512], nkc=8)
                for j in range(4):
                    dc = db * 4 + j
                    res = []
                    for i, (w_, bw_, gT) in enumerate(((wa, bwa, gaT), (wbb, bwbb, gbT))):
                        ps, bps = C.acc[(2 * j + i) % 6]
                        for kc in range(8):
                            kw = dict(writes=[bps]) if kc == 0 else dict(pwrites=[bps])
                            P.op("pe", lambda e, ps=ps, w_=w_, kc=kc, j=j, i=i: e.matmul(
                                ps[:], lhsT=w_[:, kc, j * 128:(j + 1) * 128], rhs=yb[i][0][:, kc, :],
                                start=(kc == 0), stop=(kc == 7)), reads=[bw_, yb[i][1]], **kw)
                        g, bg = gt[gi % 4]
                        t, bt = t12[gi % 4]
                        gi += 1
                        P.dma(g[:], gT[dc * 128:(dc + 1) * 128, tok], writes=[bg])
                        P.op("act", lambda e, g=g: e.activation(out=g[:], in_=g[:], func=AF.Sigmoid), reads=[bg], writes=[bg])
                        P.op("dve", lambda e, t=t, g=g, ps=ps: e.tensor_tensor(out=t[:], in0=g[:], in1=ps[:], op=ALU.mult),
                             reads=[bg, bps], writes=[bt])
                        res.append((t, bt))
                    P.op("pool", lambda e, dc=dc, a=res[0][0], b=res[1][0]: e.tensor_tensor(out=mT[:, dc, :], in0=a[:], in1=b[:], op=ALU.add),
                         reads=[res[0][1], res[1][1]], writes=[bmT[dc]])
            bx = bxres[tt]
            for cb in range(D // 512):
                wb, bwb = load_w_block(P, C, w_out[:, cb * 512:(cb + 1) * 512])
                for s in range(NSUB):
                    ps, bps = C.acc[s]
                    for kc in range(KC):
                        kw = dict(writes=[bps]) if kc == 0 else dict(pwrites=[bps])
                        P.op("pe", lambda e, ps=ps, wb=wb, kc=kc, s=s: e.matmul(
                            ps[:], lhsT=mT[:, kc, s * 128:(s + 1) * 128], rhs=wb[:, kc, :],
                            start=(kc == 0), stop=(kc == KC - 1)), reads=[bwb, bmT[kc]], **kw)
                    o, bo = xo[oi % 2]
                    oi += 1
                    rows = slice(tt * TT + s * 128, tt * TT + (s + 1) * 128)
                    P.dma(o[:], xres[rows, cb * 512:(cb + 1) * 512], reads=[bx], writes=[bo])
                    P.op("dve", lambda e, o=o, ps=ps: e.tensor_tensor(out=o[:], in0=o[:], in1=ps[:], op=ALU.add),
                         reads=[bps, bo], writes=[bo])
                    P.dma(xres[rows, cb * 512:(cb + 1) * 512], o[:], reads=[bo], pwrites=[bx])


def final_norm_phase(P, C, xres, bxres, grep, out, bout, ntiles):
    with P.scope():
        alloc_psum(P, C)
        alloc_norm(P, C)
        gsb, bg = C.gsb
        P.dma(gsb[:], grep, writes=[bg])
        junk, bjunk = C.junk
        ob = [(P.sbuf("fo%d" % i, [128, D], F32), P.buf("fo%d" % i)) for i in range(2)]
        for r in range(ntiles * NSUB):
            xt, bxt = C.xt[r % 2]
            ss, bss = C.ss[r % 2]
            o, bo = ob[r % 2]
            rows = slice(r * 128, (r + 1) * 128)
            P.dma(xt[:], xres[rows, :], reads=[bxres[r // NSUB]], writes=[bxt])
            P.op("act", lambda e, xt=xt, ss=ss: e.activation(out=junk[:], in_=xt[:], func=AF.Square, accum_out=ss[:, 0:1]),
                 reads=[bxt], writes=[bjunk, bss])
            P.op("act", lambda e, ss=ss: e.activation(out=ss[:, 1:2], in_=ss[:, 0:1], func=AF.Sqrt, scale=1.0 / D, bias=C.epsb[:, 0:1]),
                 reads=[bss, C.bconst], pwrites=[bss])
            P.op("dve", lambda e, ss=ss: e.reciprocal(out=ss[:, 1:2], in_=ss[:, 1:2]), reads=[bss], pwrites=[bss])
            P.op("dve", lambda e, xt=xt, ss=ss, o=o: e.scalar_tensor_tensor(out=o[:], in0=xt[:], scalar=ss[:, 1:2], in1=gsb[:],
                                                                             op0=ALU.mult, op1=ALU.mult),
                 reads=[bxt, bss, bg], writes=[bo])
            P.dma(out[rows, :], o[:], reads=[bo], pwrites=[bout])


def build_C(N, final):
    nc = bass.Bass("TRN2", target_bir_lowering=False)
    dt = lambda n, s, k="ExternalInput": nc.dram_tensor(n, s, F32, kind=k).ap()
    x = dt("x", [N, D])
    yretT = dt("yretT", [1024, N]); yrwT = dt("yrwT", [1024, N])
    gaT = dt("gaT", [D, N]); gbT = dt("gbT", [D, N])
    w_a = dt("w_a", [1024, D]); w_b = dt("w_b", [1024, D]); w_out = dt("w_out", [D, D])
    w_up = dt("w_up", [D, DFF]); w_down = dt("w_down", [DFF, D])
    g2 = dt("g2", [128, D]); gf = dt("gf", [128, D]); identf = dt("identf", [128, 128])
    y = dt("y", [N, D], "ExternalOutput")
    xres = nc.dram_tensor("xres", [N, D], F32).ap()
    P = Prog(nc); C = Ctx()
    setup_common(P, C, {"identf": identf})
    nt = N // TT
    bxres = [P.buf() for _ in range(nt)]
    for tt in range(nt):
        P.dma(xres[tt * TT:(tt + 1) * TT, :], x[tt * TT:(tt + 1) * TT, :], writes=[bxres[tt]])
    merge_phase(P, C, xres, bxres, yretT, yrwT, gaT, gbT, w_a, w_b, w_out, nt)
    ffn_phase(P, C, xres, bxres, w_up, w_down, g2, nt)
    bout = P.buf()
    if final:
        final_norm_phase(P, C, xres, bxres, gf, y, bout, nt)
    else:
        for tt in range(nt):
            P.dma(y[tt * TT:(tt + 1) * TT, :], xres[tt * TT:(tt + 1) * TT, :], reads=[bxres[tt]], pwrites=[bout])
    P.wait_all("sp", [bout])
    P.emit(); P.close()
    return nc


def build_A(N, ncols=11520):
    nc = bass.Bass("TRN2", target_bir_lowering=False)
    dt = lambda n, s, k="ExternalInput": nc.dram_tensor(n, s, F32, kind=k).ap()
    x = dt("x", [N, D]); w = dt("w_in", [D, ncols]); g = dt("g1", [128, D]); identf = dt("identf", [128, 128])
    z = dt("z", [N, ncols], "ExternalOutput")
    P = Prog(nc); C = Ctx()
    setup_common(P, C, {"identf": identf})
    bz = P.buf()
    proj_phase(P, C, x, None, w, g, z, bz, N // TT, ncols)
    P.wait_all("sp", [bz])
    P.emit(); P.close()
    return nc


TWO_PI = 6.283185307179586
LN_EPS = 64e-5
GN_EPS = 1e-5


def scan_phase(P, S, A):
    with P.scope():
        ptm = A.ptm
        mu_l = A.mu_l
        mu_vT = A.mu_vT
        wa_up = A.wa_up
        g_up = A.g_up
        sel = A.sel
        identf = A.identf
        invf = A.invf
        gam = A.gam
        gn = A.gn
        selr = A.selr
        y_rw = A.y_rw
        y_ret = A.y_ret
        pos = A.pos
        sb = P.sbuf
        bc = P.buf("const")
        c_ptm = sb("ptm", [128, 10, 256], F32); c_mul = sb("mul", [128, 2], F32); c_muv = sb("muv", [64, 4], F32)
        c_wa = sb("waup", [128, 256], F32); c_gu = sb("gup", [128, 256], F32)
        c_sel = sb("sel", [128, 128, 64], F32); c_id = sb("id", [128, 128], F32)
        c_invf = sb("invf", [128, 64], F32); c_gam = sb("gam", [128, 2, 128], F32); c_gn = sb("gn", [128, 2, 256], F32)
        c_selr = sb("selr", [128, 128, 128], F32)
        c_omka = sb("omka", [128, 256], F32); c_bias = sb("cbias", [128, 4], F32)
        for t_, a_ in ((c_ptm, ptm), (c_mul, mu_l), (c_muv, mu_vT), (c_wa, wa_up), (c_gu, g_up), (c_sel, sel), (c_id, identf),
                       (c_invf, invf), (c_gam, gam), (c_gn, gn), (c_selr, selr)):
            P.dma(t_[:], a_, pwrites=[bc])
        P.op("dve", lambda e: e.tensor_scalar(out=c_omka[:], in0=c_ptm[:, 6, :], scalar1=-1.0, scalar2=1.0, op0=ALU.mult, op1=ALU.add),
             reads=[bc], pwrites=[bc])
        P.op("dve", lambda e: e.memset(c_bias[:, 0:1], -3.141592653589793), pwrites=[bc])
        P.op("dve", lambda e: e.memset(c_bias[:, 1:2], LN_EPS), pwrites=[bc])
        P.op("dve", lambda e: e.memset(c_bias[:, 2:3], GN_EPS), pwrites=[bc])

        Srw = sb("Srw", [64, 4, 64], F32); bS = P.buf("Srw")
        Rr = sb("Rr", [128, 2, 128], F32); bR = P.buf("Rr")
        P.op("dve", lambda e: e.memset(Srw[:], 0.0), writes=[bS])
        P.op("pool", lambda e: e.memset(Rr[:], 0.0), writes=[bR])

        psRow = [[(P.psum("prow%d_%d" % (i, j), [64, 512], F32), P.buf()) for j in range(3)] for i in range(2)]
        psMisc = (P.psum("pmisc", [128, 512], F32), P.buf("pmisc"))
        psRet = (P.psum("pret", [128, 512], F32), P.buf("pret"))

        def T(name, shape, n=1, dtype=F32):
            return [(sb(name + str(i), shape, dtype), P.buf(name + str(i))) for i in range(n)]
        zt = T("zt", [128, 3, 256], 2); zp = T("zp", [128, 3, 256], 2)
        vT = T("vT", [64, 4, 128], 2); vTp = T("vTp", [64, 4, 128], 2)
        lT = T("lT", [128, 2, 128], 2); lTp = T("lTp", [128, 2, 128], 2)
        rows = T("rows", [128, 5, 256], 2)
        yT = T("yT", [64, 4, 128], 2)
        w1 = T("w1", [128, 256], 6)
        sm = T("sm", [128, 16], 4)
        vtm = T("vtm", [128, 256], 2)
        gtm = T("gtm", [128, 256], 2)
        bon = T("bon", [128, 4], 2)
        tmpS = T("tmpS", [64, 4, 64], 2); saS = T("saS", [64, 4], 2)
        tmpP = T("tmpP", [64, 4, 64], 2)
        orw = T("orw", [128, 256], 2)
        st6 = T("st6", [128, 4, 6], 2); mv = T("mv", [128, 4, 2], 2)
        rqt = T("rqt", [128, 256], 2); rkt = T("rkt", [128, 256], 2); rgt = T("rgt", [128, 256], 2)
        rvt = T("rvt", [128, 2, 128], 2); post = T("post", [128, 2], 2, I32); posf = T("posf", [128, 2], 2)
        cs = T("cs", [128, 2, 64], 2)
        rrow = T("rrow", [128, 2, 256], 2)
        rrs = T("rrs", [128, 512], 2)
        ryT = T("ryT", [128, 2, 128], 2)
        tmpR = T("tmpR", [128, 2, 128], 2); tmpR2 = T("tmpR2", [128, 2, 128], 2)
        oret = T("oret", [128, 256], 2)
        w2 = T("w2", [128, 256], 4)
        kit = T("kit", [128, 64], 2, I32)

        NCH = S // 128
        for c in range(NCH):
            i2 = c % 2
            tok = slice(c * 128, (c + 1) * 128)
            z, bz = zt[i2]; zpp, bzp = zp[i2]
            P.dma(z[:], A.ztm(c), writes=[bz]); P.dma(zpp[:], A.ztm_p(c), writes=[bzp])
            v_, bv_ = vT[i2]; vp_, bvp_ = vTp[i2]
            P.dma(v_[:], A.zvT(c), writes=[bv_]); P.dma(vp_[:], A.zvT_p(c), writes=[bvp_])
            l_, bl_ = lT[i2]; lp_, blp_ = lTp[i2]
            P.dma(l_[:], A.zlT(c), writes=[bl_]); P.dma(lp_[:], A.zlT_p(c), writes=[blp_])
            P.op("dve", lambda e, z=z, zpp=zpp: e.tensor_tensor(out=zpp[:], in0=zpp[:], in1=z[:], op=ALU.subtract), reads=[bz, bzp], writes=[bzp])
            P.op("dve", lambda e, z=z, zpp=zpp: e.tensor_tensor(out=zpp[:], in0=zpp[:], in1=c_ptm[:, 0:3, :], op=ALU.mult), reads=[bzp, bc], writes=[bzp])
            P.op("dve", lambda e, z=z, zpp=zpp: e.tensor_tensor(out=z[:], in0=z[:], in1=zpp[:], op=ALU.add), reads=[bz, bzp], writes=[bz])
            P.op("pool", lambda e, v_=v_, vp_=vp_: e.tensor_tensor(out=vp_[:], in0=vp_[:], in1=v_[:], op=ALU.subtract), reads=[bv_, bvp_], writes=[bvp_])
            P.op("pool", lambda e, v_=v_, vp_=vp_: e.tensor_tensor(out=vp_[:], in0=vp_[:], in1=c_muv[:].unsqueeze(2).to_broadcast([64, 4, 128]), op=ALU.mult), reads=[bvp_, bc], writes=[bvp_])
            P.op("pool", lambda e, v_=v_, vp_=vp_: e.tensor_tensor(out=v_[:], in0=v_[:], in1=vp_[:], op=ALU.add), reads=[bv_, bvp_], writes=[bv_])
            P.op("pool", lambda e, l_=l_, lp_=lp_: e.tensor_tensor(out=lp_[:], in0=lp_[:], in1=l_[:], op=ALU.subtract), reads=[bl_, blp_], writes=[blp_])
            P.op("pool", lambda e, l_=l_, lp_=lp_: e.tensor_tensor(out=lp_[:], in0=lp_[:], in1=c_mul[:].unsqueeze(2).to_broadcast([128, 2, 128]), op=ALU.mult), reads=[blp_, bc], writes=[blp_])
            P.op("pool", lambda e, l_=l_, lp_=lp_: e.tensor_tensor(out=l_[:], in0=l_[:], in1=lp_[:], op=ALU.add), reads=[bl_, blp_], writes=[bl_])
            P.op("act", lambda e, l_=l_: e.activation(out=l_[0:64, 0, :], in_=l_[0:64, 0, :], func=AF.Tanh), reads=[bl_], writes=[bl_])
            P.op("act", lambda e, l_=l_: e.activation(out=l_[:, 1, :], in_=l_[:, 1, :], func=AF.Sigmoid), reads=[bl_], writes=[bl_])
            pm, bpm = psMisc
            rw_, brw_ = rows[i2]
            P.op("pe", lambda e, l_=l_: e.matmul(pm[:, 0:256], lhsT=l_[0:64, 0, :], rhs=c_wa[0:64, :], start=True, stop=True), reads=[bl_, bc], writes=[bpm])
            a1, ba1 = w1[0]
            P.op("dve", lambda e: e.tensor_tensor(out=a1[:], in0=pm[:, 0:256], in1=c_ptm[:, 3, :], op=ALU.add), reads=[bpm, bc], writes=[ba1])
            P.op("act", lambda e: e.activation(out=a1[:], in_=a1[:], func=AF.Sigmoid), reads=[ba1], writes=[ba1])
            P.op("act", lambda e, rw_=rw_: e.activation(out=rw_[:, 0, :], in_=a1[:], func=AF.Exp, scale=-0.6065306597126334), reads=[ba1], writes=[brw_])
            P.op("pe", lambda e, l_=l_: e.matmul(pm[:, 0:256], lhsT=l_[64:128, 0, :], rhs=c_wa[64:128, :], start=True, stop=True), reads=[bl_, bc, ba1], writes=[bpm])
            al, bal = w1[1]
            P.op("dve", lambda e: e.tensor_tensor(out=al[:], in0=pm[:, 0:256], in1=c_ptm[:, 4, :], op=ALU.add), reads=[bpm, bc], writes=[bal])
            P.op("act", lambda e: e.activation(out=al[:], in_=al[:], func=AF.Sigmoid), reads=[bal], writes=[bal])
            g_, bg_ = gtm[i2]
            P.op("pe", lambda e, l_=l_: e.matmul(pm[:, 0:256], lhsT=l_[:, 1, :], rhs=c_gu[:], start=True, stop=True), reads=[bl_, bc, bal], writes=[bpm])
            P.op("act", lambda e, g_=g_: e.activation(out=g_[:], in_=pm[:, 0:256], func=AF.Copy), reads=[bpm], writes=[bg_])
            kk, bkk = w1[2]; k2, bk2 = w1[3]
            s_, bs_ = sm[i2]
            P.op("dve", lambda e, z=z: e.tensor_tensor(out=kk[:], in0=z[:, 1, :], in1=c_ptm[:, 5, :], op=ALU.mult), reads=[bz, bc], writes=[bkk])
            P.op("dve", lambda e: e.tensor_tensor(out=k2[:], in0=kk[:], in1=kk[:], op=ALU.mult), reads=[bkk], writes=[bk2])
            P.op("dve", lambda e, s_=s_: e.tensor_reduce(out=s_[:, 0:4], in_=k2[:].rearrange("p (h k) -> p h k", h=4), axis=AX.X, op=ALU.add), reads=[bk2], writes=[bs_])
            P.op("act", lambda e, s_=s_: e.activation(out=s_[:, 0:4], in_=s_[:, 0:4], func=AF.Sqrt), reads=[bs_], writes=[bs_])
            P.op("dve", lambda e, s_=s_: e.tensor_scalar(out=s_[:, 0:4], in0=s_[:, 0:4], scalar1=1e-12, scalar2=None, op0=ALU.max), reads=[bs_], writes=[bs_])
            P.op("dve", lambda e, s_=s_: e.reciprocal(out=s_[:, 0:4], in_=s_[:, 0:4]), reads=[bs_], writes=[bs_])
            P.op("dve", lambda e, s_=s_: e.tensor_tensor(out=kk[:].rearrange("p (h k) -> p h k", h=4), in0=kk[:].rearrange("p (h k) -> p h k", h=4),
                                                         in1=s_[:, 0:4].unsqueeze(2).to_broadcast([128, 4, 64]), op=ALU.mult), reads=[bkk, bs_], writes=[bkk])
            P.op("dve", lambda e, rw_=rw_: e.tensor_scalar(out=rw_[:, 1, :], in0=kk[:], scalar1=-1.0, scalar2=None, op0=ALU.mult), reads=[bkk], pwrites=[brw_])
            P.op("dve", lambda e, rw_=rw_: e.tensor_tensor(out=rw_[:, 2, :], in0=kk[:], in1=al[:], op=ALU.mult), reads=[bkk, bal], pwrites=[brw_])
            P.op("dve", lambda e: e.tensor_tensor(out=k2[:], in0=al[:], in1=c_ptm[:, 6, :], op=ALU.mult), reads=[bal, bc], writes=[bk2])
            P.op("dve", lambda e: e.tensor_tensor(out=k2[:], in0=k2[:], in1=c_omka[:], op=ALU.add), reads=[bk2, bc], writes=[bk2])
            P.op("dve", lambda e, rw_=rw_, z=z: e.tensor_tensor(out=rw_[:, 3, :], in0=z[:, 1, :], in1=k2[:], op=ALU.mult), reads=[bz, bk2], pwrites=[brw_])
            P.op("dve", lambda e, rw_=rw_, z=z: e.tensor_copy(out=rw_[:, 4, :], in_=z[:, 0, :]), reads=[bz], pwrites=[brw_])
            bn_, bbn_ = bon[i2]
            P.op("dve", lambda e, z=z: e.tensor_tensor(out=k2[:], in0=z[:, 0, :], in1=c_ptm[:, 7, :], op=ALU.mult), reads=[bz, bc], writes=[bk2])
            P.op("dve", lambda e, rw_=rw_: e.tensor_tensor(out=k2[:], in0=k2[:], in1=rw_[:, 3, :], op=ALU.mult), reads=[bk2, brw_], writes=[bk2])
            P.op("dve", lambda e, bn_=bn_: e.tensor_reduce(out=bn_[:], in_=k2[:].rearrange("p (h k) -> p h k", h=4), axis=AX.X, op=ALU.add), reads=[bk2], writes=[bbn_])
            y_, by_ = yT[i2]
            for t in range(128):
                pr = psRow[t % 2]
                for j, (c0, n) in enumerate(((0, 512), (512, 512), (1024, 256))):
                    P.op("pe", lambda e, pr=pr, j=j, c0=c0, n=n, rw_=rw_, t=t: e.matmul(
                        pr[j][0][:, 0:n], lhsT=c_sel[:, t, :], rhs=rw_[:].rearrange("p a b -> p (a b)")[:, c0:c0 + n], start=True, stop=True),
                        reads=[brw_, bc], writes=[pr[j][1]])
                row = lambda r: (pr[(r * 256) // 512][0][:, (r * 256) % 512:(r * 256) % 512 + 256].rearrange("p (h k) -> p h k", h=4), pr[(r * 256) // 512][1])
                wr, bwr = row(0); ar, bar = row(1); br_, bbr = row(2); kr, bkr = row(3); rr, brr = row(4)
                tS, btS = tmpS[t % 2]; sa, bsa = saS[t % 2]; tP, btP = tmpP[t % 2]
                P.op("dve", lambda e, tS=tS, ar=ar: e.tensor_tensor(out=tS[:], in0=Srw[:], in1=ar, op=ALU.mult), reads=[bS, bar], writes=[btS])
                P.op("dve", lambda e, tS=tS, sa=sa: e.tensor_reduce(out=sa[:], in_=tS[:], axis=AX.X, op=ALU.add), reads=[btS], writes=[bsa])
                P.op("dve", lambda e, wr=wr: e.tensor_tensor(out=Srw[:], in0=Srw[:], in1=wr, op=ALU.mult), reads=[bS, bwr], writes=[bS])
                P.op("dve", lambda e, tS=tS, sa=sa, br_=br_: e.tensor_tensor(out=tS[:], in0=br_, in1=sa[:].unsqueeze(2).to_broadcast([64, 4, 64]), op=ALU.mult),
                     reads=[bbr, bsa], writes=[btS])
                P.op("dve", lambda e, tS=tS: e.tensor_tensor(out=Srw[:], in0=Srw[:], in1=tS[:], op=ALU.add), reads=[bS, btS], writes=[bS])
                P.op("dve", lambda e, tP=tP, kr=kr, v_=v_, t=t: e.tensor_tensor(out=tP[:], in0=kr, in1=v_[:, :, t:t + 1].to_broadcast([64, 4, 64]), op=ALU.mult),
                     reads=[bkr, bv_], writes=[btP])
                P.op("dve", lambda e, tP=tP: e.tensor_tensor(out=Srw[:], in0=Srw[:], in1=tP[:], op=ALU.add), reads=[bS, btP], writes=[bS])
                P.op("dve", lambda e, tS=tS, rr=rr: e.tensor_tensor(out=tS[:], in0=Srw[:], in1=rr, op=ALU.mult), reads=[bS, brr], writes=[btS])
                kw = dict(writes=[by_]) if t == 0 else dict(pwrites=[by_])
                P.op("dve", lambda e, tS=tS, y_=y_, t=t: e.tensor_reduce(out=y_[:, :, t], in_=tS[:], axis=AX.X, op=ALU.add), reads=[btS], **kw)
            vt_, bvt_ = vtm[i2]
            o_, bo_ = orw[i2]
            for h in range(4):
                P.op("pe", lambda e, h=h, y_=y_: e.transpose(out=pm[:, h * 64:(h + 1) * 64], in_=y_[:, h, :], identity=c_id[0:64, 0:64]),
                     reads=[by_, bc] + ([bg_] if h == 0 else []), **(dict(writes=[bpm]) if h == 0 else dict(pwrites=[bpm])))
            s6, bs6 = st6[i2]; m_, bm_ = mv[i2]
            for h in range(4):
                P.op("dve", lambda e, h=h, s6=s6: e.bn_stats(out=s6[:, h, :], in_=pm[:, h * 64:(h + 1) * 64]), reads=[bpm], **(dict(writes=[bs6]) if h == 0 else dict(pwrites=[bs6])))
            for h in range(4):
                P.op("dve", lambda e, h=h, s6=s6, m_=m_: e.bn_aggr(out=m_[:, h, :], in_=s6[:, h, :]), reads=[bs6], **(dict(writes=[bm_]) if h == 0 else dict(pwrites=[bm_])))
            P.op("act", lambda e, m_=m_: e.activation(out=m_[:, :, 1], in_=m_[:, :, 1], func=AF.Sqrt, bias=c_bias[:, 1:2]), reads=[bm_, bc], writes=[bm_])
            P.op("dve", lambda e, m_=m_: e.reciprocal(out=m_[:, :, 1], in_=m_[:, :, 1]), reads=[bm_], writes=[bm_])
            for h in range(4):
                P.op("dve", lambda e, h=h, m_=m_, o_=o_: e.tensor_scalar(out=o_[:, h * 64:(h + 1) * 64], in0=pm[:, h * 64:(h + 1) * 64],
                                                                         scalar1=m_[:, h, 0:1], scalar2=m_[:, h, 1:2], op0=ALU.subtract, op1=ALU.mult),
                     reads=[bpm, bm_], **(dict(writes=[bo_]) if h == 0 else dict(pwrites=[bo_])))
            P.op("dve", lambda e, o_=o_: e.tensor_tensor(out=o_[:], in0=o_[:], in1=c_ptm[:, 8, :], op=ALU.mult), reads=[bo_, bc], writes=[bo_])
            P.op("dve", lambda e, o_=o_: e.tensor_tensor(out=o_[:], in0=o_[:], in1=c_ptm[:, 9, :], op=ALU.add), reads=[bo_, bc], writes=[bo_])
            P.op("dve", lambda e, z=z, bn_=bn_: e.tensor_tensor(out=k2[:].rearrange("p (h k) -> p h k", h=4), in0=z[:, 2, :].rearrange("p (h k) -> p h k", h=4),
                                                                in1=bn_[:].unsqueeze(2).to_broadcast([128, 4, 64]), op=ALU.mult), reads=[bz, bbn_], writes=[bk2])
            P.op("dve", lambda e, o_=o_: e.tensor_tensor(out=o_[:], in0=o_[:], in1=k2[:], op=ALU.add), reads=[bo_, bk2], writes=[bo_])
            P.op("dve", lambda e, o_=o_, g_=g_: e.tensor_tensor(out=o_[:], in0=o_[:], in1=g_[:], op=ALU.mult), reads=[bo_, bg_], writes=[bo_])
            P.dma(y_rw[tok, :], o_[:], reads=[bo_])

            q_, bq_ = rqt[i2]; k_, bk_ = rkt[i2]; gr_, bgr_ = rgt[i2]; rv_, brv_ = rvt[i2]
            pt_, bpt_ = post[i2]; pf_, bpf_ = posf[i2]; cs_, bcs_ = cs[i2]; rr_, brr_ = rrow[i2]
            P.dma(q_[:], A.rq(c), writes=[bq_]); P.dma(k_[:], A.rk(c), writes=[bk_]); P.dma(gr_[:], A.rgr(c), writes=[bgr_])
            P.dma(rv_[:], A.rvT(c), writes=[brv_]); P.dma(pt_[:, 0:1], pos[tok, :], writes=[bpt_])
            P.op("pool", lambda e, pt_=pt_, pf_=pf_: e.tensor_copy(out=pf_[:, 0:1], in_=pt_[:, 0:1]), reads=[bpt_], writes=[bpf_])
            ang, bang = w2[0]; ang2, bang2 = w2[1]
            P.op("dve", lambda e, pf_=pf_: e.tensor_scalar(out=ang[:, 0:64], in0=c_invf[:], scalar1=pf_[:, 0:1], scalar2=None, op0=ALU.mult), reads=[bpf_, bc], writes=[bang])
            P.op("dve", lambda e: e.tensor_scalar(out=ang2[:, 0:64], in0=ang[:, 0:64], scalar1=1.5707963267948966, scalar2=None, op0=ALU.add), reads=[bang], writes=[bang2])
            ki_, bki_ = kit[i2]
            for (a_, ba_, col) in ((ang2, bang2, 0), (ang, bang, 1)):
                P.op("dve", lambda e, a_=a_: e.tensor_scalar(out=a_[:, 64:128], in0=a_[:, 0:64], scalar1=1.0 / TWO_PI, scalar2=None, op0=ALU.mult), reads=[ba_], writes=[ba_])
                P.op("dve", lambda e, a_=a_, ki_=ki_: e.tensor_copy(out=ki_[:], in_=a_[:, 64:128]), reads=[ba_], writes=[bki_])
                P.op("dve", lambda e, a_=a_, ki_=ki_: e.tensor_copy(out=a_[:, 64:128], in_=ki_[:]), reads=[bki_], writes=[ba_])
                P.op("dve", lambda e, a_=a_: e.scalar_tensor_tensor(out=a_[:, 0:64], in0=a_[:, 64:128], scalar=-TWO_PI, in1=a_[:, 0:64], op0=ALU.mult, op1=ALU.add), reads=[ba_], writes=[ba_])
                P.op("dve", lambda e, a_=a_: e.tensor_scalar(out=a_[:, 64:128], in0=a_[:, 0:64], scalar1=3.141592653589793, scalar2=-TWO_PI, op0=ALU.is_gt, op1=ALU.mult), reads=[ba_], writes=[ba_])
                P.op("dve", lambda e, a_=a_: e.tensor_tensor(out=a_[:, 0:64], in0=a_[:, 0:64], in1=a_[:, 64:128], op=ALU.add), reads=[ba_], writes=[ba_])
                kw = dict(writes=[bcs_]) if col == 0 else dict(pwrites=[bcs_])
                P.op("act", lambda e, cs_=cs_, a_=a_, col=col: e.activation(out=cs_[:, col, :], in_=a_[:, 0:64], func=AF.Sin), reads=[ba_], **kw)
            ta, bta = w2[2]; tb, btb = w2[3]
            for xi, (x_, bx_) in enumerate(((k_, bk_), (q_, bq_))):
                xv = x_[:].rearrange("p (h two d) -> p h two d", h=2, two=2)
                ov = rr_[:, xi, :].rearrange("p (h two d) -> p h two d", h=2, two=2)
                tav = ta[:, 0:128].rearrange("p (h d) -> p h d", h=2); tbv = tb[:, 0:128].rearrange("p (h d) -> p h d", h=2)
                ncb = lambda cs_=cs_: cs_[:, 0:1, :].to_broadcast([128, 2, 64])
                nsb = lambda cs_=cs_: cs_[:, 1:2, :].to_broadcast([128, 2, 64])
                kw0 = dict(writes=[brr_]) if xi == 0 else dict(pwrites=[brr_])
                P.op("pool", lambda e, xv=xv, tav=tav, ncb=ncb: e.tensor_tensor(out=tav, in0=xv[:, :, 0, :], in1=ncb(), op=ALU.mult), reads=[bx_, bcs_], writes=[bta])
                P.op("pool", lambda e, xv=xv, tbv=tbv, nsb=nsb: e.tensor_tensor(out=tbv, in0=xv[:, :, 1, :], in1=nsb(), op=ALU.mult), reads=[bx_, bcs_], writes=[btb])
                P.op("pool", lambda e, ov=ov, tav=tav, tbv=tbv: e.tensor_tensor(out=ov[:, :, 0, :], in0=tav, in1=tbv, op=ALU.subtract), reads=[bta, btb], **kw0)
                P.op("pool", lambda e, xv=xv, tav=tav, ncb=ncb: e.tensor_tensor(out=tav, in0=xv[:, :, 1, :], in1=ncb(), op=ALU.mult), reads=[bx_, bcs_], writes=[bta])
                P.op("pool", lambda e, xv=xv, tbv=tbv, nsb=nsb: e.tensor_tensor(out=tbv, in0=xv[:, :, 0, :], in1=nsb(), op=ALU.mult), reads=[bx_, bcs_], writes=[btb])
                P.op("pool", lambda e, ov=ov, tav=tav, tbv=tbv: e.tensor_tensor(out=ov[:, :, 1, :], in0=tav, in1=tbv, op=ALU.add), reads=[bta, btb], pwrites=[brr_])
            P.op("pool", lambda e, rr_=rr_: e.tensor_scalar(out=rr_[:, 0, :], in0=rr_[:, 0, :], scalar1=128.0 ** -0.5, scalar2=None, op0=ALU.mult), reads=[brr_], pwrites=[brr_])
            pr_, bpr_ = psRet
            ry_, bry_ = ryT[i2]
            for t in range(128):
                P.op("pe", lambda e, rr_=rr_, t=t: e.matmul(pr_[:], lhsT=c_selr[:, t, :], rhs=rr_[:].rearrange("p a b -> p (a b)"), start=True, stop=True),
                     reads=[brr_, bc], writes=[bpr_])
                rs_, brs_ = rrs[t % 2]
                P.op("act", lambda e, rs_=rs_: e.activation(out=rs_[:], in_=pr_[:], func=AF.Copy), reads=[bpr_], writes=[brs_])
                t1, bt1 = tmpR[t % 2]; t2, bt2 = tmpR2[t % 2]
                krow = rs_[:, 0:256].rearrange("p (h d) -> p h d", h=2); qrow = rs_[:, 256:512].rearrange("p (h d) -> p h d", h=2)
                P.op("pool", lambda e: e.tensor_tensor(out=Rr[:], in0=Rr[:], in1=c_gam[:], op=ALU.mult), reads=[bR, bc], writes=[bR])
                P.op("pool", lambda e, t1=t1, krow=krow, rv_=rv_, t=t: e.tensor_tensor(out=t1[:], in0=krow, in1=rv_[:, :, t:t + 1].to_broadcast([128, 2, 128]), op=ALU.mult),
                     reads=[brs_, brv_], writes=[bt1])
                P.op("pool", lambda e, t1=t1: e.tensor_tensor(out=Rr[:], in0=Rr[:], in1=t1[:], op=ALU.add), reads=[bR, bt1], writes=[bR])
                P.op("pool", lambda e, t2=t2, qrow=qrow: e.tensor_tensor(out=t2[:], in0=Rr[:], in1=qrow, op=ALU.mult), reads=[bR, brs_], writes=[bt2])
                kw = dict(writes=[bry_]) if t == 0 else dict(pwrites=[bry_])
                P.op("dve", lambda e, t2=t2, ry_=ry_, t=t: e.tensor_reduce(out=ry_[:, :, t], in_=t2[:], axis=AX.X, op=ALU.add), reads=[bt2], **kw)
            for h in range(2):
                P.op("pe", lambda e, h=h, ry_=ry_: e.transpose(out=pm[:, h * 128:(h + 1) * 128], in_=ry_[:, h, :], identity=c_id[:]),
                     reads=[bry_, bc], **(dict(writes=[bpm]) if h == 0 else dict(pwrites=[bpm])))
            s6, bs6 = st6[i2]; m_, bm_ = mv[i2]
            orr, borr = oret[i2]
            for h in range(2):
                P.op("dve", lambda e, h=h, s6=s6: e.bn_stats(out=s6[:, h, :], in_=pm[:, h * 128:(h + 1) * 128]), reads=[bpm], **(dict(writes=[bs6]) if h == 0 else dict(pwrites=[bs6])))
            for h in range(2):
                P.op("dve", lambda e, h=h, s6=s6, m_=m_: e.bn_aggr(out=m_[:, h, :], in_=s6[:, h, :]), reads=[bs6], **(dict(writes=[bm_]) if h == 0 else dict(pwrites=[bm_])))
            P.op("act", lambda e, m_=m_: e.activation(out=m_[:, 0:2, 1], in_=m_[:, 0:2, 1], func=AF.Sqrt, bias=c_bias[:, 2:3]), reads=[bm_, bc], writes=[bm_])
            P.op("dve", lambda e, m_=m_: e.reciprocal(out=m_[:, 0:2, 1], in_=m_[:, 0:2, 1]), reads=[bm_], writes=[bm_])
            for h in range(2):
                P.op("dve", lambda e, h=h, m_=m_, orr=orr: e.tensor_scalar(out=orr[:, h * 128:(h + 1) * 128], in0=pm[:, h * 128:(h + 1) * 128],
                                                                           scalar1=m_[:, h, 0:1], scalar2=m_[:, h, 1:2], op0=ALU.subtract, op1=ALU.mult),
                     reads=[bpm, bm_], **(dict(writes=[borr]) if h == 0 else dict(pwrites=[borr])))
            P.op("dve", lambda e, orr=orr: e.tensor_tensor(out=orr[:], in0=orr[:], in1=c_gn[:, 0, :], op=ALU.mult), reads=[borr, bc], writes=[borr])
            P.op("dve", lambda e, orr=orr: e.tensor_tensor(out=orr[:], in0=orr[:], in1=c_gn[:, 1, :], op=ALU.add), reads=[borr, bc], writes=[borr])
            P.op("act", lambda e, gr_=gr_: e.activation(out=gr_[:], in_=gr_[:], func=AF.Silu), reads=[bgr_], writes=[bgr_])
            P.op("dve", lambda e, orr=orr, gr_=gr_: e.tensor_tensor(out=orr[:], in0=orr[:], in1=gr_[:], op=ALU.mult), reads=[borr, bgr_], writes=[borr])
            P.dma(y_ret[tok, :], orr[:], reads=[borr])


def build_B(S):
    nc = bass.Bass("TRN2", target_bir_lowering=False)
    dt = lambda n, s, k="ExternalInput", d=F32: nc.dram_tensor(n, s, d, kind=k).ap()
    ztm = dt("ztm", [S, 3, 256]); ztm_p = dt("ztm_p", [S, 3, 256])
    zvT = dt("zvT", [64, 4, S]); zvT_p = dt("zvT_p", [64, 4, S])
    zlT = dt("zlT", [128, 2, S]); zlT_p = dt("zlT_p", [128, 2, S])
    ptm = dt("ptm", [128, 10, 256]); mu_l = dt("mu_l", [128, 2]); mu_vT = dt("mu_vT", [64, 4])
    wa_up = dt("wa_up", [128, 256]); g_up = dt("g_up", [128, 256])
    sel = dt("sel", [128, 128, 64]); identf = dt("identf", [128, 128])
    rq = dt("rq", [S, 256]); rk = dt("rk", [S, 256]); rvT = dt("rvT", [128, 2, S]); rgr = dt("rgr", [S, 256])
    pos = dt("pos", [S, 1], d=I32); invf = dt("invf", [128, 64]); gam = dt("gam", [128, 2, 128])
    gn = dt("gn", [128, 2, 256]); selr = dt("selr", [128, 128, 128])
    y_rw = dt("y_rw", [S, 256], "ExternalOutput"); y_ret = dt("y_ret", [S, 256], "ExternalOutput")

    P = Prog(nc)
    class A: pass
    A.ptm, A.mu_l, A.mu_vT, A.wa_up, A.g_up, A.sel, A.identf = ptm, mu_l, mu_vT, wa_up, g_up, sel, identf
    A.invf, A.gam, A.gn, A.selr, A.y_rw, A.y_ret, A.pos = invf, gam, gn, selr, y_rw, y_ret, pos
    ck = lambda c: slice(c * 128, (c + 1) * 128)
    A.ztm = lambda c: ztm[ck(c)]; A.ztm_p = lambda c: ztm_p[ck(c)]
    A.zvT = lambda c: zvT[:, :, ck(c)]; A.zvT_p = lambda c: zvT_p[:, :, ck(c)]
    A.zlT = lambda c: zlT[:, :, ck(c)]; A.zlT_p = lambda c: zlT_p[:, :, ck(c)]
    A.rq = lambda c: rq[ck(c), :]; A.rk = lambda c: rk[ck(c), :]; A.rgr = lambda c: rgr[ck(c), :]
    A.rvT = lambda c: rvT[:, :, ck(c)]
    scan_phase(P, S, A)
    P.barrier()
    P.emit(); P.close()
    return nc


def shift_prev(a, axis=0):
    out = np.zeros_like(a)
    sl_dst = [slice(None)] * a.ndim; sl_src = [slice(None)] * a.ndim
    sl_dst[axis] = slice(1, None); sl_src[axis] = slice(0, -1)
    out[tuple(sl_dst)] = a[tuple(sl_src)]
    return out


def rep128(v):
    return np.ascontiguousarray(np.broadcast_to(np.asarray(v, np.float32).reshape(1, -1), (128, v.size)))


def prep_B_rwkv(zr, zk, zv, zw, za, zg, p):
    S = zr.shape[0]
    d = {}
    ztm = np.stack([zr, zk, zv], 1).astype(np.float32)
    d["ztm"] = ztm; d["ztm_p"] = shift_prev(ztm, 0)
    zvT = np.ascontiguousarray(zv.reshape(S, 4, 64).transpose(2, 1, 0))
    d["zvT"] = zvT; d["zvT_p"] = shift_prev(zvT, 2)
    zl = np.stack([np.concatenate([zw, za], 1).T, zg.T], 1)
    zl = np.ascontiguousarray(zl.astype(np.float32))
    d["zlT"] = zl; d["zlT_p"] = shift_prev(zl, 2)
    ptm = np.stack([rep128(p[k]) for k in ("mu_r", "mu_k", "mu_v", "w0", "a0", "k_k", "k_a", "r_k", "ln_g", "ln_b")], 1)
    d["ptm"] = np.ascontiguousarray(ptm)
    d["mu_l"] = np.ascontiguousarray(np.stack([np.concatenate([p["mu_w"], p["mu_a"]]), p["mu_g"]], 1).astype(np.float32))
    d["mu_vT"] = np.ascontiguousarray(p["mu_v"].reshape(4, 64).T.astype(np.float32))
    d["wa_up"] = np.ascontiguousarray(np.concatenate([p["w_up"], p["a_up"]], 0).astype(np.float32))
    d["g_up"] = np.ascontiguousarray(p["g_up"].astype(np.float32))
    sel = np.zeros((128, 128, 64), np.float32)
    sel[np.arange(128), np.arange(128), :] = 1.0
    d["sel"] = sel
    d["identf"] = np.eye(128, dtype=np.float32)
    return d


def prep_B_ret(zq, zk, zv, zgr, pos, gn_g, gn_b, heads):
    S = zq.shape[0]
    d = {}
    d["rq"] = np.ascontiguousarray(zq, np.float32); d["rk"] = np.ascontiguousarray(zk, np.float32)
    d["rgr"] = np.ascontiguousarray(zgr, np.float32)
    d["rvT"] = np.ascontiguousarray(zv.reshape(S, 2, 128).transpose(2, 1, 0).astype(np.float32))
    d["pos"] = np.ascontiguousarray(pos.reshape(S, 1).astype(np.int32))
    inv = (10000.0 ** (-np.arange(64, dtype=np.float32) / np.float32(64))).astype(np.float32)
    d["invf"] = rep128(inv)
    gamma = 1.0 - 2.0 ** (-5.0 - np.asarray(heads, np.float64))
    d["gam"] = np.ascontiguousarray(np.broadcast_to(gamma.astype(np.float32).reshape(1, 2, 1), (128, 2, 128)))
    d["gn"] = np.ascontiguousarray(np.stack([rep128(gn_g), rep128(gn_b)], 1))
    selr = np.zeros((128, 128, 128), np.float32)
    selr[np.arange(128), np.arange(128), :] = 1.0
    d["selr"] = selr
    return d


RG4 = [[0, 1, 2, 3], [4, 5, 6, 7]]
NTM = 1536
NFM = 768


def load_w_resident(P, C, w_ap, dst, bdst, ncols, nkc=16):
    wv = w_ap.rearrange("(kc p) c -> p kc c", p=128)
    for c0 in range(0, ncols, 512):
        n_c = min(512, ncols - c0)
        for q in range(0, nkc, 4):
            n = min(4, nkc - q)
            st, bst = C.wst[C.wst_i % 4]
            C.wst_i += 1
            P.dma(st[:, 0:n, 0:n_c], wv[:, q:q + n, c0:c0 + n_c], writes=[bst])
            eng = "pool" if (C.wst_i % 2) else "dve"
            P.op(eng, lambda e, st=st, q=q, n=n, c0=c0, n_c=n_c: e.tensor_copy(out=dst[:, q:q + n, c0:c0 + n_c], in_=st[:, 0:n, 0:n_c]),
                 reads=[bst], pwrites=[bdst])


def scanproj_phase(P, C, xb, w_tm, w_fm, g1rep, ZT, ZF, S):
    with P.scope():
        alloc_psum(P, C)
        alloc_norm(P, C)
        C.wst = [(P.sbuf("wst%d" % i, [128, 4, 512], F32), P.buf("wst%d" % i)) for i in range(4)]
        C.wst_i = 0
        Wr = P.sbuf("Wres", [128, KC, NTM + NFM], BF16)
        bWr = P.buf("Wres")
        load_w_resident(P, C, w_tm, Wr[:, :, 0:NTM], bWr, NTM)
        load_w_resident(P, C, w_fm, Wr[:, :, NTM:NTM + NFM], bWr, NFM)
        zo = [(P.sbuf("zo%d" % i, [128, 512], F32), P.buf("zo%d" % i)) for i in range(4)]
        P.dma(C.gsb[0][:], g1rep, writes=[C.gsb[1]])
        oi = 0
        ai = 0
        for tt in range(S // TT):
            def xload(s_, xt, bxt, tt=tt):
                t0 = tt * TT + s_ * 128
                q, i0 = t0 // (S // 4), t0 % (S // 4)
                r0 = i0 // 64
                P.dma(xt[0:64, :], xb[r0, q * 64:(q + 1) * 64, :], writes=[bxt])
                P.dma(xt[64:128, :], xb[r0 + 1, q * 64:(q + 1) * 64, :], pwrites=[bxt])
            norm_T(P, C, None, None, xload=xload)
            hT, bhT = C.hT
            for cb in range(NTM // 512):
                for s in range(NSUB):
                    ps, bps = C.acc[ai % 6]
                    ai += 1
                    for kc in range(KC):
                        kw = dict(writes=[bps]) if kc == 0 else dict(pwrites=[bps])
                        P.op("pe", lambda e, ps=ps, kc=kc, s=s, cb=cb: e.matmul(
                            ps[:], lhsT=hT[:, kc, s * 128:(s + 1) * 128], rhs=Wr[:, kc, cb * 512:(cb + 1) * 512],
                            start=(kc == 0), stop=(kc == KC - 1)), reads=[bWr, bhT], **kw)
                    o, bo = zo[oi % 4]
                    oi += 1
                    eng = "act" if oi % 2 else "dve"
                    if eng == "act":
                        P.op("act", lambda e, o=o, ps=ps: e.activation(out=o[:], in_=ps[:], func=AF.Copy), reads=[bps], writes=[bo])
                    else:
                        P.op("dve", lambda e, o=o, ps=ps: e.tensor_copy(out=o[:], in_=ps[:]), reads=[bps], writes=[bo])
                    r0 = 1 + tt * TT + s * 128
                    P.dma(ZT[r0:r0 + 128, cb * 512:(cb + 1) * 512], o[:], reads=[bo])
            for r in range(NFM // 128):
                ps, bps = C.acc[ai % 6]
                ai += 1
                for kc in range(KC):
                    kw = dict(writes=[bps]) if kc == 0 else dict(pwrites=[bps])
                    P.op("pe", lambda e, ps=ps, kc=kc, r=r: e.matmul(
                        ps[:], lhsT=Wr[:, kc, NTM + r * 128:NTM + (r + 1) * 128], rhs=hT[:, kc, :],
                        start=(kc == 0), stop=(kc == KC - 1)), reads=[bWr, bhT], **kw)
                o, bo = zo[oi % 4]
                oi += 1
                if oi % 2:
                    P.op("act", lambda e, o=o, ps=ps: e.activation(out=o[:], in_=ps[:], func=AF.Copy), reads=[bps], writes=[bo])
                else:
                    P.op("dve", lambda e, o=o, ps=ps: e.tensor_copy(out=o[:], in_=ps[:]), reads=[bps], writes=[bo])
                P.dma(ZF[r * 128:(r + 1) * 128, 1 + tt * TT:1 + (tt + 1) * TT], o[:], reads=[bo])


def partial_phase(P, C, Y, w_ab, PA, PB, S):
    with P.scope():
        alloc_psum(P, C)
        C.wst = [(P.sbuf("wst%d" % i, [128, 4, 512], F32), P.buf("wst%d" % i)) for i in range(4)]
        C.wst_i = 0
        wab = P.sbuf("wab", [128, 4, D], BF16)
        bwab = P.buf("wab")
        load_w_resident(P, C, w_ab.rearrange("a r c -> (a r) c"), wab, bwab, D, nkc=4)
        yt = [(P.sbuf("yt%d" % i, [128, 512], F32), P.buf("yt%d" % i)) for i in range(2)]
        ybf = [(P.sbuf("ybf%d" % i, [128, 512], BF16), P.buf("ybf%d" % i)) for i in range(2)]
        yT = [(P.sbuf("yTp%d" % i, [128, 4, 128], BF16), P.buf("yTp%d" % i)) for i in range(2)]
        ot = [(P.sbuf("pot%d" % i, [128, D], F32), P.buf("pot%d" % i)) for i in range(2)]
        oi = 0
        ai = 0
        for r in range(S // 128):
            rows = slice(r * 128, (r + 1) * 128)
            y_, by_ = yt[r % 2]; yb_, byb_ = ybf[r % 2]; yT_, byT_ = yT[r % 2]
            P.dma(y_[:], Y[rows, :], writes=[by_])
            P.op("pool", lambda e, y_=y_, yb_=yb_: e.tensor_copy(out=yb_[:], in_=y_[:]), reads=[by_], writes=[byb_])
            pt, bpt = C.ptr[r % 2]
            for i in range(4):
                kw = dict(writes=[bpt]) if i == 0 else dict(pwrites=[bpt])
                P.op("pe", lambda e, pt=pt, i=i, yb_=yb_: e.transpose(out=pt[:, i * 128:(i + 1) * 128], in_=yb_[:, i * 128:(i + 1) * 128],
                                                                      identity=C.identb[:]), reads=[byb_, C.bconst], **kw)
            P.op("act", lambda e, pt=pt, yT_=yT_: e.activation(out=yT_[:], in_=pt[:, 0:512].rearrange("p (a b) -> p a b", a=4), func=AF.Copy),
                 reads=[bpt], writes=[byT_])
            for br, dst in ((0, PA), (1, PB)):
                o, bo = ot[oi % 2]
                oi += 1
                for cb in range(4):
                    ps, bps = C.acc[ai % 6]
                    ai += 1
                    for kc in range(2):
                        kw = dict(writes=[bps]) if kc == 0 else dict(pwrites=[bps])
                        P.op("pe", lambda e, ps=ps, kc=kc, br=br, cb=cb, yT_=yT_: e.matmul(
                            ps[:], lhsT=yT_[:, 2 * br + kc, :], rhs=wab[:, 2 * br + kc, cb * 512:(cb + 1) * 512],
                            start=(kc == 0), stop=(kc == 1)), reads=[bwab, byT_], **kw)
                    kw = dict(writes=[bo]) if cb == 0 else dict(pwrites=[bo])
                    if cb % 2:
                        P.op("act", lambda e, o=o, ps=ps, cb=cb: e.activation(out=o[:, cb * 512:(cb + 1) * 512], in_=ps[:], func=AF.Copy), reads=[bps], **kw)
                    else:
                        P.op("dve", lambda e, o=o, ps=ps, cb=cb: e.tensor_copy(out=o[:, cb * 512:(cb + 1) * 512], in_=ps[:]), reads=[bps], **kw)
                P.dma(dst[0][rows, :], o[:, 0:D // 2], reads=[bo])
                P.dma(dst[1][rows, :], o[:, D // 2:D], reads=[bo])


def merge2_phase(P, C, xres, bxres, AO, BO, G, w_out, ntiles):
    with P.scope():
        alloc_psum(P, C)
        alloc_wstream(P, C)
        tl = [[(P.sbuf("mg%d_%d" % (i, j), [128, D], F32), P.buf()) for j in range(4)] for i in range(2)]
        mb = (P.sbuf("mb", [128, D], BF16), P.buf("mb"))
        mT = P.sbuf("mT", [128, KC, TT], BF16)
        bmT = P.buf("mT")
        xo = [(P.sbuf("xo%d" % i, [128, 512], F32), P.buf("xo%d" % i)) for i in range(2)]
        oi = 0
        si = 0
        for tt in range(ntiles):
            for s in range(NSUB):
                rows = slice(tt * TT + s * 128, tt * TT + (s + 1) * 128)
                (a_, ba_), (b_, bb_), (ga_, bga_), (gb_, bgb_) = tl[si % 2]
                si += 1
                P.dma(a_[:, 0:D // 2], AO[0][rows, :], writes=[ba_]); P.dma(a_[:, D // 2:D], AO[1][rows, :], pwrites=[ba_])
                P.dma(b_[:, 0:D // 2], BO[0][rows, :], writes=[bb_]); P.dma(b_[:, D // 2:D], BO[1][rows, :], pwrites=[bb_])
                P.dma(ga_[:], G[rows, 0:D], writes=[bga_]); P.dma(gb_[:], G[rows, D:2 * D], writes=[bgb_])
                P.op("act", lambda e, ga_=ga_: e.activation(out=ga_[:], in_=ga_[:], func=AF.Sigmoid), reads=[bga_], writes=[bga_])
                P.op("act", lambda e, gb_=gb_: e.activation(out=gb_[:], in_=gb_[:], func=AF.Sigmoid), reads=[bgb_], writes=[bgb_])
                P.op("dve", lambda e, a_=a_, ga_=ga_: e.tensor_tensor(out=a_[:], in0=a_[:], in1=ga_[:], op=ALU.mult), reads=[ba_, bga_], writes=[ba_])
                P.op("pool", lambda e, b_=b_, gb_=gb_: e.tensor_tensor(out=b_[:], in0=b_[:], in1=gb_[:], op=ALU.mult), reads=[bb_, bgb_], writes=[bb_])
                m_, bm_ = mb
                P.op("dve", lambda e, a_=a_, b_=b_: e.tensor_tensor(out=m_[:], in0=a_[:], in1=b_[:], op=ALU.add), reads=[ba_, bb_], writes=[bm_])
                for j in range(0, KC, 8):
                    pt, bpt = C.ptr[(j // 8) % 2]
                    for i in range(8):
                        kw = dict(writes=[bpt]) if i == 0 else dict(pwrites=[bpt])
                        P.op("pe", lambda e, pt=pt, i=i, j=j: e.transpose(out=pt[:, i * 128:(i + 1) * 128], in_=m_[:, (j + i) * 128:(j + i + 1) * 128],
                                                                           identity=C.identb[:]), reads=[bm_, C.bconst], **kw)
                    kw = dict(writes=[bmT]) if (s == 0 and j == 0) else dict(pwrites=[bmT])
                    P.op("act", lambda e, pt=pt, j=j, s=s: e.activation(out=mT[:, j:j + 8, s * 128:(s + 1) * 128],
                                                                         in_=pt[:].rearrange("p (a b) -> p a b", a=8), func=AF.Copy), reads=[bpt], **kw)
            bx = bxres[tt]
            for cb in range(D // 512):
                wb, bwb = load_w_block(P, C, w_out[:, cb * 512:(cb + 1) * 512])
                for s in range(NSUB):
                    ps, bps = C.acc[s]
                    for kc in range(KC):
                        kw = dict(writes=[bps]) if kc == 0 else dict(pwrites=[bps])
                        P.op("pe", lambda e, ps=ps, wb=wb, kc=kc, s=s: e.matmul(
                            ps[:], lhsT=mT[:, kc, s * 128:(s + 1) * 128], rhs=wb[:, kc, :],
                            start=(kc == 0), stop=(kc == KC - 1)), reads=[bwb, bmT], **kw)
                    o, bo = xo[oi % 2]
                    oi += 1
                    rows = slice(tt * TT + s * 128, tt * TT + (s + 1) * 128)
                    P.dma(o[:], xres[rows, cb * 512:(cb + 1) * 512], reads=[bx], writes=[bo])
                    P.op("dve", lambda e, o=o, ps=ps: e.tensor_tensor(out=o[:], in0=o[:], in1=ps[:], op=ALU.add), reads=[bps, bo], writes=[bo])
                    P.dma(xres[rows, cb * 512:(cb + 1) * 512], o[:], reads=[bo], pwrites=[bx])


def build_fused(S, L, stop=None):
    N = S // 4
    nc = bass.Bass("TRN2", target_bir_lowering=False)
    dt = lambda n, s, k="ExternalInput", d=F32: nc.dram_tensor(n, s, d, kind=k).ap()
    x = dt("x", [N, D]); pos = dt("pos", [S, 1], d=I32)
    w_tm = dt("w_tm", [L, D, NTM]); w_fm = dt("w_fm", [L, D, NFM]); w_gate = dt("w_gate", [L, D, 2 * D])
    w_ab = dt("w_ab", [L, 2, 256, D]); w_out = dt("w_out", [L, D, D]); w_up = dt("w_up", [L, D, DFF]); w_down = dt("w_down", [L, DFF, D])
    g1 = dt("g1", [L, 128, D]); g2 = dt("g2", [L, 128, D]); gf = dt("gf", [128, D])
    ptm = dt("ptm", [L, 128, 10, 256]); mu_l = dt("mu_l", [L, 128, 2]); mu_vT = dt("mu_vT", [L, 64, 4])
    wa_up = dt("wa_up", [L, 128, 256]); g_up = dt("g_up", [L, 128, 256]); gn = dt("gn", [L, 128, 2, 256])
    sel = dt("sel", [128, 128, 64]); selr = dt("selr", [128, 128, 128]); identf = dt("identf", [128, 128])
    invf = dt("invf", [128, 64]); gam = dt("gam", [128, 2, 128])
    y = dt("y", [N, D], "ExternalOutput")
    it = lambda n, s: nc.dram_tensor(n, s, F32).ap()
    xres = it("xres", [N, D]); XB = it("XB", [N // 64, 256, D]); ZT = it("ZT", [1 + S, NTM]); ZF = it("ZF", [NFM, 1 + S])
    G = it("G", [N, 2 * D]); Y = it("Y", [S, 512]); PA = [it("PA%d" % i, [S, D // 2]) for i in range(2)]; PB = [it("PB%d" % i, [S, D // 2]) for i in range(2)]
    AO = [it("AO%d" % i, [N, D // 2]) for i in range(2)]; BO = [it("BO%d" % i, [N, D // 2]) for i in range(2)]

    P = Prog(nc); C = Ctx()
    setup_common(P, C, {"identf": identf})
    nt = N // TT
    bxres = [P.buf() for _ in range(nt)]
    for tt in range(nt):
        P.dma(xres[tt * TT:(tt + 1) * TT, :], x[tt * TT:(tt + 1) * TT, :], writes=[bxres[tt]])
    with P.scope():
        zt_ = P.sbuf("zeros", [128, NTM], F32); bz_ = P.buf()
        P.op("dve", lambda e: e.memset(zt_[:], 0.0), writes=[bz_])
        P.dma(ZT[0:1, :], zt_[0:1, :], reads=[bz_])
        for r in range(NFM // 128):
            P.dma(ZF[r * 128:(r + 1) * 128, 0:1], zt_[:, 0:1], reads=[bz_], allow_slow_non_contiguous=True)
    ck1 = lambda c: slice(1 + c * 128, 1 + (c + 1) * 128)
    ck0 = lambda c: slice(c * 128, (c + 1) * 128)
    for l in range(L):
        P.barrier()
        for r_ in range(N // 64):
            P.cc(lambda e, r_=r_: e.collective_compute("AllGather", ALU.bypass, replica_groups=RG4,
                                                       ins=[xres[r_ * 64:(r_ + 1) * 64, :].opt()], outs=[XB[r_].opt()]))
        P.barrier()
        if stop == "ag":
            break
        scanproj_phase(P, C, XB, w_tm[l], w_fm[l], g1[l], ZT, ZF, S)
        if stop == "scanproj":
            break
        bG = P.buf()
        proj_phase(P, C, xres, None, w_gate[l], g1[l], G, bG, nt, 2 * D)
        if stop == "gates":
            break

        class A:
            pass
        A.ptm, A.mu_l, A.mu_vT, A.wa_up, A.g_up, A.gn = ptm[l], mu_l[l], mu_vT[l], wa_up[l], g_up[l], gn[l]
        A.sel, A.selr, A.identf, A.invf, A.gam, A.pos = sel, selr, identf, invf, gam, pos
        A.y_ret, A.y_rw = Y[:, 0:256], Y[:, 256:512]
        A.rq = lambda c: ZT[ck1(c), 0:256]; A.rk = lambda c: ZT[ck1(c), 256:512]; A.rgr = lambda c: ZT[ck1(c), 512:768]
        A.ztm = lambda c: ZT[ck1(c), 768:1536].rearrange("t (a n) -> t a n", a=3)
        A.ztm_p = lambda c: ZT[ck0(c), 768:1536].rearrange("t (a n) -> t a n", a=3)
        A.rvT = lambda c: ZF[0:256, ck1(c)].rearrange("(h e) t -> e h t", h=2)
        A.zvT = lambda c: ZF[256:512, ck1(c)].rearrange("(h d) t -> d h t", h=4)
        A.zvT_p = lambda c: ZF[256:512, ck0(c)].rearrange("(h d) t -> d h t", h=4)
        A.zlT = lambda c: ZF[512:768, ck1(c)].rearrange("(a p) t -> p a t", a=2)
        A.zlT_p = lambda c: ZF[512:768, ck0(c)].rearrange("(a p) t -> p a t", a=2)
        scan_phase(P, S, A)
        if stop == "scan":
            break
        partial_phase(P, C, Y, w_ab[l], PA, PB, S)
        if stop == "partial":
            break
        P.barrier()
        for src_, dst_ in ((PA[0], AO[0]), (PA[1], AO[1]), (PB[0], BO[0]), (PB[1], BO[1])):
            P.cc(lambda e, src_=src_, dst_=dst_: e.collective_compute("ReduceScatter", ALU.add, replica_groups=RG4, ins=[src_.opt()], outs=[dst_.opt()]))
        P.barrier()
        if stop == "rs":
            break
        merge2_phase(P, C, xres, bxres, AO, BO, G, w_out[l], nt)
        if stop == "merge":
            break
        ffn_phase(P, C, xres, bxres, w_up[l], w_down[l], g2[l], nt)
    bout = P.buf()
    final_norm_phase(P, C, xres, bxres, gf, y, bout, nt)
    P.barrier()
    P.emit(); P.close()
    return nc


def host_inputs_fused(inp, S, L):
    N = S // 4
    f32 = lambda a: np.ascontiguousarray(np.asarray(a), dtype=np.float32)
    x = np.asarray(inp["x"]); positions = np.asarray(inp["positions"]).astype(np.int32)
    w_in = np.asarray(inp["w_in"])
    common = dict(
        w_gate=f32(w_in[:L, :, 7424:11520]), w_out=f32(inp["w_out"][:L]), w_up=f32(inp["mlp_up"][:L]), w_down=f32(inp["mlp_down"][:L]),
        g1=np.stack([rep128(np.asarray(inp["norm1_g"][l], np.float32)) for l in range(L)]),
        g2=np.stack([rep128(np.asarray(inp["norm2_g"][l], np.float32)) for l in range(L)]),
        gf=rep128(np.asarray(inp["final_g"], np.float32)), identf=np.eye(128, dtype=np.float32))
    sel = np.zeros((128, 128, 64), np.float32); sel[np.arange(128), np.arange(128), :] = 1.0
    selr = np.zeros((128, 128, 128), np.float32); selr[np.arange(128), np.arange(128), :] = 1.0
    inv = (10000.0 ** (-np.arange(64, dtype=np.float32) / np.float32(64))).astype(np.float32)
    common.update(sel=sel, selr=selr, invf=rep128(inv))
    mu_all = np.asarray(inp["rwkv_mu"], np.float32)
    per_hg = {}
    for hg in range(4):
        cs = slice(hg * 256, (hg + 1) * 256)
        R0 = 4096
        col = lambda base: np.arange(base + hg * 256, base + (hg + 1) * 256)
        tm_cols = np.concatenate([col(0), col(1024), col(3072), col(R0), col(R0 + 1024), col(R0 + 2048)])
        fm_cols = np.concatenate([col(2048), col(R0 + 2048), np.arange(R0 + 3072, R0 + 3328)])
        d = dict(w_tm=f32(w_in[:L][:, :, tm_cols]), w_fm=f32(w_in[:L][:, :, fm_cols]),
                 w_ab=f32(np.stack([np.asarray(inp["w_branch_a"])[:L, cs, :], np.asarray(inp["w_branch_b"])[:L, cs, :]], 1)))
        pv = lambda name, l: np.asarray(inp[name][l], np.float32)
        ptm, mul, muv, waup, gup, gn = [], [], [], [], [], []
        for l in range(L):
            mu = mu_all[l]
            p = dict(mu_r=mu[0:1024][cs], mu_k=mu[1024:2048][cs], mu_v=mu[2048:3072][cs], mu_w=mu[3072:3136], mu_a=mu[3136:3200], mu_g=mu[3200:3328],
                     w0=pv("rwkv_w0", l)[cs], a0=pv("rwkv_a0", l)[cs], k_k=pv("rwkv_k_k", l)[cs], k_a=pv("rwkv_k_a", l)[cs], r_k=pv("rwkv_r_k", l)[cs],
                     ln_g=pv("rwkv_ln_g", l)[cs], ln_b=pv("rwkv_ln_b", l)[cs])
            ptm.append(np.stack([rep128(p[k]) for k in ("mu_r", "mu_k", "mu_v", "w0", "a0", "k_k", "k_a", "r_k", "ln_g", "ln_b")], 1))
            mul.append(np.stack([np.concatenate([p["mu_w"], p["mu_a"]]), p["mu_g"]], 1))
            muv.append(p["mu_v"].reshape(4, 64).T)
            waup.append(np.concatenate([pv("rwkv_w_up", l)[:, cs], pv("rwkv_a_up", l)[:, cs]], 0))
            gup.append(pv("rwkv_g_up", l)[:, cs])
            gn.append(np.stack([rep128(pv("ret_gn_g", l)[cs]), rep128(pv("ret_gn_b", l)[cs])], 1))
        d.update(ptm=f32(np.stack(ptm)), mu_l=f32(np.stack(mul)), mu_vT=f32(np.stack(muv)), wa_up=f32(np.stack(waup)), g_up=f32(np.stack(gup)), gn=f32(np.stack(gn)))
        gamma = 1.0 - 2.0 ** (-5.0 - np.asarray([2 * hg, 2 * hg + 1], np.float64))
        d["gam"] = np.ascontiguousarray(np.broadcast_to(gamma.astype(np.float32).reshape(1, 2, 1), (128, 2, 128)))
        per_hg[hg] = d
    ins = []
    for c in range(8):
        b, j = c // 4, c % 4
        d = dict(common); d.update(per_hg[j])
        d["x"] = f32(x[b, j * N:(j + 1) * N]); d["pos"] = np.ascontiguousarray(positions[b, :S].reshape(S, 1))
        ins.append(d)
    return ins


from concourse.bass_utils import run_bass_kernel_spmd

_PROG = {}


def kernel(**inp):
    S, L = 8192, 4
    if "nc" not in _PROG:
        _PROG["nc"] = build_fused(S, L)
    ins = host_inputs_fused(inp, S, L)
    res = run_bass_kernel_spmd(_PROG["nc"], ins, core_ids=list(range(8)))
    out = np.stack([np.concatenate([res.results[4 * b + j]["y"] for j in range(4)], 0) for b in range(2)], 0)
    return np.ascontiguousarray(out, dtype=np.float32)
```

```python
import contextlib
import numpy as np
import concourse.bass as bass
import concourse.mybir as mybir

F32 = mybir.dt.float32
BF16 = mybir.dt.bfloat16
I32 = mybir.dt.int32
ALU = mybir.AluOpType
AF = mybir.ActivationFunctionType
AX = mybir.AxisListType

EPOCH = 30000
NDMA = 24


class Buf:
    __slots__ = ("name", "w", "rs")

    def __init__(self, name=""):
        self.name = name
        self.w = {}
        self.rs = {}


def _put(d, ev):
    k = id(ev[0])
    if k not in d or d[k][1] < ev[1]:
        d[k] = ev


class Prog:
    ENGS = ("pe", "act", "dve", "pool", "sp")

    def __init__(self, nc):
        self.nc = nc
        self.stack = contextlib.ExitStack()
        self.semstack = contextlib.ExitStack()
        self.eobj = {"pe": nc.tensor, "act": nc.scalar, "dve": nc.vector,
                     "pool": nc.gpsimd, "sp": nc.sync}
        self.lists = {e: [] for e in self.ENGS}
        self.cnt = {e: 0 for e in self.ENGS}
        self.cursem = {}
        self.seen = {e: {} for e in self.ENGS}
        self.nsem = 0
        self.own = {e: set() for e in self.ENGS}
        self.skip_self = set()
        for e in ("pe", "act", "dve", "pool"):
            self.cursem[e] = self.new_sem(e)
            self.own[e].add(id(self.cursem[e]))
        self.dsem = [self.new_sem("dma%d" % i) for i in range(NDMA)]
        self.dcum = [0] * NDMA
        self.dnext = 0
        self.nbuf = 0
        self.ccsem = self.new_sem("cc")
        self.cccum = 0

    def new_sem(self, name):
        self.nsem += 1
        return self.semstack.enter_context(self.nc.semaphore("s_%s_%d" % (name, self.nsem)))

    def sbuf(self, name, shape, dtype):
        self.nalloc = getattr(self, "nalloc", 0) + 1
        return self.stack.enter_context(self.nc.sbuf_tensor("sb_%s_%d" % (name, self.nalloc), list(shape), dtype))

    def psum(self, name, shape, dtype):
        self.nalloc = getattr(self, "nalloc", 0) + 1
        return self.stack.enter_context(self.nc.psum_tensor("ps_%s_%d" % (name, self.nalloc), list(shape), dtype))

    def buf(self, name=""):
        self.nbuf += 1
        return Buf(name or ("b%d" % self.nbuf))

    def _deps(self, reads, writes, pwrites=(), selfsem=None):
        evs = []
        for b in reads:
            if b is not None:
                evs.extend(b.w.values())
        for b in writes:
            if b is not None:
                evs.extend(b.w.values())
                evs.extend(b.rs.values())
        for b in pwrites:
            if b is not None:
                evs.extend(e for e in b.w.values() if e[0] is not selfsem)
                evs.extend(b.rs.values())
        return evs

    def _waits(self, eng, evs):
        seen = self.seen[eng]
        need = {}
        for (s, v) in evs:
            if seen.get(id(s), (None, 0))[1] >= v:
                continue
            if id(s) not in need or need[id(s)][1] < v:
                need[id(s)] = (s, v)
        for k, (s, v) in need.items():
            seen[k] = (s, v)
        return list(need.values())

    def _record(self, ev, reads, writes, pwrites=()):
        for b in reads:
            if b is not None:
                _put(b.rs, ev)
        for b in writes:
            if b is not None:
                b.w = {id(ev[0]): ev}
                b.rs = {}
        for b in pwrites:
            if b is not None:
                _put(b.w, ev)

    def op(self, eng, fn, reads=(), writes=(), pwrites=()):
        if self.cnt[eng] >= EPOCH:
            self.cursem[eng] = self.new_sem(eng)
            self.own[eng].add(id(self.cursem[eng]))
            self.cnt[eng] = 0
        evs = self._deps(reads, writes, pwrites, self.cursem[eng])
        if eng in self.skip_self:
            own = self.own[eng]
            evs = [ev_ for ev_ in evs if id(ev_[0]) not in own]
        waits = self._waits(eng, evs)
        self.cnt[eng] += 1
        ev = (self.cursem[eng], self.cnt[eng])
        self.lists[eng].append((waits, fn, ev[0], 1))
        self._record(ev, reads, writes, pwrites)
        return ev

    def dma(self, out_ap, in_ap, reads=(), writes=(), pwrites=(), q="sp", **kw):
        i = self.dnext
        self.dnext = (self.dnext + 1) % NDMA
        s = self.dsem[i]
        evs = self._deps(reads, writes)
        for b in pwrites:
            evs.extend(b.rs.values())
        if self.dcum[i] > 0:
            evs.append((s, self.dcum[i]))
        waits = self._waits(q, evs)
        self.dcum[i] += 16
        ev = (s, self.dcum[i])
        fn = (lambda e, o=out_ap, a=in_ap, k=kw: e.dma_start(out=o, in_=a, **k))
        self.lists[q].append((waits, fn, s, 16))
        self._record(ev, reads, writes, pwrites)
        return ev

    def cc(self, fn, reads=(), writes=()):
        s = self.ccsem
        evs = self._deps(reads, writes)
        if self.cccum > 0:
            evs.append((s, self.cccum))
        waits = self._waits("pool", evs)
        self.cccum += 1
        ev = (s, self.cccum)
        self.lists["pool"].append((waits, fn, s, 1))
        self._record(ev, reads, writes)
        return ev

    def barrier(self):
        evs = []
        for e in ("pe", "act", "dve", "pool"):
            if self.cnt[e] > 0:
                evs.append((self.cursem[e], self.cnt[e]))
        for i in range(NDMA):
            if self.dcum[i] > 0:
                evs.append((self.dsem[i], self.dcum[i]))
        if self.cccum > 0:
            evs.append((self.ccsem, self.cccum))
        for e in self.ENGS:
            waits = self._waits(e, list(evs))
            if waits:
                self.lists[e].append((waits, None, None, 0))

    @contextlib.contextmanager
    def scope(self):
        outer = self.stack
        self.stack = contextlib.ExitStack()
        try:
            yield
        finally:
            self.barrier()
            self.stack.close()
            self.stack = outer

    def wait_all(self, eng, bufs):
        evs = []
        for b in bufs:
            evs.extend(b.w.values())
            evs.extend(b.rs.values())
        waits = self._waits(eng, evs)
        self.lists[eng].append((waits, None, None, 0))

    def emit(self):
        nc = self.nc
        with nc.Block() as block:
            def run(engname):
                def body(e):
                    for waits, fn, sem, inc in self.lists[engname]:
                        for (s, v) in waits:
                            e.wait_ge(s, v)
                        if fn is not None:
                            fn(e).then_inc(sem, inc)
                return body
            block.tensor(run("pe"))
            block.scalar(run("act"))
            block.vector(run("dve"))
            block.gpsimd(run("pool"))
            block.sync(run("sp"))

    def close(self):
        self.stack.close()
        self.semstack.close()
import ml_dtypes

D = 2048
DFF = 8192
KC = D // 128
TT = 512
NSUB = TT // 128
EPS = 1e-6


class Ctx:
    pass


def setup_common(P, C, consts):
    C.identb = P.sbuf("identb", [128, 128], BF16)
    C.identf = P.sbuf("identf", [128, 128], F32)
    C.bconst = P.buf("consts")
    P.dma(C.identf[:], consts["identf"], writes=[C.bconst])
    P.op("dve", lambda e: e.tensor_copy(out=C.identb[:], in_=C.identf[:]), reads=[C.bconst], pwrites=[C.bconst])
    C.epsb = P.sbuf("epsb", [128, 4], F32)
    P.op("dve", lambda e: e.memset(C.epsb[:, 0:1], EPS), pwrites=[C.bconst])


def alloc_psum(P, C):
    C.acc = []
    for i in range(6):
        C.acc.append((P.psum("acc%d" % i, [128, 512], F32), P.buf("acc%d" % i)))
    C.ptr = []
    for i in range(2):
        C.ptr.append((P.psum("ptr%d" % i, [128, 1024], BF16), P.buf("ptr%d" % i)))


def alloc_wstream(P, C):
    C.wst = [(P.sbuf("wst%d" % i, [128, 4, 512], F32), P.buf("wst%d" % i)) for i in range(4)]
    C.wb = [(P.sbuf("wb%d" % i, [128, 16, 512], BF16), P.buf("wb%d" % i)) for i in range(2)]
    C.wst_i = 0
    C.wb_i = 0


def load_w_block(P, C, w_ap, nkc=16, ncol=512):
    wb, bwb = C.wb[C.wb_i % 2]
    C.wb_i += 1
    wv = w_ap.rearrange("(kc p) c -> p kc c", p=128)
    first = True
    for q in range(0, nkc, 4):
        n = min(4, nkc - q)
        st, bst = C.wst[C.wst_i % 4]
        C.wst_i += 1
        P.dma(st[:, 0:n, 0:ncol], wv[:, q:q + n, :], writes=[bst])
        eng = "pool" if (C.wst_i % 4) != 0 else "dve"
        kw = dict(writes=[bwb]) if first else dict(pwrites=[bwb])
        P.op(eng, lambda e, st=st, q=q, n=n: e.tensor_copy(out=wb[:, q:q + n, 0:ncol], in_=st[:, 0:n, 0:ncol]),
             reads=[bst], **kw)
        first = False
    return wb, bwb


def alloc_norm(P, C):
    C.xt = [(P.sbuf("xt%d" % i, [128, D], F32), P.buf("xt%d" % i)) for i in range(2)]
    C.junk = (P.sbuf("junk", [128, D], BF16), P.buf("junk"))
    C.hb = (P.sbuf("hb", [128, D], BF16), P.buf("hb"))
    C.ss = [(P.sbuf("ss%d" % i, [128, 2], F32), P.buf("ss%d" % i)) for i in range(2)]
    C.gsb = (P.sbuf("gsb", [128, D], F32), P.buf("gsb"))
    C.hT = (P.sbuf("hT", [128, KC, TT], BF16), P.buf("hT"))
    C.xt_i = 0


def norm_T(P, C, x_tile, bx, xload=None):
    hT, bhT = C.hT
    gsb, bg = C.gsb
    junk, bjunk = C.junk
    hb, bhb = C.hb
    for s in range(NSUB):
        xt, bxt = C.xt[C.xt_i % 2]
        ss, bss = C.ss[C.xt_i % 2]
        C.xt_i += 1
        if xload is None:
            P.dma(xt[:], x_tile[s * 128:(s + 1) * 128, :], reads=[bx], writes=[bxt])
        else:
            xload(s, xt, bxt)
        P.op("act", lambda e, xt=xt, ss=ss: e.activation(out=junk[:], in_=xt[:], func=AF.Square, accum_out=ss[:, 0:1]),
             reads=[bxt], writes=[bjunk, bss])
        P.op("act", lambda e, ss=ss: e.activation(out=ss[:, 1:2], in_=ss[:, 0:1], func=AF.Sqrt, scale=1.0 / D, bias=C.epsb[:, 0:1]),
             reads=[bss, C.bconst], pwrites=[bss])
        P.op("dve", lambda e, ss=ss: e.reciprocal(out=ss[:, 1:2], in_=ss[:, 1:2]), reads=[bss], pwrites=[bss])
        P.op("dve", lambda e, xt=xt, ss=ss: e.scalar_tensor_tensor(out=hb[:], in0=xt[:], scalar=ss[:, 1:2], in1=gsb[:],
                                                                    op0=ALU.mult, op1=ALU.mult),
             reads=[bxt, bss, bg], writes=[bhb])
        for j in range(0, KC, 8):
            pt, bpt = C.ptr[(j // 8) % 2]
            for i in range(8):
                kw = dict(writes=[bpt]) if i == 0 else dict(pwrites=[bpt])
                P.op("pe", lambda e, pt=pt, i=i, j=j: e.transpose(out=pt[:, i * 128:(i + 1) * 128],
                                                                   in_=hb[:, (j + i) * 128:(j + i + 1) * 128],
                                                                   identity=C.identb[:]),
                     reads=[bhb, C.bconst], **kw)
            kw = dict(writes=[bhT]) if (s == 0 and j == 0) else dict(pwrites=[bhT])
            P.op("act", lambda e, pt=pt, j=j, s=s: e.activation(
                out=hT[:, j:j + 8, s * 128:(s + 1) * 128],
                in_=pt[:].rearrange("p (a b) -> p a b", a=8), func=AF.Copy),
                reads=[bpt], **kw)


def ffn_phase(P, C, xres, bxres, w_up, w_down, g2rep, ntiles):
    with P.scope():
        alloc_psum(P, C)
        alloc_norm(P, C)
        alloc_wstream(P, C)
        uT = P.sbuf("uT", [128, DFF // 128, TT], BF16)
        buT = [P.buf("uT%d" % i) for i in range(DFF // 128)]
        tmp = [(P.sbuf("rtmp%d" % i, [128, 512], F32), P.buf("rtmp%d" % i)) for i in range(2)]
        xo = [(P.sbuf("xo%d" % i, [128, 512], F32), P.buf("xo%d" % i)) for i in range(2)]
        P.dma(C.gsb[0][:], g2rep, writes=[C.gsb[1]])
        ti = 0
        oi = 0
        for tt in range(ntiles):
            x_tile = xres[tt * TT:(tt + 1) * TT, :]
            bx = bxres[tt]
            norm_T(P, C, x_tile, bx)
            hT, bhT = C.hT
            for blk in range(DFF // 512):
                wb, bwb = load_w_block(P, C, w_up[:, blk * 512:(blk + 1) * 512])
                for j in range(4):
                    ps, bps = C.acc[j]
                    fc = blk * 4 + j
                    for kc in range(KC):
                        kw = dict(writes=[bps]) if kc == 0 else dict(pwrites=[bps])
                        P.op("pe", lambda e, ps=ps, wb=wb, kc=kc, j=j: e.matmul(
                            ps[:], lhsT=wb[:, kc, j * 128:(j + 1) * 128], rhs=hT[:, kc, :],
                            start=(kc == 0), stop=(kc == KC - 1)), reads=[bwb, bhT], **kw)
                    t, bt = tmp[ti % 2]
                    ti += 1
                    P.op("act", lambda e, t=t, ps=ps: e.activation(out=t[:], in_=ps[:], func=AF.Relu),
                         reads=[bps], writes=[bt])
                    P.op("pool", lambda e, t=t, fc=fc: e.tensor_tensor(out=uT[:, fc, :], in0=t[:], in1=t[:], op=ALU.mult),
                         reads=[bt], writes=[buT[fc]])
            for cb in range(D // 512):
                for fb in range(DFF // 2048):
                    wb, bwb = load_w_block(P, C, w_down[fb * 2048:(fb + 1) * 2048, cb * 512:(cb + 1) * 512])
                    for s in range(NSUB):
                        ps, bps = C.acc[s]
                        for fc in range(16):
                            first = (fb == 0 and fc == 0)
                            last = (fb == DFF // 2048 - 1 and fc == 15)
                            kw = dict(writes=[bps]) if first else dict(pwrites=[bps])
                            P.op("pe", lambda e, ps=ps, wb=wb, fc=fc, fb=fb, s=s, first=first, last=last: e.matmul(
                                ps[:], lhsT=uT[:, fb * 16 + fc, s * 128:(s + 1) * 128], rhs=wb[:, fc, :],
                                start=first, stop=last), reads=[bwb, buT[fb * 16 + fc]], **kw)
                for s in range(NSUB):
                    ps, bps = C.acc[s]
                    o, bo = xo[oi % 2]
                    oi += 1
                    rows = slice(tt * TT + s * 128, tt * TT + (s + 1) * 128)
                    P.dma(o[:], xres[rows, cb * 512:(cb + 1) * 512], reads=[bx], writes=[bo])
                    P.op("dve", lambda e, o=o, ps=ps: e.tensor_tensor(out=o[:], in0=o[:], in1=ps[:], op=ALU.add),
                         reads=[bps, bo], writes=[bo])
                    P.dma(xres[rows, cb * 512:(cb + 1) * 512], o[:], reads=[bo], pwrites=[bx])


def proj_phase(P, C, x, bx, w_in, g1rep, z_out, bz, ntiles, ncols):
    with P.scope():
        alloc_psum(P, C)
        alloc_norm(P, C)
        alloc_wstream(P, C)
        zo = [(P.sbuf("zo%d" % i, [128, 512], F32), P.buf("zo%d" % i)) for i in range(4)]
        P.dma(C.gsb[0][:], g1rep, writes=[C.gsb[1]])
        oi = 0
        for tt in range(ntiles):
            norm_T(P, C, x[tt * TT:(tt + 1) * TT, :], bx)
            hT, bhT = C.hT
            for c0 in range(0, ncols, 512):
                nc_ = min(512, ncols - c0)
                wb, bwb = load_w_block(P, C, w_in[:, c0:c0 + nc_], ncol=nc_)
                for s in range(NSUB):
                    ps, bps = C.acc[s]
                    for kc in range(KC):
                        kw = dict(writes=[bps]) if kc == 0 else dict(pwrites=[bps])
                        P.op("pe", lambda e, ps=ps, wb=wb, kc=kc, s=s, nc_=nc_: e.matmul(
                            ps[:, 0:nc_], lhsT=hT[:, kc, s * 128:(s + 1) * 128], rhs=wb[:, kc, 0:nc_],
                            start=(kc == 0), stop=(kc == KC - 1)), reads=[bwb, bhT], **kw)
                    o, bo = zo[oi % 4]
                    oi += 1
                    if oi % 2:
                        P.op("act", lambda e, o=o, ps=ps, nc_=nc_: e.activation(out=o[:, 0:nc_], in_=ps[:, 0:nc_], func=AF.Copy),
                             reads=[bps], writes=[bo])
                    else:
                        P.op("dve", lambda e, o=o, ps=ps, nc_=nc_: e.tensor_copy(out=o[:, 0:nc_], in_=ps[:, 0:nc_]),
                             reads=[bps], writes=[bo])
                    rows = slice(tt * TT + s * 128, tt * TT + (s + 1) * 128)
                    P.dma(z_out[rows, c0:c0 + nc_], o[:, 0:nc_], reads=[bo], pwrites=[bz])


def merge_phase(P, C, xres, bxres, yretT, yrwT, gaT, gbT, w_a, w_b, w_out, ntiles):
    with P.scope():
        alloc_psum(P, C)
        alloc_wstream(P, C)
        yst = [(P.sbuf("yst%d" % i, [128, 8, TT], F32), P.buf("yst%d" % i)) for i in range(2)]
        yb = [(P.sbuf("yb%d" % i, [128, 8, TT], BF16), P.buf("yb%d" % i)) for i in range(2)]
        gt = [(P.sbuf("gt%d" % i, [128, TT], F32), P.buf("gt%d" % i)) for i in range(4)]
        t12 = [(P.sbuf("t12_%d" % i, [128, TT], F32), P.buf("t12_%d" % i)) for i in range(4)]
        mT = P.sbuf("mT", [128, KC, TT], BF16)
        bmT = [P.buf("mT%d" % i) for i in range(KC)]
        xo = [(P.sbuf("xo%d" % i, [128, 512], F32), P.buf("xo%d" % i)) for i in range(2)]
        gi = 0
        oi = 0
        for tt in range(ntiles):
            tok = slice(tt * TT, (tt + 1) * TT)
            for i, src in enumerate((yretT, yrwT)):
                st, bst = yst[i]
                P.dma(st[:], src.rearrange("(kc p) t -> p kc t", p=128)[:, :, tok], writes=[bst])
                eng = "pool" if i == 0 else "dve"
                P.op(eng, lambda e, st=st, i=i: e.tensor_copy(out=yb[i][0][:], in_=st[:]), reads=[bst], writes=[yb[i][1]])
            for db in range(D // 512):
                wa, bwa = load_w_block(P, C, w_a[:, db * 512:(db + 1) * 512], nkc=8)
                wbb, bwbb = load_w_block(P, C, w_b[:, db * 512:(db + 1) * 512], nkc=8)
                for j in range(4):
                    dc = db * 4 + j
                    res = []
                    for i, (w_, bw_, gT) in enumerate(((wa, bwa, gaT), (wbb, bwbb, gbT))):
                        ps, bps = C.acc[(2 * j + i) % 6]
                        for kc in range(8):
                            kw = dict(writes=[bps]) if kc == 0 else dict(pwrites=[bps])
                            P.op("pe", lambda e, ps=ps, w_=w_, kc=kc, j=j, i=i: e.matmul(
                                ps[:], lhsT=w_[:, kc, j * 128:(j + 1) * 128], rhs=yb[i][0][:, kc, :],
                                start=(kc == 0), stop=(kc == 7)), reads=[bw_, yb[i][1]], **kw)
                        g, bg = gt[gi % 4]
                        t, bt = t12[gi % 4]
                        gi += 1
                        P.dma(g[:], gT[dc * 128:(dc + 1) * 128, tok], writes=[bg])
                        P.op("act", lambda e, g=g: e.activation(out=g[:], in_=g[:], func=AF.Sigmoid), reads=[bg], writes=[bg])
                        P.op("dve", lambda e, t=t, g=g, ps=ps: e.tensor_tensor(out=t[:], in0=g[:], in1=ps[:], op=ALU.mult),
                             reads=[bg, bps], writes=[bt])
                        res.append((t, bt))
                    P.op("pool", lambda e, dc=dc, a=res[0][0], b=res[1][0]: e.tensor_tensor(out=mT[:, dc, :], in0=a[:], in1=b[:], op=ALU.add),
                         reads=[res[0][1], res[1][1]], writes=[bmT[dc]])
            bx = bxres[tt]
            for cb in range(D // 512):
                wb, bwb = load_w_block(P, C, w_out[:, cb * 512:(cb + 1) * 512])
                for s in range(NSUB):
                    ps, bps = C.acc[s]
                    for kc in range(KC):
                        kw = dict(writes=[bps]) if kc == 0 else dict(pwrites=[bps])
                        P.op("pe", lambda e, ps=ps, wb=wb, kc=kc, s=s: e.matmul(
                            ps[:], lhsT=mT[:, kc, s * 128:(s + 1) * 128], rhs=wb[:, kc, :],
                            start=(kc == 0), stop=(kc == KC - 1)), reads=[bwb, bmT[kc]], **kw)
                    o, bo = xo[oi % 2]
                    oi += 1
                    rows = slice(tt * TT + s * 128, tt * TT + (s + 1) * 128)
                    P.dma(o[:], xres[rows, cb * 512:(cb + 1) * 512], reads=[bx], writes=[bo])
                    P.op("dve", lambda e, o=o, ps=ps: e.tensor_tensor(out=o[:], in0=o[:], in1=ps[:], op=ALU.add),
                         reads=[bps, bo], writes=[bo])
                    P.dma(xres[rows, cb * 512:(cb + 1) * 512], o[:], reads=[bo], pwrites=[bx])


def final_norm_phase(P, C, xres, bxres, grep, out, bout, ntiles):
    with P.scope():
        alloc_psum(P, C)
        alloc_norm(P, C)
        gsb, bg = C.gsb
        P.dma(gsb[:], grep, writes=[bg])
        junk, bjunk = C.junk
        ob = [(P.sbuf("fo%d" % i, [128, D], F32), P.buf("fo%d" % i)) for i in range(2)]
        for r in range(ntiles * NSUB):
            xt, bxt = C.xt[r % 2]
            ss, bss = C.ss[r % 2]
            o, bo = ob[r % 2]
            rows = slice(r * 128, (r + 1) * 128)
            P.dma(xt[:], xres[rows, :], reads=[bxres[r // NSUB]], writes=[bxt])
            P.op("act", lambda e, xt=xt, ss=ss: e.activation(out=junk[:], in_=xt[:], func=AF.Square, accum_out=ss[:, 0:1]),
                 reads=[bxt], writes=[bjunk, bss])
            P.op("act", lambda e, ss=ss: e.activation(out=ss[:, 1:2], in_=ss[:, 0:1], func=AF.Sqrt, scale=1.0 / D, bias=C.epsb[:, 0:1]),
                 reads=[bss, C.bconst], pwrites=[bss])
            P.op("dve", lambda e, ss=ss: e.reciprocal(out=ss[:, 1:2], in_=ss[:, 1:2]), reads=[bss], pwrites=[bss])
            P.op("dve", lambda e, xt=xt, ss=ss, o=o: e.scalar_tensor_tensor(out=o[:], in0=xt[:], scalar=ss[:, 1:2], in1=gsb[:],
                                                                             op0=ALU.mult, op1=ALU.mult),
                 reads=[bxt, bss, bg], writes=[bo])
            P.dma(out[rows, :], o[:], reads=[bo], pwrites=[bout])


def build_C(N, final):
    nc = bass.Bass("TRN2", target_bir_lowering=False)
    dt = lambda n, s, k="ExternalInput": nc.dram_tensor(n, s, F32, kind=k).ap()
    x = dt("x", [N, D])
    yretT = dt("yretT", [1024, N]); yrwT = dt("yrwT", [1024, N])
    gaT = dt("gaT", [D, N]); gbT = dt("gbT", [D, N])
    w_a = dt("w_a", [1024, D]); w_b = dt("w_b", [1024, D]); w_out = dt("w_out", [D, D])
    w_up = dt("w_up", [D, DFF]); w_down = dt("w_down", [DFF, D])
    g2 = dt("g2", [128, D]); gf = dt("gf", [128, D]); identf = dt("identf", [128, 128])
    y = dt("y", [N, D], "ExternalOutput")
    xres = nc.dram_tensor("xres", [N, D], F32).ap()
    P = Prog(nc); C = Ctx()
    setup_common(P, C, {"identf": identf})
    nt = N // TT
    bxres = [P.buf() for _ in range(nt)]
    for tt in range(nt):
        P.dma(xres[tt * TT:(tt + 1) * TT, :], x[tt * TT:(tt + 1) * TT, :], writes=[bxres[tt]])
    merge_phase(P, C, xres, bxres, yretT, yrwT, gaT, gbT, w_a, w_b, w_out, nt)
    ffn_phase(P, C, xres, bxres, w_up, w_down, g2, nt)
    bout = P.buf()
    if final:
        final_norm_phase(P, C, xres, bxres, gf, y, bout, nt)
    else:
        for tt in range(nt):
            P.dma(y[tt * TT:(tt + 1) * TT, :], xres[tt * TT:(tt + 1) * TT, :], reads=[bxres[tt]], pwrites=[bout])
    P.wait_all("sp", [bout])
    P.emit(); P.close()
    return nc


def build_A(N, ncols=11520):
    nc = bass.Bass("TRN2", target_bir_lowering=False)
    dt = lambda n, s, k="ExternalInput": nc.dram_tensor(n, s, F32, kind=k).ap()
    x = dt("x", [N, D]); w = dt("w_in", [D, ncols]); g = dt("g1", [128, D]); identf = dt("identf", [128, 128])
    z = dt("z", [N, ncols], "ExternalOutput")
    P = Prog(nc); C = Ctx()
    setup_common(P, C, {"identf": identf})
    bz = P.buf()
    proj_phase(P, C, x, None, w, g, z, bz, N // TT, ncols)
    P.wait_all("sp", [bz])
    P.emit(); P.close()
    return nc


TWO_PI = 6.283185307179586
LN_EPS = 64e-5
GN_EPS = 1e-5


def scan_phase(P, S, A):
    with P.scope():
        ptm = A.ptm
        mu_l = A.mu_l
        mu_vT = A.mu_vT
        wa_up = A.wa_up
        g_up = A.g_up
        sel = A.sel
        identf = A.identf
        invf = A.invf
        gam = A.gam
        gn = A.gn
        selr = A.selr
        y_rw = A.y_rw
        y_ret = A.y_ret
        pos = A.pos
        sb = P.sbuf
        bc = P.buf("const")
        c_ptm = sb("ptm", [128, 10, 256], F32); c_mul = sb("mul", [128, 2], F32); c_muv = sb("muv", [64, 4], F32)
        c_wa = sb("waup", [128, 256], F32); c_gu = sb("gup", [128, 256], F32)
        c_sel = sb("sel", [128, 128, 64], F32); c_id = sb("id", [128, 128], F32)
        c_invf = sb("invf", [128, 64], F32); c_gam = sb("gam", [128, 2, 128], F32); c_gn = sb("gn", [128, 2, 256], F32)
        c_selr = sb("selr", [128, 128, 128], F32); c_kq = sb("kq", [128, 4], F32)
        c_omka = sb("omka", [128, 256], F32); c_bias = sb("cbias", [128, 4], F32)
        for t_, a_ in ((c_ptm, ptm), (c_mul, mu_l), (c_muv, mu_vT), (c_wa, wa_up), (c_gu, g_up), (c_sel, sel), (c_id, identf),
                       (c_invf, invf), (c_gam, gam), (c_gn, gn), (c_selr, selr), (c_kq, A.kq)):
            P.dma(t_[:], a_, pwrites=[bc])
        P.op("dve", lambda e: e.tensor_scalar(out=c_omka[:], in0=c_ptm[:, 6, :], scalar1=-1.0, scalar2=1.0, op0=ALU.mult, op1=ALU.add),
             reads=[bc], pwrites=[bc])
        P.op("dve", lambda e: e.memset(c_bias[:, 0:1], -3.141592653589793), pwrites=[bc])
        P.op("dve", lambda e: e.memset(c_bias[:, 1:2], LN_EPS), pwrites=[bc])
        P.op("dve", lambda e: e.memset(c_bias[:, 2:3], GN_EPS), pwrites=[bc])

        Srw = sb("Srw", [64, 4, 64], F32); bS = P.buf("Srw")
        Rr = sb("Rr", [128, 2, 128], F32); bR = P.buf("Rr")
        bSg = [P.buf("Sg0"), P.buf("Sg1")]
        bTS = [[P.buf(), P.buf()] for _ in range(2)]; bSA = [[P.buf(), P.buf()] for _ in range(2)]; bTP = [[P.buf(), P.buf()] for _ in range(2)]
        P.op("dve", lambda e: e.memset(Srw[:], 0.0), writes=[bS, bSg[0], bSg[1]])
        bRg = [P.buf("Rg0"), P.buf("Rg1")]
        bT1 = [[P.buf(), P.buf()] for _ in range(2)]; bT2 = [[P.buf(), P.buf()] for _ in range(2)]
        P.op("pool", lambda e: e.memset(Rr[:], 0.0), writes=[bR, bRg[0], bRg[1]])

        psRow = [[(P.psum("prow%d_%d" % (i, j), [64, 512], F32), P.buf()) for j in range(3)] for i in range(2)]
        psMisc = (P.psum("pmisc", [128, 512], F32), P.buf("pmisc"))
        psRet = (P.psum("pret", [128, 512], F32), P.buf("pret"))

        def T(name, shape, n=1, dtype=F32):
            return [(sb(name + str(i), shape, dtype), P.buf(name + str(i))) for i in range(n)]
        zt = T("zt", [128, 3, 256], 2); zp = T("zp", [128, 3, 256], 2)
        vT = T("vT", [64, 4, 128], 2); vTp = T("vTp", [64, 4, 128], 2)
        lT = T("lT", [128, 2, 128], 2); lTp = T("lTp", [128, 2, 128], 2)
        rows = T("rows", [128, 5, 256], 2)
        yT = T("yT", [64, 4, 128], 2)
        w1 = T("w1", [128, 256], 6)
        sm = T("sm", [128, 16], 4)
        vtm = T("vtm", [128, 256], 2)
        gtm = T("gtm", [128, 256], 2)
        bon = T("bon", [128, 4], 2)
        tmpS = T("tmpS", [64, 4, 64], 2); saS = T("saS", [64, 4], 2)
        tmpP = T("tmpP", [64, 4, 64], 2)
        orw = T("orw", [128, 256], 2)
        st6 = T("st6", [128, 4, 6], 2); mv = T("mv", [128, 4, 2], 2)
        rqt = T("rqt", [128, 256], 2); rkt = T("rkt", [128, 256], 2); rgt = T("rgt", [128, 256], 2)
        rvt = T("rvt", [128, 2, 128], 2); post = T("post", [128, 2], 2, I32); posf = T("posf", [128, 2], 2)
        cs = T("cs", [128, 2, 64], 2)
        rrow = T("rrow", [128, 2, 256], 2)
        rrs = T("rrs", [128, 512], 2)
        ryT = T("ryT", [128, 2, 128], 2)
        tmpR = T("tmpR", [128, 2, 128], 2); tmpR2 = T("tmpR2", [128, 2, 128], 2)
        oret = T("oret", [128, 256], 2)
        w2 = T("w2", [128, 256], 4)
        kit = T("kit", [128, 64], 2, I32)

        NCH = S // 128
        for c in range(NCH):
            i2 = c % 2
            tok = slice(c * 128, (c + 1) * 128)
            z, bz = zt[i2]; zpp, bzp = zp[i2]
            P.dma(z[:], A.ztm(c), writes=[bz]); P.dma(zpp[:], A.ztm_p(c), writes=[bzp])
            v_, bv_ = vT[i2]; vp_, bvp_ = vTp[i2]
            P.dma(v_[:], A.zvT(c), writes=[bv_]); P.dma(vp_[:], A.zvT_p(c), writes=[bvp_])
            l_, bl_ = lT[i2]; lp_, blp_ = lTp[i2]
            P.dma(l_[:], A.zlT(c), writes=[bl_]); P.dma(lp_[:], A.zlT_p(c), writes=[blp_])
            P.op("dve", lambda e, z=z, zpp=zpp: e.tensor_tensor(out=zpp[:], in0=zpp[:], in1=z[:], op=ALU.subtract), reads=[bz, bzp], writes=[bzp])
            P.op("dve", lambda e, z=z, zpp=zpp: e.tensor_tensor(out=zpp[:], in0=zpp[:], in1=c_ptm[:, 0:3, :], op=ALU.mult), reads=[bzp, bc], writes=[bzp])
            P.op("dve", lambda e, z=z, zpp=zpp: e.tensor_tensor(out=z[:], in0=z[:], in1=zpp[:], op=ALU.add), reads=[bz, bzp], writes=[bz])
            P.op("pool", lambda e, v_=v_, vp_=vp_: e.tensor_tensor(out=vp_[:], in0=vp_[:], in1=v_[:], op=ALU.subtract), reads=[bv_, bvp_], writes=[bvp_])
            P.op("pool", lambda e, v_=v_, vp_=vp_: e.tensor_tensor(out=vp_[:], in0=vp_[:], in1=c_muv[:].unsqueeze(2).to_broadcast([64, 4, 128]), op=ALU.mult), reads=[bvp_, bc], writes=[bvp_])
            P.op("pool", lambda e, v_=v_, vp_=vp_: e.tensor_tensor(out=v_[:], in0=v_[:], in1=vp_[:], op=ALU.add), reads=[bv_, bvp_], writes=[bv_])
            P.op("pool", lambda e, l_=l_, lp_=lp_: e.tensor_tensor(out=lp_[:], in0=lp_[:], in1=l_[:], op=ALU.subtract), reads=[bl_, blp_], writes=[blp_])
            P.op("pool", lambda e, l_=l_, lp_=lp_: e.tensor_tensor(out=lp_[:], in0=lp_[:], in1=c_mul[:].unsqueeze(2).to_broadcast([128, 2, 128]), op=ALU.mult), reads=[blp_, bc], writes=[blp_])
            P.op("pool", lambda e, l_=l_, lp_=lp_: e.tensor_tensor(out=l_[:], in0=l_[:], in1=lp_[:], op=ALU.add), reads=[bl_, blp_], writes=[bl_])
            P.op("act", lambda e, l_=l_: e.activation(out=l_[0:64, 0, :], in_=l_[0:64, 0, :], func=AF.Tanh), reads=[bl_], writes=[bl_])
            P.op("act", lambda e, l_=l_: e.activation(out=l_[:, 1, :], in_=l_[:, 1, :], func=AF.Sigmoid), reads=[bl_], writes=[bl_])
            pm, bpm = psMisc
            rw_, brw_ = rows[i2]
            P.op("pe", lambda e, l_=l_: e.matmul(pm[:, 0:256], lhsT=l_[0:64, 0, :], rhs=c_wa[0:64, :], start=True, stop=True), reads=[bl_, bc], writes=[bpm])
            a1, ba1 = w1[0]
            P.op("dve", lambda e: e.tensor_tensor(out=a1[:], in0=pm[:, 0:256], in1=c_ptm[:, 3, :], op=ALU.add), reads=[bpm, bc], writes=[ba1])
            P.op("act", lambda e: e.activation(out=a1[:], in_=a1[:], func=AF.Sigmoid), reads=[ba1], writes=[ba1])
            P.op("act", lambda e, rw_=rw_: e.activation(out=rw_[:, 0, :], in_=a1[:], func=AF.Exp, scale=-0.6065306597126334), reads=[ba1], writes=[brw_])
            P.op("pe", lambda e, l_=l_: e.matmul(pm[:, 0:256], lhsT=l_[64:128, 0, :], rhs=c_wa[64:128, :], start=True, stop=True), reads=[bl_, bc, ba1], writes=[bpm])
            al, bal = w1[1]
            P.op("dve", lambda e: e.tensor_tensor(out=al[:], in0=pm[:, 0:256], in1=c_ptm[:, 4, :], op=ALU.add), reads=[bpm, bc], writes=[bal])
            P.op("act", lambda e: e.activation(out=al[:], in_=al[:], func=AF.Sigmoid), reads=[bal], writes=[bal])
            g_, bg_ = gtm[i2]
            P.op("pe", lambda e, l_=l_: e.matmul(pm[:, 0:256], lhsT=l_[:, 1, :], rhs=c_gu[:], start=True, stop=True), reads=[bl_, bc, bal], writes=[bpm])
            P.op("act", lambda e, g_=g_: e.activation(out=g_[:], in_=pm[:, 0:256], func=AF.Copy), reads=[bpm], writes=[bg_])
            kk, bkk = w1[2]; k2, bk2 = w1[3]
            s_, bs_ = sm[i2]
            P.op("dve", lambda e, z=z: e.tensor_tensor(out=kk[:], in0=z[:, 1, :], in1=c_ptm[:, 5, :], op=ALU.mult), reads=[bz, bc], writes=[bkk])
            P.op("dve", lambda e: e.tensor_tensor(out=k2[:], in0=kk[:], in1=kk[:], op=ALU.mult), reads=[bkk], writes=[bk2])
            P.op("dve", lambda e, s_=s_: e.tensor_reduce(out=s_[:, 0:4], in_=k2[:].rearrange("p (h k) -> p h k", h=4), axis=AX.X, op=ALU.add), reads=[bk2], writes=[bs_])
            P.op("act", lambda e, s_=s_: e.activation(out=s_[:, 0:4], in_=s_[:, 0:4], func=AF.Sqrt), reads=[bs_], writes=[bs_])
            P.op("dve", lambda e, s_=s_: e.tensor_scalar(out=s_[:, 0:4], in0=s_[:, 0:4], scalar1=1e-12, scalar2=None, op0=ALU.max), reads=[bs_], writes=[bs_])
            P.op("dve", lambda e, s_=s_: e.reciprocal(out=s_[:, 0:4], in_=s_[:, 0:4]), reads=[bs_], writes=[bs_])
            P.op("dve", lambda e, s_=s_: e.tensor_tensor(out=kk[:].rearrange("p (h k) -> p h k", h=4), in0=kk[:].rearrange("p (h k) -> p h k", h=4),
                                                         in1=s_[:, 0:4].unsqueeze(2).to_broadcast([128, 4, 64]), op=ALU.mult), reads=[bkk, bs_], writes=[bkk])
            P.op("dve", lambda e, rw_=rw_: e.tensor_scalar(out=rw_[:, 1, :], in0=kk[:], scalar1=-1.0, scalar2=None, op0=ALU.mult), reads=[bkk], pwrites=[brw_])
            P.op("dve", lambda e, rw_=rw_: e.tensor_tensor(out=rw_[:, 2, :], in0=kk[:], in1=al[:], op=ALU.mult), reads=[bkk, bal], pwrites=[brw_])
            P.op("dve", lambda e: e.tensor_tensor(out=k2[:], in0=al[:], in1=c_ptm[:, 6, :], op=ALU.mult), reads=[bal, bc], writes=[bk2])
            P.op("dve", lambda e: e.tensor_tensor(out=k2[:], in0=k2[:], in1=c_omka[:], op=ALU.add), reads=[bk2, bc], writes=[bk2])
            P.op("dve", lambda e, rw_=rw_, z=z: e.tensor_tensor(out=rw_[:, 3, :], in0=z[:, 1, :], in1=k2[:], op=ALU.mult), reads=[bz, bk2], pwrites=[brw_])
            P.op("dve", lambda e, rw_=rw_, z=z: e.tensor_copy(out=rw_[:, 4, :], in_=z[:, 0, :]), reads=[bz], pwrites=[brw_])
            bn_, bbn_ = bon[i2]
            P.op("dve", lambda e, z=z: e.tensor_tensor(out=k2[:], in0=z[:, 0, :], in1=c_ptm[:, 7, :], op=ALU.mult), reads=[bz, bc], writes=[bk2])
            P.op("dve", lambda e, rw_=rw_: e.tensor_tensor(out=k2[:], in0=k2[:], in1=rw_[:, 3, :], op=ALU.mult), reads=[bk2, brw_], writes=[bk2])
            P.op("dve", lambda e, bn_=bn_: e.tensor_reduce(out=bn_[:], in_=k2[:].rearrange("p (h k) -> p h k", h=4), axis=AX.X, op=ALU.add), reads=[bk2], writes=[bbn_])
            y_, by_ = yT[i2]
            for t in range(128):
                pr = psRow[t % 2]
                for j, (c0, n) in enumerate(((0, 512), (512, 512), (1024, 256))):
                    P.op("pe", lambda e, pr=pr, j=j, c0=c0, n=n, rw_=rw_, t=t: e.matmul(
                        pr[j][0][:, 0:n], lhsT=c_sel[:, t, :], rhs=rw_[:].rearrange("p a b -> p (a b)")[:, c0:c0 + n], start=True, stop=True),
                        reads=[brw_, bc], writes=[pr[j][1]])
                row = lambda r: (pr[(r * 256) // 512][0][:, (r * 256) % 512:(r * 256) % 512 + 256].rearrange("p (h k) -> p h k", h=4), pr[(r * 256) // 512][1])
                wr, bwr = row(0); ar, bar = row(1); br_, bbr = row(2); kr, bkr = row(3); rr, brr = row(4)
                tS, _ = tmpS[t % 2]; sa, _ = saS[t % 2]; tP, _ = tmpP[t % 2]
                G2 = (slice(0, 2), slice(2, 4))
                bts = bTS[t % 2]; bsa2 = bSA[t % 2]; btp = bTP[t % 2]
                def both(fn):
                    for g in range(2):
                        fn(g, G2[g])
                both(lambda g, hs: P.op("dve", lambda e, hs=hs, tS=tS, sa=sa, tP=tP, ar=ar, wr=wr, br_=br_, kr=kr, rr=rr, v_=v_, y_=y_: e.tensor_tensor(out=tS[:, hs, :], in0=Srw[:, hs, :], in1=ar[:, hs, :], op=ALU.mult),
                                        reads=[bSg[g], bar], writes=[bts[g]]))
                both(lambda g, hs: P.op("dve", lambda e, hs=hs, tS=tS, sa=sa, tP=tP, ar=ar, wr=wr, br_=br_, kr=kr, rr=rr, v_=v_, y_=y_: e.tensor_reduce(out=sa[:, hs], in_=tS[:, hs, :], axis=AX.X, op=ALU.add),
                                        reads=[bts[g]], writes=[bsa2[g]]))
                both(lambda g, hs: P.op("dve", lambda e, hs=hs, tS=tS, sa=sa, tP=tP, ar=ar, wr=wr, br_=br_, kr=kr, rr=rr, v_=v_, y_=y_: e.tensor_tensor(out=Srw[:, hs, :], in0=Srw[:, hs, :], in1=wr[:, hs, :], op=ALU.mult),
                                        reads=[bSg[g], bwr], writes=[bSg[g]]))
                both(lambda g, hs: P.op("dve", lambda e, hs=hs, tS=tS, sa=sa, tP=tP, ar=ar, wr=wr, br_=br_, kr=kr, rr=rr, v_=v_, y_=y_: e.tensor_tensor(out=tS[:, hs, :], in0=br_[:, hs, :], in1=sa[:, hs].unsqueeze(2).to_broadcast([64, 2, 64]), op=ALU.mult),
                                        reads=[bbr, bsa2[g]], writes=[bts[g]]))
                both(lambda g, hs: P.op("dve", lambda e, hs=hs, tS=tS, sa=sa, tP=tP, ar=ar, wr=wr, br_=br_, kr=kr, rr=rr, v_=v_, y_=y_: e.tensor_tensor(out=Srw[:, hs, :], in0=Srw[:, hs, :], in1=tS[:, hs, :], op=ALU.add),
                                        reads=[bSg[g], bts[g]], writes=[bSg[g]]))
                both(lambda g, hs: P.op("dve", lambda e, hs=hs, t=t, tS=tS, sa=sa, tP=tP, ar=ar, wr=wr, br_=br_, kr=kr, rr=rr, v_=v_, y_=y_: e.tensor_tensor(out=tP[:, hs, :], in0=kr[:, hs, :], in1=v_[:, hs, t:t + 1].to_broadcast([64, 2, 64]), op=ALU.mult),
                                        reads=[bkr, bv_], writes=[btp[g]]))
                both(lambda g, hs: P.op("dve", lambda e, hs=hs, tS=tS, sa=sa, tP=tP, ar=ar, wr=wr, br_=br_, kr=kr, rr=rr, v_=v_, y_=y_: e.tensor_tensor(out=Srw[:, hs, :], in0=Srw[:, hs, :], in1=tP[:, hs, :], op=ALU.add),
                                        reads=[bSg[g], btp[g]], writes=[bSg[g]]))
                both(lambda g, hs: P.op("dve", lambda e, hs=hs, tS=tS, sa=sa, tP=tP, ar=ar, wr=wr, br_=br_, kr=kr, rr=rr, v_=v_, y_=y_: e.tensor_tensor(out=tS[:, hs, :], in0=Srw[:, hs, :], in1=rr[:, hs, :], op=ALU.mult),
                                        reads=[bSg[g], brr], writes=[bts[g]]))
                both(lambda g, hs: P.op("dve", lambda e, hs=hs, t=t, tS=tS, sa=sa, tP=tP, ar=ar, wr=wr, br_=br_, kr=kr, rr=rr, v_=v_, y_=y_: e.tensor_reduce(out=y_[:, hs, t], in_=tS[:, hs, :], axis=AX.X, op=ALU.add),
                                        reads=[bts[g]], **(dict(writes=[by_]) if (t == 0 and g == 0) else dict(pwrites=[by_]))))
            vt_, bvt_ = vtm[i2]
            o_, bo_ = orw[i2]
            for h in range(4):
                P.op("pe", lambda e, h=h, y_=y_: e.transpose(out=pm[:, h * 64:(h + 1) * 64], in_=y_[:, h, :], identity=c_id[0:64, 0:64]),
                     reads=[by_, bc] + ([bg_] if h == 0 else []), **(dict(writes=[bpm]) if h == 0 else dict(pwrites=[bpm])))
            s6, bs6 = st6[i2]; m_, bm_ = mv[i2]
            for h in range(4):
                P.op("dve", lambda e, h=h, s6=s6: e.bn_stats(out=s6[:, h, :], in_=pm[:, h * 64:(h + 1) * 64]), reads=[bpm], **(dict(writes=[bs6]) if h == 0 else dict(pwrites=[bs6])))
            for h in range(4):
                P.op("dve", lambda e, h=h, s6=s6, m_=m_: e.bn_aggr(out=m_[:, h, :], in_=s6[:, h, :]), reads=[bs6], **(dict(writes=[bm_]) if h == 0 else dict(pwrites=[bm_])))
            P.op("act", lambda e, m_=m_: e.activation(out=m_[:, :, 1], in_=m_[:, :, 1], func=AF.Sqrt, bias=c_bias[:, 1:2]), reads=[bm_, bc], writes=[bm_])
            P.op("dve", lambda e, m_=m_: e.reciprocal(out=m_[:, :, 1], in_=m_[:, :, 1]), reads=[bm_], writes=[bm_])
            for h in range(4):
                P.op("dve", lambda e, h=h, m_=m_, o_=o_: e.tensor_scalar(out=o_[:, h * 64:(h + 1) * 64], in0=pm[:, h * 64:(h + 1) * 64],
                                                                         scalar1=m_[:, h, 0:1], scalar2=m_[:, h, 1:2], op0=ALU.subtract, op1=ALU.mult),
                     reads=[bpm, bm_], **(dict(writes=[bo_]) if h == 0 else dict(pwrites=[bo_])))
            P.op("dve", lambda e, o_=o_: e.tensor_tensor(out=o_[:], in0=o_[:], in1=c_ptm[:, 8, :], op=ALU.mult), reads=[bo_, bc], writes=[bo_])
            P.op("dve", lambda e, o_=o_: e.tensor_tensor(out=o_[:], in0=o_[:], in1=c_ptm[:, 9, :], op=ALU.add), reads=[bo_, bc], writes=[bo_])
            P.op("dve", lambda e, z=z, bn_=bn_: e.tensor_tensor(out=k2[:].rearrange("p (h k) -> p h k", h=4), in0=z[:, 2, :].rearrange("p (h k) -> p h k", h=4),
                                                                in1=bn_[:].unsqueeze(2).to_broadcast([128, 4, 64]), op=ALU.mult), reads=[bz, bbn_], writes=[bk2])
            P.op("dve", lambda e, o_=o_: e.tensor_tensor(out=o_[:], in0=o_[:], in1=k2[:], op=ALU.add), reads=[bo_, bk2], writes=[bo_])
            P.op("dve", lambda e, o_=o_, g_=g_: e.tensor_tensor(out=o_[:], in0=o_[:], in1=g_[:], op=ALU.mult), reads=[bo_, bg_], writes=[bo_])
            P.dma(y_rw[tok, :], o_[:], reads=[bo_])

            q_, bq_ = rqt[i2]; k_, bk_ = rkt[i2]; gr_, bgr_ = rgt[i2]; rv_, brv_ = rvt[i2]
            pt_, bpt_ = post[i2]; pf_, bpf_ = posf[i2]; cs_, bcs_ = cs[i2]; rr_, brr_ = rrow[i2]
            P.dma(q_[:], A.rq(c), writes=[bq_]); P.dma(k_[:], A.rk(c), writes=[bk_]); P.dma(gr_[:], A.rgr(c), writes=[bgr_])
            P.dma(rv_[:], A.rvT(c), writes=[brv_]); P.dma(pt_[:, 0:1], pos[tok, :], writes=[bpt_])
            P.op("pool", lambda e, pt_=pt_, pf_=pf_: e.tensor_copy(out=pf_[:, 0:1], in_=pt_[:, 0:1]), reads=[bpt_], writes=[bpf_])
            ang, bang = w2[0]; ang2, bang2 = w2[1]
            P.op("dve", lambda e, pf_=pf_: e.tensor_scalar(out=ang[:, 0:64], in0=c_invf[:], scalar1=pf_[:, 0:1], scalar2=None, op0=ALU.mult), reads=[bpf_, bc], writes=[bang])
            P.op("dve", lambda e: e.tensor_scalar(out=ang2[:, 0:64], in0=ang[:, 0:64], scalar1=1.5707963267948966, scalar2=None, op0=ALU.add), reads=[bang], writes=[bang2])
            ki_, bki_ = kit[i2]
            for (a_, ba_, col) in ((ang2, bang2, 0), (ang, bang, 1)):
                P.op("dve", lambda e, a_=a_: e.tensor_scalar(out=a_[:, 64:128], in0=a_[:, 0:64], scalar1=1.0 / TWO_PI, scalar2=None, op0=ALU.mult), reads=[ba_], writes=[ba_])
                P.op("dve", lambda e, a_=a_, ki_=ki_: e.tensor_copy(out=ki_[:], in_=a_[:, 64:128]), reads=[ba_], writes=[bki_])
                P.op("dve", lambda e, a_=a_, ki_=ki_: e.tensor_copy(out=a_[:, 64:128], in_=ki_[:]), reads=[bki_], writes=[ba_])
                P.op("dve", lambda e, a_=a_: e.scalar_tensor_tensor(out=a_[:, 0:64], in0=a_[:, 64:128], scalar=-TWO_PI, in1=a_[:, 0:64], op0=ALU.mult, op1=ALU.add), reads=[ba_], writes=[ba_])
                P.op("dve", lambda e, a_=a_: e.tensor_scalar(out=a_[:, 64:128], in0=a_[:, 0:64], scalar1=3.141592653589793, scalar2=-TWO_PI, op0=ALU.is_gt, op1=ALU.mult), reads=[ba_], writes=[ba_])
                P.op("dve", lambda e, a_=a_: e.tensor_tensor(out=a_[:, 0:64], in0=a_[:, 0:64], in1=a_[:, 64:128], op=ALU.add), reads=[ba_], writes=[ba_])
                kw = dict(writes=[bcs_]) if col == 0 else dict(pwrites=[bcs_])
                P.op("act", lambda e, cs_=cs_, a_=a_, col=col: e.activation(out=cs_[:, col, :], in_=a_[:, 0:64], func=AF.Sin), reads=[ba_], **kw)
            ta, bta = w2[2]; tb, btb = w2[3]
            for xi, (x_, bx_) in enumerate(((k_, bk_), (q_, bq_))):
                xv = x_[:].rearrange("p (h two d) -> p h two d", h=2, two=2)
                ov = rr_[:, xi, :].rearrange("p (h two d) -> p h two d", h=2, two=2)
                tav = ta[:, 0:128].rearrange("p (h d) -> p h d", h=2); tbv = tb[:, 0:128].rearrange("p (h d) -> p h d", h=2)
                ncb = lambda cs_=cs_: cs_[:, 0:1, :].to_broadcast([128, 2, 64])
                nsb = lambda cs_=cs_: cs_[:, 1:2, :].to_broadcast([128, 2, 64])
                kw0 = dict(writes=[brr_]) if xi == 0 else dict(pwrites=[brr_])
                P.op("pool", lambda e, xv=xv, tav=tav, ncb=ncb: e.tensor_tensor(out=tav, in0=xv[:, :, 0, :], in1=ncb(), op=ALU.mult), reads=[bx_, bcs_], writes=[bta])
                P.op("pool", lambda e, xv=xv, tbv=tbv, nsb=nsb: e.tensor_tensor(out=tbv, in0=xv[:, :, 1, :], in1=nsb(), op=ALU.mult), reads=[bx_, bcs_], writes=[btb])
                P.op("pool", lambda e, ov=ov, tav=tav, tbv=tbv: e.tensor_tensor(out=ov[:, :, 0, :], in0=tav, in1=tbv, op=ALU.subtract), reads=[bta, btb], **kw0)
                P.op("pool", lambda e, xv=xv, tav=tav, ncb=ncb: e.tensor_tensor(out=tav, in0=xv[:, :, 1, :], in1=ncb(), op=ALU.mult), reads=[bx_, bcs_], writes=[bta])
                P.op("pool", lambda e, xv=xv, tbv=tbv, nsb=nsb: e.tensor_tensor(out=tbv, in0=xv[:, :, 0, :], in1=nsb(), op=ALU.mult), reads=[bx_, bcs_], writes=[btb])
                P.op("pool", lambda e, ov=ov, tav=tav, tbv=tbv: e.tensor_tensor(out=ov[:, :, 1, :], in0=tav, in1=tbv, op=ALU.add), reads=[bta, btb], pwrites=[brr_])
            for xi in range(2):
                for h in range(2):
                    P.op("pool", lambda e, rr_=rr_, xi=xi, h=h: e.tensor_scalar(out=rr_[:, xi, h * 128:(h + 1) * 128], in0=rr_[:, xi, h * 128:(h + 1) * 128],
                                                                                scalar1=c_kq[:, 2 * xi + h:2 * xi + h + 1], scalar2=None, op0=ALU.mult),
                         reads=[brr_, bc], pwrites=[brr_])
            pr_, bpr_ = psRet
            ry_, bry_ = ryT[i2]
            for t in range(128):
                P.op("pe", lambda e, rr_=rr_, t=t: e.matmul(pr_[:], lhsT=c_selr[:, t, :], rhs=rr_[:].rearrange("p a b -> p (a b)"), start=True, stop=True),
                     reads=[brr_, bc], writes=[bpr_])
                rs_, brs_ = rrs[t % 2]
                P.op("act", lambda e, rs_=rs_: e.activation(out=rs_[:], in_=pr_[:], func=AF.Copy), reads=[bpr_], writes=[brs_])
                t1, bt1 = tmpR[t % 2]; t2, bt2 = tmpR2[t % 2]
                krow = rs_[:, 0:256].rearrange("p (h d) -> p h d", h=2); qrow = rs_[:, 256:512].rearrange("p (h d) -> p h d", h=2)
                for h in range(2):
                    P.op("pool", lambda e, t1=t1, krow=krow, rv_=rv_, t=t, h=h: e.tensor_tensor(out=t1[:, h, :], in0=krow[:, h, :], in1=rv_[:, h, t:t + 1].to_broadcast([128, 128]), op=ALU.mult),
                         reads=[brs_, brv_], writes=[bT1[t % 2][h]])
                for h in range(2):
                    P.op("pool", lambda e, t1=t1, h=h: e.tensor_tensor(out=Rr[:, h, :], in0=Rr[:, h, :], in1=t1[:, h, :], op=ALU.add), reads=[bRg[h], bT1[t % 2][h]], writes=[bRg[h]])
                for h in range(2):
                    P.op("pool", lambda e, t2=t2, qrow=qrow, h=h: e.tensor_tensor(out=t2[:, h, :], in0=Rr[:, h, :], in1=qrow[:, h, :], op=ALU.mult), reads=[bRg[h], brs_], writes=[bT2[t % 2][h]])
                kw = dict(writes=[bry_]) if t == 0 else dict(pwrites=[bry_])
                P.op("dve", lambda e, t2=t2, ry_=ry_, t=t: e.tensor_reduce(out=ry_[:, :, t], in_=t2[:], axis=AX.X, op=ALU.add), reads=[bT2[t % 2][0], bT2[t % 2][1]], **kw)
            for h in range(2):
                P.op("pool", lambda e, h=h: e.tensor_tensor(out=Rr[:, h, :], in0=Rr[:, h, :], in1=c_gam[:, h, :], op=ALU.mult), reads=[bRg[h], bc], writes=[bRg[h]])
            for h in range(2):
                P.op("pe", lambda e, h=h, ry_=ry_: e.transpose(out=pm[:, h * 128:(h + 1) * 128], in_=ry_[:, h, :], identity=c_id[:]),
                     reads=[bry_, bc], **(dict(writes=[bpm]) if h == 0 else dict(pwrites=[bpm])))
            s6, bs6 = st6[i2]; m_, bm_ = mv[i2]
            orr, borr = oret[i2]
            for h in range(2):
                P.op("dve", lambda e, h=h, s6=s6: e.bn_stats(out=s6[:, h, :], in_=pm[:, h * 128:(h + 1) * 128]), reads=[bpm], **(dict(writes=[bs6]) if h == 0 else dict(pwrites=[bs6])))
            for h in range(2):
                P.op("dve", lambda e, h=h, s6=s6, m_=m_: e.bn_aggr(out=m_[:, h, :], in_=s6[:, h, :]), reads=[bs6], **(dict(writes=[bm_]) if h == 0 else dict(pwrites=[bm_])))
            P.op("act", lambda e, m_=m_: e.activation(out=m_[:, 0:2, 1], in_=m_[:, 0:2, 1], func=AF.Sqrt, bias=c_bias[:, 2:3]), reads=[bm_, bc], writes=[bm_])
            P.op("dve", lambda e, m_=m_: e.reciprocal(out=m_[:, 0:2, 1], in_=m_[:, 0:2, 1]), reads=[bm_], writes=[bm_])
            for h in range(2):
                P.op("dve", lambda e, h=h, m_=m_, orr=orr: e.tensor_scalar(out=orr[:, h * 128:(h + 1) * 128], in0=pm[:, h * 128:(h + 1) * 128],
                                                                           scalar1=m_[:, h, 0:1], scalar2=m_[:, h, 1:2], op0=ALU.subtract, op1=ALU.mult),
                     reads=[bpm, bm_], **(dict(writes=[borr]) if h == 0 else dict(pwrites=[borr])))
            P.op("dve", lambda e, orr=orr: e.tensor_tensor(out=orr[:], in0=orr[:], in1=c_gn[:, 0, :], op=ALU.mult), reads=[borr, bc], writes=[borr])
            P.op("dve", lambda e, orr=orr: e.tensor_tensor(out=orr[:], in0=orr[:], in1=c_gn[:, 1, :], op=ALU.add), reads=[borr, bc], writes=[borr])
            P.op("act", lambda e, gr_=gr_: e.activation(out=gr_[:], in_=gr_[:], func=AF.Silu), reads=[bgr_], writes=[bgr_])
            P.op("dve", lambda e, orr=orr, gr_=gr_: e.tensor_tensor(out=orr[:], in0=orr[:], in1=gr_[:], op=ALU.mult), reads=[borr, bgr_], writes=[borr])
            P.dma(y_ret[tok, :], orr[:], reads=[borr])


def build_B(S):
    nc = bass.Bass("TRN2", target_bir_lowering=False)
    dt = lambda n, s, k="ExternalInput", d=F32: nc.dram_tensor(n, s, d, kind=k).ap()
    ztm = dt("ztm", [S, 3, 256]); ztm_p = dt("ztm_p", [S, 3, 256])
    zvT = dt("zvT", [64, 4, S]); zvT_p = dt("zvT_p", [64, 4, S])
    zlT = dt("zlT", [128, 2, S]); zlT_p = dt("zlT_p", [128, 2, S])
    ptm = dt("ptm", [128, 10, 256]); mu_l = dt("mu_l", [128, 2]); mu_vT = dt("mu_vT", [64, 4])
    wa_up = dt("wa_up", [128, 256]); g_up = dt("g_up", [128, 256])
    sel = dt("sel", [128, 128, 64]); identf = dt("identf", [128, 128])
    rq = dt("rq", [S, 256]); rk = dt("rk", [S, 256]); rvT = dt("rvT", [128, 2, S]); rgr = dt("rgr", [S, 256])
    pos = dt("pos", [S, 1], d=I32); invf = dt("invf", [128, 64]); gam = dt("gam", [128, 2, 128])
    gn = dt("gn", [128, 2, 256]); selr = dt("selr", [128, 128, 128]); kq = dt("kq", [128, 4])
    y_rw = dt("y_rw", [S, 256], "ExternalOutput"); y_ret = dt("y_ret", [S, 256], "ExternalOutput")

    P = Prog(nc)
    class A: pass
    A.ptm, A.mu_l, A.mu_vT, A.wa_up, A.g_up, A.sel, A.identf = ptm, mu_l, mu_vT, wa_up, g_up, sel, identf
    A.invf, A.gam, A.gn, A.selr, A.y_rw, A.y_ret, A.pos = invf, gam, gn, selr, y_rw, y_ret, pos
    A.kq = kq
    ck = lambda c: slice(c * 128, (c + 1) * 128)
    A.ztm = lambda c: ztm[ck(c)]; A.ztm_p = lambda c: ztm_p[ck(c)]
    A.zvT = lambda c: zvT[:, :, ck(c)]; A.zvT_p = lambda c: zvT_p[:, :, ck(c)]
    A.zlT = lambda c: zlT[:, :, ck(c)]; A.zlT_p = lambda c: zlT_p[:, :, ck(c)]
    A.rq = lambda c: rq[ck(c), :]; A.rk = lambda c: rk[ck(c), :]; A.rgr = lambda c: rgr[ck(c), :]
    A.rvT = lambda c: rvT[:, :, ck(c)]
    scan_phase(P, S, A)
    P.barrier()
    P.emit(); P.close()
    return nc


def shift_prev(a, axis=0):
    out = np.zeros_like(a)
    sl_dst = [slice(None)] * a.ndim; sl_src = [slice(None)] * a.ndim
    sl_dst[axis] = slice(1, None); sl_src[axis] = slice(0, -1)
    out[tuple(sl_dst)] = a[tuple(sl_src)]
    return out


def rep128(v):
    return np.ascontiguousarray(np.broadcast_to(np.asarray(v, np.float32).reshape(1, -1), (128, v.size)))


def prep_B_rwkv(zr, zk, zv, zw, za, zg, p):
    S = zr.shape[0]
    d = {}
    ztm = np.stack([zr, zk, zv], 1).astype(np.float32)
    d["ztm"] = ztm; d["ztm_p"] = shift_prev(ztm, 0)
    zvT = np.ascontiguousarray(zv.reshape(S, 4, 64).transpose(2, 1, 0))
    d["zvT"] = zvT; d["zvT_p"] = shift_prev(zvT, 2)
    zl = np.stack([np.concatenate([zw, za], 1).T, zg.T], 1)
    zl = np.ascontiguousarray(zl.astype(np.float32))
    d["zlT"] = zl; d["zlT_p"] = shift_prev(zl, 2)
    ptm = np.stack([rep128(p[k]) for k in ("mu_r", "mu_k", "mu_v", "w0", "a0", "k_k", "k_a", "r_k", "ln_g", "ln_b")], 1)
    d["ptm"] = np.ascontiguousarray(ptm)
    d["mu_l"] = np.ascontiguousarray(np.stack([np.concatenate([p["mu_w"], p["mu_a"]]), p["mu_g"]], 1).astype(np.float32))
    d["mu_vT"] = np.ascontiguousarray(p["mu_v"].reshape(4, 64).T.astype(np.float32))
    d["wa_up"] = np.ascontiguousarray(np.concatenate([p["w_up"], p["a_up"]], 0).astype(np.float32))
    d["g_up"] = np.ascontiguousarray(p["g_up"].astype(np.float32))
    sel = np.zeros((128, 128, 64), np.float32)
    sel[np.arange(128), np.arange(128), :] = 1.0
    d["sel"] = sel
    d["identf"] = np.eye(128, dtype=np.float32)
    return d


def ret_consts(heads):
    gamma = 1.0 - 2.0 ** (-5.0 - np.asarray(heads, np.float64))
    t1 = np.arange(1, 129, dtype=np.float64)[:, None]
    kq = np.concatenate([gamma[None, :] ** (-t1) * 128.0 ** -0.5, gamma[None, :] ** t1], 1).astype(np.float32)
    gam = np.ascontiguousarray(np.broadcast_to((gamma ** 128).astype(np.float32).reshape(1, 2, 1), (128, 2, 128)))
    return gam, np.ascontiguousarray(kq)


def prep_B_ret(zq, zk, zv, zgr, pos, gn_g, gn_b, heads):
    S = zq.shape[0]
    d = {}
    d["rq"] = np.ascontiguousarray(zq, np.float32); d["rk"] = np.ascontiguousarray(zk, np.float32)
    d["rgr"] = np.ascontiguousarray(zgr, np.float32)
    d["rvT"] = np.ascontiguousarray(zv.reshape(S, 2, 128).transpose(2, 1, 0).astype(np.float32))
    d["pos"] = np.ascontiguousarray(pos.reshape(S, 1).astype(np.int32))
    inv = (10000.0 ** (-np.arange(64, dtype=np.float32) / np.float32(64))).astype(np.float32)
    d["invf"] = rep128(inv)
    d["gam"], d["kq"] = ret_consts(heads)
    d["gn"] = np.ascontiguousarray(np.stack([rep128(gn_g), rep128(gn_b)], 1))
    selr = np.zeros((128, 128, 128), np.float32)
    selr[np.arange(128), np.arange(128), :] = 1.0
    d["selr"] = selr
    return d


RG4 = [[0, 1, 2, 3], [4, 5, 6, 7]]
NTM = 1536
NFM = 768


def load_w_resident(P, C, w_ap, dst, bdst, ncols, nkc=16):
    wv = w_ap.rearrange("(kc p) c -> p kc c", p=128)
    for c0 in range(0, ncols, 512):
        n_c = min(512, ncols - c0)
        for q in range(0, nkc, 4):
            n = min(4, nkc - q)
            st, bst = C.wst[C.wst_i % 4]
            C.wst_i += 1
            P.dma(st[:, 0:n, 0:n_c], wv[:, q:q + n, c0:c0 + n_c], writes=[bst])
            eng = "pool" if (C.wst_i % 2) else "dve"
            P.op(eng, lambda e, st=st, q=q, n=n, c0=c0, n_c=n_c: e.tensor_copy(out=dst[:, q:q + n, c0:c0 + n_c], in_=st[:, 0:n, 0:n_c]),
                 reads=[bst], pwrites=[bdst])


def scanproj_phase(P, C, xb, w_tm, w_fm, g1rep, ZT, ZF, S):
    with P.scope():
        alloc_psum(P, C)
        alloc_norm(P, C)
        C.wst = [(P.sbuf("wst%d" % i, [128, 4, 512], F32), P.buf("wst%d" % i)) for i in range(4)]
        C.wst_i = 0
        Wr = P.sbuf("Wres", [128, KC, NTM + NFM], BF16)
        bWr = P.buf("Wres")
        load_w_resident(P, C, w_tm, Wr[:, :, 0:NTM], bWr, NTM)
        load_w_resident(P, C, w_fm, Wr[:, :, NTM:NTM + NFM], bWr, NFM)
        zo = [(P.sbuf("zo%d" % i, [128, 512], F32), P.buf("zo%d" % i)) for i in range(4)]
        P.dma(C.gsb[0][:], g1rep, writes=[C.gsb[1]])
        oi = 0
        ai = 0
        for tt in range(S // TT):
            def xload(s_, xt, bxt, tt=tt):
                t0 = tt * TT + s_ * 128
                q, i0 = t0 // (S // 4), t0 % (S // 4)
                r0 = i0 // 64
                P.dma(xt[0:64, :], xb[r0, q * 64:(q + 1) * 64, :], writes=[bxt])
                P.dma(xt[64:128, :], xb[r0 + 1, q * 64:(q + 1) * 64, :], pwrites=[bxt])
            norm_T(P, C, None, None, xload=xload)
            hT, bhT = C.hT
            for cb in range(NTM // 512):
                for s in range(NSUB):
                    ps, bps = C.acc[ai % 6]
                    ai += 1
                    for kc in range(KC):
                        kw = dict(writes=[bps]) if kc == 0 else dict(pwrites=[bps])
                        P.op("pe", lambda e, ps=ps, kc=kc, s=s, cb=cb: e.matmul(
                            ps[:], lhsT=hT[:, kc, s * 128:(s + 1) * 128], rhs=Wr[:, kc, cb * 512:(cb + 1) * 512],
                            start=(kc == 0), stop=(kc == KC - 1)), reads=[bWr, bhT], **kw)
                    o, bo = zo[oi % 4]
                    oi += 1
                    eng = "act" if oi % 2 else "dve"
                    if eng == "act":
                        P.op("act", lambda e, o=o, ps=ps: e.activation(out=o[:], in_=ps[:], func=AF.Copy), reads=[bps], writes=[bo])
                    else:
                        P.op("dve", lambda e, o=o, ps=ps: e.tensor_copy(out=o[:], in_=ps[:]), reads=[bps], writes=[bo])
                    r0 = 1 + tt * TT + s * 128
                    P.dma(ZT[r0:r0 + 128, cb * 512:(cb + 1) * 512], o[:], reads=[bo])
            for r in range(NFM // 128):
                ps, bps = C.acc[ai % 6]
                ai += 1
                for kc in range(KC):
                    kw = dict(writes=[bps]) if kc == 0 else dict(pwrites=[bps])
                    P.op("pe", lambda e, ps=ps, kc=kc, r=r: e.matmul(
                        ps[:], lhsT=Wr[:, kc, NTM + r * 128:NTM + (r + 1) * 128], rhs=hT[:, kc, :],
                        start=(kc == 0), stop=(kc == KC - 1)), reads=[bWr, bhT], **kw)
                o, bo = zo[oi % 4]
                oi += 1
                if oi % 2:
                    P.op("act", lambda e, o=o, ps=ps: e.activation(out=o[:], in_=ps[:], func=AF.Copy), reads=[bps], writes=[bo])
                else:
                    P.op("dve", lambda e, o=o, ps=ps: e.tensor_copy(out=o[:], in_=ps[:]), reads=[bps], writes=[bo])
                P.dma(ZF[r * 128:(r + 1) * 128, 1 + tt * TT:1 + (tt + 1) * TT], o[:], reads=[bo])


def partial_phase(P, C, Y, w_ab, PA, PB, S):
    with P.scope():
        alloc_psum(P, C)
        C.wst = [(P.sbuf("wst%d" % i, [128, 4, 512], F32), P.buf("wst%d" % i)) for i in range(4)]
        C.wst_i = 0
        wab = P.sbuf("wab", [128, 4, D], BF16)
        bwab = P.buf("wab")
        load_w_resident(P, C, w_ab.rearrange("a r c -> (a r) c"), wab, bwab, D, nkc=4)
        yt = [(P.sbuf("yt%d" % i, [128, 512], F32), P.buf("yt%d" % i)) for i in range(2)]
        ybf = [(P.sbuf("ybf%d" % i, [128, 512], BF16), P.buf("ybf%d" % i)) for i in range(2)]
        yT = [(P.sbuf("yTp%d" % i, [128, 4, 128], BF16), P.buf("yTp%d" % i)) for i in range(2)]
        ot = [(P.sbuf("pot%d" % i, [128, D], F32), P.buf("pot%d" % i)) for i in range(2)]
        oi = 0
        ai = 0
        for r in range(S // 128):
            rows = slice(r * 128, (r + 1) * 128)
            y_, by_ = yt[r % 2]; yb_, byb_ = ybf[r % 2]; yT_, byT_ = yT[r % 2]
            P.dma(y_[:], Y[rows, :], writes=[by_])
            P.op("pool", lambda e, y_=y_, yb_=yb_: e.tensor_copy(out=yb_[:], in_=y_[:]), reads=[by_], writes=[byb_])
            pt, bpt = C.ptr[r % 2]
            for i in range(4):
                kw = dict(writes=[bpt]) if i == 0 else dict(pwrites=[bpt])
                P.op("pe", lambda e, pt=pt, i=i, yb_=yb_: e.transpose(out=pt[:, i * 128:(i + 1) * 128], in_=yb_[:, i * 128:(i + 1) * 128],
                                                                      identity=C.identb[:]), reads=[byb_, C.bconst], **kw)
            P.op("act", lambda e, pt=pt, yT_=yT_: e.activation(out=yT_[:], in_=pt[:, 0:512].rearrange("p (a b) -> p a b", a=4), func=AF.Copy),
                 reads=[bpt], writes=[byT_])
            for br, dst in ((0, PA), (1, PB)):
                o, bo = ot[oi % 2]
                oi += 1
                for cb in range(4):
                    ps, bps = C.acc[ai % 6]
                    ai += 1
                    for kc in range(2):
                        kw = dict(writes=[bps]) if kc == 0 else dict(pwrites=[bps])
                        P.op("pe", lambda e, ps=ps, kc=kc, br=br, cb=cb, yT_=yT_: e.matmul(
                            ps[:], lhsT=yT_[:, 2 * br + kc, :], rhs=wab[:, 2 * br + kc, cb * 512:(cb + 1) * 512],
                            start=(kc == 0), stop=(kc == 1)), reads=[bwab, byT_], **kw)
                    kw = dict(writes=[bo]) if cb == 0 else dict(pwrites=[bo])
                    if cb % 2:
                        P.op("act", lambda e, o=o, ps=ps, cb=cb: e.activation(out=o[:, cb * 512:(cb + 1) * 512], in_=ps[:], func=AF.Copy), reads=[bps], **kw)
                    else:
                        P.op("dve", lambda e, o=o, ps=ps, cb=cb: e.tensor_copy(out=o[:, cb * 512:(cb + 1) * 512], in_=ps[:]), reads=[bps], **kw)
                P.dma(dst[0][rows, :], o[:, 0:D // 2], reads=[bo])
                P.dma(dst[1][rows, :], o[:, D // 2:D], reads=[bo])


def merge2_phase(P, C, xres, bxres, AO, BO, G, w_out, ntiles):
    with P.scope():
        alloc_psum(P, C)
        alloc_wstream(P, C)
        tl = [[(P.sbuf("mg%d_%d" % (i, j), [128, D], F32), P.buf()) for j in range(4)] for i in range(2)]
        mb = (P.sbuf("mb", [128, D], BF16), P.buf("mb"))
        mT = P.sbuf("mT", [128, KC, TT], BF16)
        bmT = P.buf("mT")
        xo = [(P.sbuf("xo%d" % i, [128, 512], F32), P.buf("xo%d" % i)) for i in range(2)]
        oi = 0
        si = 0
        for tt in range(ntiles):
            for s in range(NSUB):
                rows = slice(tt * TT + s * 128, tt * TT + (s + 1) * 128)
                (a_, ba_), (b_, bb_), (ga_, bga_), (gb_, bgb_) = tl[si % 2]
                si += 1
                P.dma(a_[:, 0:D // 2], AO[0][rows, :], writes=[ba_]); P.dma(a_[:, D // 2:D], AO[1][rows, :], pwrites=[ba_])
                P.dma(b_[:, 0:D // 2], BO[0][rows, :], writes=[bb_]); P.dma(b_[:, D // 2:D], BO[1][rows, :], pwrites=[bb_])
                P.dma(ga_[:], G[rows, 0:D], writes=[bga_]); P.dma(gb_[:], G[rows, D:2 * D], writes=[bgb_])
                P.op("act", lambda e, ga_=ga_: e.activation(out=ga_[:], in_=ga_[:], func=AF.Sigmoid), reads=[bga_], writes=[bga_])
                P.op("act", lambda e, gb_=gb_: e.activation(out=gb_[:], in_=gb_[:], func=AF.Sigmoid), reads=[bgb_], writes=[bgb_])
                P.op("dve", lambda e, a_=a_, ga_=ga_: e.tensor_tensor(out=a_[:], in0=a_[:], in1=ga_[:], op=ALU.mult), reads=[ba_, bga_], writes=[ba_])
                P.op("pool", lambda e, b_=b_, gb_=gb_: e.tensor_tensor(out=b_[:], in0=b_[:], in1=gb_[:], op=ALU.mult), reads=[bb_, bgb_], writes=[bb_])
                m_, bm_ = mb
                P.op("dve", lambda e, a_=a_, b_=b_: e.tensor_tensor(out=m_[:], in0=a_[:], in1=b_[:], op=ALU.add), reads=[ba_, bb_], writes=[bm_])
                for j in range(0, KC, 8):
                    pt, bpt = C.ptr[(j // 8) % 2]
                    for i in range(8):
                        kw = dict(writes=[bpt]) if i == 0 else dict(pwrites=[bpt])
                        P.op("pe", lambda e, pt=pt, i=i, j=j: e.transpose(out=pt[:, i * 128:(i + 1) * 128], in_=m_[:, (j + i) * 128:(j + i + 1) * 128],
                                                                           identity=C.identb[:]), reads=[bm_, C.bconst], **kw)
                    kw = dict(writes=[bmT]) if (s == 0 and j == 0) else dict(pwrites=[bmT])
                    P.op("act", lambda e, pt=pt, j=j, s=s: e.activation(out=mT[:, j:j + 8, s * 128:(s + 1) * 128],
                                                                         in_=pt[:].rearrange("p (a b) -> p a b", a=8), func=AF.Copy), reads=[bpt], **kw)
            bx = bxres[tt]
            for cb in range(D // 512):
                wb, bwb = load_w_block(P, C, w_out[:, cb * 512:(cb + 1) * 512])
                for s in range(NSUB):
                    ps, bps = C.acc[s]
                    for kc in range(KC):
                        kw = dict(writes=[bps]) if kc == 0 else dict(pwrites=[bps])
                        P.op("pe", lambda e, ps=ps, wb=wb, kc=kc, s=s: e.matmul(
                            ps[:], lhsT=mT[:, kc, s * 128:(s + 1) * 128], rhs=wb[:, kc, :],
                            start=(kc == 0), stop=(kc == KC - 1)), reads=[bwb, bmT], **kw)
                    o, bo = xo[oi % 2]
                    oi += 1
                    rows = slice(tt * TT + s * 128, tt * TT + (s + 1) * 128)
                    P.dma(o[:], xres[rows, cb * 512:(cb + 1) * 512], reads=[bx], writes=[bo])
                    P.op("dve", lambda e, o=o, ps=ps: e.tensor_tensor(out=o[:], in0=o[:], in1=ps[:], op=ALU.add), reads=[bps, bo], writes=[bo])
                    P.dma(xres[rows, cb * 512:(cb + 1) * 512], o[:], reads=[bo], pwrites=[bx])


def build_fused(S, L, stop=None):
    N = S // 4
    nc = bass.Bass("TRN2", target_bir_lowering=False)
    dt = lambda n, s, k="ExternalInput", d=F32: nc.dram_tensor(n, s, d, kind=k).ap()
    x = dt("x", [N, D]); pos = dt("pos", [S, 1], d=I32)
    w_tm = dt("w_tm", [L, D, NTM]); w_fm = dt("w_fm", [L, D, NFM]); w_gate = dt("w_gate", [L, D, 2 * D])
    w_ab = dt("w_ab", [L, 2, 256, D]); w_out = dt("w_out", [L, D, D]); w_up = dt("w_up", [L, D, DFF]); w_down = dt("w_down", [L, DFF, D])
    g1 = dt("g1", [L, 128, D]); g2 = dt("g2", [L, 128, D]); gf = dt("gf", [128, D])
    ptm = dt("ptm", [L, 128, 10, 256]); mu_l = dt("mu_l", [L, 128, 2]); mu_vT = dt("mu_vT", [L, 64, 4])
    wa_up = dt("wa_up", [L, 128, 256]); g_up = dt("g_up", [L, 128, 256]); gn = dt("gn", [L, 128, 2, 256])
    sel = dt("sel", [128, 128, 64]); selr = dt("selr", [128, 128, 128]); identf = dt("identf", [128, 128])
    invf = dt("invf", [128, 64]); gam = dt("gam", [128, 2, 128]); kq = dt("kq", [128, 4])
    y = dt("y", [N, D], "ExternalOutput")
    it = lambda n, s: nc.dram_tensor(n, s, F32).ap()
    xres = it("xres", [N, D]); XB = it("XB", [N // 64, 256, D]); ZT = it("ZT", [1 + S, NTM]); ZF = it("ZF", [NFM, 1 + S])
    G = it("G", [N, 2 * D]); Y = it("Y", [S, 512]); PA = [it("PA%d" % i, [S, D // 2]) for i in range(2)]; PB = [it("PB%d" % i, [S, D // 2]) for i in range(2)]
    AO = [it("AO%d" % i, [N, D // 2]) for i in range(2)]; BO = [it("BO%d" % i, [N, D // 2]) for i in range(2)]

    P = Prog(nc); C = Ctx()
    setup_common(P, C, {"identf": identf})
    nt = N // TT
    bxres = [P.buf() for _ in range(nt)]
    for tt in range(nt):
        P.dma(xres[tt * TT:(tt + 1) * TT, :], x[tt * TT:(tt + 1) * TT, :], writes=[bxres[tt]])
    with P.scope():
        zt_ = P.sbuf("zeros", [128, NTM], F32); bz_ = P.buf()
        P.op("dve", lambda e: e.memset(zt_[:], 0.0), writes=[bz_])
        P.dma(ZT[0:1, :], zt_[0:1, :], reads=[bz_])
        for r in range(NFM // 128):
            P.dma(ZF[r * 128:(r + 1) * 128, 0:1], zt_[:, 0:1], reads=[bz_], allow_slow_non_contiguous=True)
    ck1 = lambda c: slice(1 + c * 128, 1 + (c + 1) * 128)
    ck0 = lambda c: slice(c * 128, (c + 1) * 128)
    for l in range(L):
        P.barrier()
        for r_ in range(N // 64):
            P.cc(lambda e, r_=r_: e.collective_compute("AllGather", ALU.bypass, replica_groups=RG4,
                                                       ins=[xres[r_ * 64:(r_ + 1) * 64, :].opt()], outs=[XB[r_].opt()]))
        P.barrier()
        if stop == "ag":
            break
        scanproj_phase(P, C, XB, w_tm[l], w_fm[l], g1[l], ZT, ZF, S)
        if stop == "scanproj":
            break
        bG = P.buf()
        proj_phase(P, C, xres, None, w_gate[l], g1[l], G, bG, nt, 2 * D)
        if stop == "gates":
            break

        class A:
            pass
        A.ptm, A.mu_l, A.mu_vT, A.wa_up, A.g_up, A.gn = ptm[l], mu_l[l], mu_vT[l], wa_up[l], g_up[l], gn[l]
        A.sel, A.selr, A.identf, A.invf, A.gam, A.pos = sel, selr, identf, invf, gam, pos
        A.kq = kq
        A.y_ret, A.y_rw = Y[:, 0:256], Y[:, 256:512]
        A.rq = lambda c: ZT[ck1(c), 0:256]; A.rk = lambda c: ZT[ck1(c), 256:512]; A.rgr = lambda c: ZT[ck1(c), 512:768]
        A.ztm = lambda c: ZT[ck1(c), 768:1536].rearrange("t (a n) -> t a n", a=3)
        A.ztm_p = lambda c: ZT[ck0(c), 768:1536].rearrange("t (a n) -> t a n", a=3)
        A.rvT = lambda c: ZF[0:256, ck1(c)].rearrange("(h e) t -> e h t", h=2)
        A.zvT = lambda c: ZF[256:512, ck1(c)].rearrange("(h d) t -> d h t", h=4)
        A.zvT_p = lambda c: ZF[256:512, ck0(c)].rearrange("(h d) t -> d h t", h=4)
        A.zlT = lambda c: ZF[512:768, ck1(c)].rearrange("(a p) t -> p a t", a=2)
        A.zlT_p = lambda c: ZF[512:768, ck0(c)].rearrange("(a p) t -> p a t", a=2)
        scan_phase(P, S, A)
        if stop == "scan":
            break
        partial_phase(P, C, Y, w_ab[l], PA, PB, S)
        if stop == "partial":
            break
        P.barrier()
        for src_, dst_ in ((PA[0], AO[0]), (PA[1], AO[1]), (PB[0], BO[0]), (PB[1], BO[1])):
            P.cc(lambda e, src_=src_, dst_=dst_: e.collective_compute("ReduceScatter", ALU.add, replica_groups=RG4, ins=[src_.opt()], outs=[dst_.opt()]))
        P.barrier()
        if stop == "rs":
            break
        merge2_phase(P, C, xres, bxres, AO, BO, G, w_out[l], nt)
        if stop == "merge":
            break
        ffn_phase(P, C, xres, bxres, w_up[l], w_down[l], g2[l], nt)
    bout = P.buf()
    final_norm_phase(P, C, xres, bxres, gf, y, bout, nt)
    P.barrier()
    P.emit(); P.close()
    return nc


def host_inputs_fused(inp, S, L):
    N = S // 4
    f32 = lambda a: np.ascontiguousarray(np.asarray(a), dtype=np.float32)
    x = np.asarray(inp["x"]); positions = np.asarray(inp["positions"]).astype(np.int32)
    w_in = np.asarray(inp["w_in"])
    common = dict(
        w_gate=f32(w_in[:L, :, 7424:11520]), w_out=f32(inp["w_out"][:L]), w_up=f32(inp["mlp_up"][:L]), w_down=f32(inp["mlp_down"][:L]),
        g1=np.stack([rep128(np.asarray(inp["norm1_g"][l], np.float32)) for l in range(L)]),
        g2=np.stack([rep128(np.asarray(inp["norm2_g"][l], np.float32)) for l in range(L)]),
        gf=rep128(np.asarray(inp["final_g"], np.float32)), identf=np.eye(128, dtype=np.float32))
    sel = np.zeros((128, 128, 64), np.float32); sel[np.arange(128), np.arange(128), :] = 1.0
    selr = np.zeros((128, 128, 128), np.float32); selr[np.arange(128), np.arange(128), :] = 1.0
    inv = (10000.0 ** (-np.arange(64, dtype=np.float32) / np.float32(64))).astype(np.float32)
    common.update(sel=sel, selr=selr, invf=rep128(inv))
    mu_all = np.asarray(inp["rwkv_mu"], np.float32)
    per_hg = {}
    for hg in range(4):
        cs = slice(hg * 256, (hg + 1) * 256)
        R0 = 4096
        col = lambda base: np.arange(base + hg * 256, base + (hg + 1) * 256)
        tm_cols = np.concatenate([col(0), col(1024), col(3072), col(R0), col(R0 + 1024), col(R0 + 2048)])
        fm_cols = np.concatenate([col(2048), col(R0 + 2048), np.arange(R0 + 3072, R0 + 3328)])
        d = dict(w_tm=f32(w_in[:L][:, :, tm_cols]), w_fm=f32(w_in[:L][:, :, fm_cols]),
                 w_ab=f32(np.stack([np.asarray(inp["w_branch_a"])[:L, cs, :], np.asarray(inp["w_branch_b"])[:L, cs, :]], 1)))
        pv = lambda name, l: np.asarray(inp[name][l], np.float32)
        ptm, mul, muv, waup, gup, gn = [], [], [], [], [], []
        for l in range(L):
            mu = mu_all[l]
            p = dict(mu_r=mu[0:1024][cs], mu_k=mu[1024:2048][cs], mu_v=mu[2048:3072][cs], mu_w=mu[3072:3136], mu_a=mu[3136:3200], mu_g=mu[3200:3328],
                     w0=pv("rwkv_w0", l)[cs], a0=pv("rwkv_a0", l)[cs], k_k=pv("rwkv_k_k", l)[cs], k_a=pv("rwkv_k_a", l)[cs], r_k=pv("rwkv_r_k", l)[cs],
                     ln_g=pv("rwkv_ln_g", l)[cs], ln_b=pv("rwkv_ln_b", l)[cs])
            ptm.append(np.stack([rep128(p[k]) for k in ("mu_r", "mu_k", "mu_v", "w0", "a0", "k_k", "k_a", "r_k", "ln_g", "ln_b")], 1))
            mul.append(np.stack([np.concatenate([p["mu_w"], p["mu_a"]]), p["mu_g"]], 1))
            muv.append(p["mu_v"].reshape(4, 64).T)
            waup.append(np.concatenate([pv("rwkv_w_up", l)[:, cs], pv("rwkv_a_up", l)[:, cs]], 0))
            gup.append(pv("rwkv_g_up", l)[:, cs])
            gn.append(np.stack([rep128(pv("ret_gn_g", l)[cs]), rep128(pv("ret_gn_b", l)[cs])], 1))
        d.update(ptm=f32(np.stack(ptm)), mu_l=f32(np.stack(mul)), mu_vT=f32(np.stack(muv)), wa_up=f32(np.stack(waup)), g_up=f32(np.stack(gup)), gn=f32(np.stack(gn)))
        d["gam"], d["kq"] = ret_consts([2 * hg, 2 * hg + 1])
        per_hg[hg] = d
    ins = []
    for c in range(8):
        b, j = c // 4, c % 4
        d = dict(common); d.update(per_hg[j])
        d["x"] = f32(x[b, j * N:(j + 1) * N]); d["pos"] = np.ascontiguousarray(positions[b, :S].reshape(S, 1))
        ins.append(d)
    return ins


from concourse.bass_utils import run_bass_kernel_spmd

_PROG = {}


def kernel(**inp):
    S, L = 8192, 4
    if "nc" not in _PROG:
        _PROG["nc"] = build_fused(S, L)
    ins = host_inputs_fused(inp, S, L)
    res = run_bass_kernel_spmd(_PROG["nc"], ins, core_ids=list(range(8)))
    out = np.stack([np.concatenate([res.results[4 * b + j]["y"] for j in range(4)], 0) for b in range(2)], 0)
    return np.ascontiguousarray(out, dtype=np.float32)
```

```python
import contextlib
import numpy as np
import concourse.bass as bass
import concourse.mybir as mybir

F32 = mybir.dt.float32
BF16 = mybir.dt.bfloat16
I32 = mybir.dt.int32
ALU = mybir.AluOpType
AF = mybir.ActivationFunctionType
AX = mybir.AxisListType

import os
ATTACH_WAIT = os.environ.get('ATTACH_WAIT', '1') == '1'
EPOCH = 30000
NDMA = 24


class Buf:
    __slots__ = ("name", "w", "rs")

    def __init__(self, name=""):
        self.name = name
        self.w = {}
        self.rs = {}


def _put(d, ev):
    k = id(ev[0])
    if k not in d or d[k][1] < ev[1]:
        d[k] = ev


class Prog:
    ENGS = ("pe", "act", "dve", "pool", "sp")

    def __init__(self, nc):
        self.nc = nc
        self.stack = contextlib.ExitStack()
        self.semstack = contextlib.ExitStack()
        self.eobj = {"pe": nc.tensor, "act": nc.scalar, "dve": nc.vector,
                     "pool": nc.gpsimd, "sp": nc.sync}
        self.lists = {e: [] for e in self.ENGS}
        self.cnt = {e: 0 for e in self.ENGS}
        self.cursem = {}
        self.seen = {e: {} for e in self.ENGS}
        self.nsem = 0
        self.own = {e: set() for e in self.ENGS}
        self.skip_self = set()
        for e in ("pe", "act", "dve", "pool"):
            self.cursem[e] = self.new_sem(e)
            self.own[e].add(id(self.cursem[e]))
        self.dsem = [self.new_sem("dma%d" % i) for i in range(NDMA)]
        self.dcum = [0] * NDMA
        self.dnext = 0
        self.nbuf = 0
        self.ccsem = self.new_sem("cc")
        self.cccum = 0

    def new_sem(self, name):
        self.nsem += 1
        return self.semstack.enter_context(self.nc.semaphore("s_%s_%d" % (name, self.nsem)))

    def sbuf(self, name, shape, dtype):
        self.nalloc = getattr(self, "nalloc", 0) + 1
        return self.stack.enter_context(self.nc.sbuf_tensor("sb_%s_%d" % (name, self.nalloc), list(shape), dtype))

    def psum(self, name, shape, dtype):
        self.nalloc = getattr(self, "nalloc", 0) + 1
        return self.stack.enter_context(self.nc.psum_tensor("ps_%s_%d" % (name, self.nalloc), list(shape), dtype))

    def buf(self, name=""):
        self.nbuf += 1
        return Buf(name or ("b%d" % self.nbuf))

    def _deps(self, reads, writes, pwrites=(), selfsem=None):
        evs = []
        for b in reads:
            if b is not None:
                evs.extend(b.w.values())
        for b in writes:
            if b is not None:
                evs.extend(b.w.values())
                evs.extend(b.rs.values())
        for b in pwrites:
            if b is not None:
                evs.extend(e for e in b.w.values() if e[0] is not selfsem)
                evs.extend(b.rs.values())
        return evs

    def _waits(self, eng, evs):
        seen = self.seen[eng]
        need = {}
        for (s, v) in evs:
            if seen.get(id(s), (None, 0))[1] >= v:
                continue
            if id(s) not in need or need[id(s)][1] < v:
                need[id(s)] = (s, v)
        for k, (s, v) in need.items():
            seen[k] = (s, v)
        return list(need.values())

    def _record(self, ev, reads, writes, pwrites=()):
        for b in reads:
            if b is not None:
                _put(b.rs, ev)
        for b in writes:
            if b is not None:
                b.w = {id(ev[0]): ev}
                b.rs = {}
        for b in pwrites:
            if b is not None:
                _put(b.w, ev)

    def op(self, eng, fn, reads=(), writes=(), pwrites=()):
        if self.cnt[eng] >= EPOCH:
            self.cursem[eng] = self.new_sem(eng)
            self.own[eng].add(id(self.cursem[eng]))
            self.cnt[eng] = 0
        evs = self._deps(reads, writes, pwrites, self.cursem[eng])
        if eng in self.skip_self:
            own = self.own[eng]
            evs = [ev_ for ev_ in evs if id(ev_[0]) not in own]
        waits = self._waits(eng, evs)
        self.cnt[eng] += 1
        ev = (self.cursem[eng], self.cnt[eng])
        self.lists[eng].append((waits, fn, ev[0], 1))
        self._record(ev, reads, writes, pwrites)
        return ev

    def dma(self, out_ap, in_ap, reads=(), writes=(), pwrites=(), q="sp", **kw):
        i = self.dnext
        self.dnext = (self.dnext + 1) % NDMA
        s = self.dsem[i]
        evs = self._deps(reads, writes)
        for b in pwrites:
            evs.extend(b.rs.values())
        if self.dcum[i] > 0:
            evs.append((s, self.dcum[i]))
        waits = self._waits(q, evs)
        self.dcum[i] += 16
        ev = (s, self.dcum[i])
        fn = (lambda e, o=out_ap, a=in_ap, k=kw: e.dma_start(out=o, in_=a, **k))
        self.lists[q].append((waits, fn, s, 16))
        self._record(ev, reads, writes, pwrites)
        return ev

    def cc(self, fn, reads=(), writes=()):
        s = self.ccsem
        evs = self._deps(reads, writes)
        if self.cccum > 0:
            evs.append((s, self.cccum))
        waits = self._waits("pool", evs)
        self.cccum += 1
        ev = (s, self.cccum)
        self.lists["pool"].append((waits, fn, s, 1))
        self._record(ev, reads, writes)
        return ev

    def barrier(self):
        evs = []
        for e in ("pe", "act", "dve", "pool"):
            if self.cnt[e] > 0:
                evs.append((self.cursem[e], self.cnt[e]))
        for i in range(NDMA):
            if self.dcum[i] > 0:
                evs.append((self.dsem[i], self.dcum[i]))
        if self.cccum > 0:
            evs.append((self.ccsem, self.cccum))
        for e in self.ENGS:
            waits = self._waits(e, list(evs))
            if waits:
                self.lists[e].append((waits, None, None, 0))

    @contextlib.contextmanager
    def scope(self):
        outer = self.stack
        self.stack = contextlib.ExitStack()
        try:
            yield
        finally:
            self.barrier()
            self.stack.close()
            self.stack = outer

    def wait_all(self, eng, bufs):
        evs = []
        for b in bufs:
            evs.extend(b.w.values())
            evs.extend(b.rs.values())
        waits = self._waits(eng, evs)
        self.lists[eng].append((waits, None, None, 0))

    def emit(self):
        nc = self.nc
        with nc.Block() as block:
            def run(engname):
                def body(e):
                    for waits, fn, sem, inc in self.lists[engname]:
                        if fn is None or not ATTACH_WAIT:
                            for (s, v) in waits:
                                e.wait_ge(s, v)
                            if fn is not None:
                                fn(e).then_inc(sem, inc)
                        else:
                            for (s, v) in waits[1:]:
                                e.wait_ge(s, v)
                            ins = fn(e)
                            if waits:
                                ins._wait_ge(waits[0][0], waits[0][1])
                            ins.then_inc(sem, inc)
                return body
            block.tensor(run("pe"))
            block.scalar(run("act"))
            block.vector(run("dve"))
            block.gpsimd(run("pool"))
            block.sync(run("sp"))

    def close(self):
        self.stack.close()
        self.semstack.close()
import ml_dtypes

D = 2048
DFF = 8192
KC = D // 128
TT = 512
NSUB = TT // 128
EPS = 1e-6


class Ctx:
    pass


def setup_common(P, C, consts):
    C.identb = P.sbuf("identb", [128, 128], BF16)
    C.identf = P.sbuf("identf", [128, 128], F32)
    C.bconst = P.buf("consts")
    P.dma(C.identf[:], consts["identf"], writes=[C.bconst])
    P.op("dve", lambda e: e.tensor_copy(out=C.identb[:], in_=C.identf[:]), reads=[C.bconst], pwrites=[C.bconst])
    C.epsb = P.sbuf("epsb", [128, 4], F32)
    P.op("dve", lambda e: e.memset(C.epsb[:, 0:1], EPS), pwrites=[C.bconst])


def alloc_psum(P, C):
    C.acc = []
    for i in range(6):
        C.acc.append((P.psum("acc%d" % i, [128, 512], F32), P.buf("acc%d" % i)))
    C.ptr = []
    for i in range(2):
        C.ptr.append((P.psum("ptr%d" % i, [128, 1024], BF16), P.buf("ptr%d" % i)))


def alloc_wstream(P, C):
    C.wst = [(P.sbuf("wst%d" % i, [128, 4, 512], F32), P.buf("wst%d" % i)) for i in range(4)]
    C.wb = [(P.sbuf("wb%d" % i, [128, 16, 512], BF16), P.buf("wb%d" % i)) for i in range(2)]
    C.wst_i = 0
    C.wb_i = 0


def load_w_block(P, C, w_ap, nkc=16, ncol=512):
    wb, bwb = C.wb[C.wb_i % 2]
    C.wb_i += 1
    wv = w_ap.rearrange("(kc p) c -> p kc c", p=128)
    first = True
    for q in range(0, nkc, 4):
        n = min(4, nkc - q)
        st, bst = C.wst[C.wst_i % 4]
        C.wst_i += 1
        P.dma(st[:, 0:n, 0:ncol], wv[:, q:q + n, :], writes=[bst])
        eng = "pool" if (C.wst_i % 4) != 0 else "dve"
        kw = dict(writes=[bwb]) if first else dict(pwrites=[bwb])
        P.op(eng, lambda e, st=st, q=q, n=n: e.tensor_copy(out=wb[:, q:q + n, 0:ncol], in_=st[:, 0:n, 0:ncol]),
             reads=[bst], **kw)
        first = False
    return wb, bwb


def alloc_norm(P, C):
    C.xt = [(P.sbuf("xt%d" % i, [128, D], F32), P.buf("xt%d" % i)) for i in range(2)]
    C.junk = (P.sbuf("junk", [128, D], BF16), P.buf("junk"))
    C.hb = (P.sbuf("hb", [128, D], BF16), P.buf("hb"))
    C.ss = [(P.sbuf("ss%d" % i, [128, 2], F32), P.buf("ss%d" % i)) for i in range(2)]
    C.gsb = (P.sbuf("gsb", [128, D], F32), P.buf("gsb"))
    C.hT = (P.sbuf("hT", [128, KC, TT], BF16), P.buf("hT"))
    C.xt_i = 0


def norm_T(P, C, x_tile, bx, xload=None):
    hT, bhT = C.hT
    gsb, bg = C.gsb
    junk, bjunk = C.junk
    hb, bhb = C.hb
    for s in range(NSUB):
        xt, bxt = C.xt[C.xt_i % 2]
        ss, bss = C.ss[C.xt_i % 2]
        C.xt_i += 1
        if xload is None:
            P.dma(xt[:], x_tile[s * 128:(s + 1) * 128, :], reads=[bx], writes=[bxt])
        else:
            xload(s, xt, bxt)
        P.op("act", lambda e, xt=xt, ss=ss: e.activation(out=junk[:], in_=xt[:], func=AF.Square, accum_out=ss[:, 0:1]),
             reads=[bxt], writes=[bjunk, bss])
        P.op("act", lambda e, ss=ss: e.activation(out=ss[:, 1:2], in_=ss[:, 0:1], func=AF.Sqrt, scale=1.0 / D, bias=C.epsb[:, 0:1]),
             reads=[bss, C.bconst], pwrites=[bss])
        P.op("dve", lambda e, ss=ss: e.reciprocal(out=ss[:, 1:2], in_=ss[:, 1:2]), reads=[bss], pwrites=[bss])
        P.op("dve", lambda e, xt=xt, ss=ss: e.scalar_tensor_tensor(out=hb[:], in0=xt[:], scalar=ss[:, 1:2], in1=gsb[:],
                                                                    op0=ALU.mult, op1=ALU.mult),
             reads=[bxt, bss, bg], writes=[bhb])
        for j in range(0, KC, 8):
            pt, bpt = C.ptr[(j // 8) % 2]
            for i in range(8):
                kw = dict(writes=[bpt]) if i == 0 else dict(pwrites=[bpt])
                P.op("pe", lambda e, pt=pt, i=i, j=j: e.transpose(out=pt[:, i * 128:(i + 1) * 128],
                                                                   in_=hb[:, (j + i) * 128:(j + i + 1) * 128],
                                                                   identity=C.identb[:]),
                     reads=[bhb, C.bconst], **kw)
            kw = dict(writes=[bhT]) if (s == 0 and j == 0) else dict(pwrites=[bhT])
            P.op("act", lambda e, pt=pt, j=j, s=s: e.activation(
                out=hT[:, j:j + 8, s * 128:(s + 1) * 128],
                in_=pt[:].rearrange("p (a b) -> p a b", a=8), func=AF.Copy),
                reads=[bpt], **kw)


def ffn_phase(P, C, xres, bxres, w_up, w_down, g2rep, ntiles):
    with P.scope():
        alloc_psum(P, C)
        alloc_norm(P, C)
        alloc_wstream(P, C)
        uT = P.sbuf("uT", [128, DFF // 128, TT], BF16)
        buT = [P.buf("uT%d" % i) for i in range(DFF // 128)]
        tmp = [(P.sbuf("rtmp%d" % i, [128, 512], F32), P.buf("rtmp%d" % i)) for i in range(2)]
        xo = [(P.sbuf("xo%d" % i, [128, 512], F32), P.buf("xo%d" % i)) for i in range(2)]
        P.dma(C.gsb[0][:], g2rep, writes=[C.gsb[1]])
        ti = 0
        oi = 0
        for tt in range(ntiles):
            x_tile = xres[tt * TT:(tt + 1) * TT, :]
            bx = bxres[tt]
            norm_T(P, C, x_tile, bx)
            hT, bhT = C.hT
            for blk in range(DFF // 512):
                wb, bwb = load_w_block(P, C, w_up[:, blk * 512:(blk + 1) * 512])
                for j in range(4):
                    ps, bps = C.acc[j]
                    fc = blk * 4 + j
                    for kc in range(KC):
                        kw = dict(writes=[bps]) if kc == 0 else dict(pwrites=[bps])
                        P.op("pe", lambda e, ps=ps, wb=wb, kc=kc, j=j: e.matmul(
                            ps[:], lhsT=wb[:, kc, j * 128:(j + 1) * 128], rhs=hT[:, kc, :],
                            start=(kc == 0), stop=(kc == KC - 1)), reads=[bwb, bhT], **kw)
                    t, bt = tmp[ti % 2]
                    ti += 1
                    P.op("act", lambda e, t=t, ps=ps: e.activation(out=t[:], in_=ps[:], func=AF.Relu),
                         reads=[bps], writes=[bt])
                    P.op("pool", lambda e, t=t, fc=fc: e.tensor_tensor(out=uT[:, fc, :], in0=t[:], in1=t[:], op=ALU.mult),
                         reads=[bt], writes=[buT[fc]])
            for cb in range(D // 512):
                for fb in range(DFF // 2048):
                    wb, bwb = load_w_block(P, C, w_down[fb * 2048:(fb + 1) * 2048, cb * 512:(cb + 1) * 512])
                    for s in range(NSUB):
                        ps, bps = C.acc[s]
                        for fc in range(16):
                            first = (fb == 0 and fc == 0)
                            last = (fb == DFF // 2048 - 1 and fc == 15)
                            kw = dict(writes=[bps]) if first else dict(pwrites=[bps])
                            P.op("pe", lambda e, ps=ps, wb=wb, fc=fc, fb=fb, s=s, first=first, last=last: e.matmul(
                                ps[:], lhsT=uT[:, fb * 16 + fc, s * 128:(s + 1) * 128], rhs=wb[:, fc, :],
                                start=first, stop=last), reads=[bwb, buT[fb * 16 + fc]], **kw)
                for s in range(NSUB):
                    ps, bps = C.acc[s]
                    o, bo = xo[oi % 2]
                    oi += 1
                    rows = slice(tt * TT + s * 128, tt * TT + (s + 1) * 128)
                    P.dma(o[:], xres[rows, cb * 512:(cb + 1) * 512], reads=[bx], writes=[bo])
                    P.op("dve", lambda e, o=o, ps=ps: e.tensor_tensor(out=o[:], in0=o[:], in1=ps[:], op=ALU.add),
                         reads=[bps, bo], writes=[bo])
                    P.dma(xres[rows, cb * 512:(cb + 1) * 512], o[:], reads=[bo], pwrites=[bx])


def proj_phase(P, C, x, bx, w_in, g1rep, z_out, bz, ntiles, ncols):
    with P.scope():
        alloc_psum(P, C)
        alloc_norm(P, C)
        alloc_wstream(P, C)
        zo = [(P.sbuf("zo%d" % i, [128, 512], F32), P.buf("zo%d" % i)) for i in range(4)]
        P.dma(C.gsb[0][:], g1rep, writes=[C.gsb[1]])
        oi = 0
        for tt in range(ntiles):
            norm_T(P, C, x[tt * TT:(tt + 1) * TT, :], bx)
            hT, bhT = C.hT
            for c0 in range(0, ncols, 512):
                nc_ = min(512, ncols - c0)
                wb, bwb = load_w_block(P, C, w_in[:, c0:c0 + nc_], ncol=nc_)
                for s in range(NSUB):
                    ps, bps = C.acc[s]
                    for kc in range(KC):
                        kw = dict(writes=[bps]) if kc == 0 else dict(pwrites=[bps])
                        P.op("pe", lambda e, ps=ps, wb=wb, kc=kc, s=s, nc_=nc_: e.matmul(
                            ps[:, 0:nc_], lhsT=hT[:, kc, s * 128:(s + 1) * 128], rhs=wb[:, kc, 0:nc_],
                            start=(kc == 0), stop=(kc == KC - 1)), reads=[bwb, bhT], **kw)
                    o, bo = zo[oi % 4]
                    oi += 1
                    if oi % 2:
                        P.op("act", lambda e, o=o, ps=ps, nc_=nc_: e.activation(out=o[:, 0:nc_], in_=ps[:, 0:nc_], func=AF.Copy),
                             reads=[bps], writes=[bo])
                    else:
                        P.op("dve", lambda e, o=o, ps=ps, nc_=nc_: e.tensor_copy(out=o[:, 0:nc_], in_=ps[:, 0:nc_]),
                             reads=[bps], writes=[bo])
                    rows = slice(tt * TT + s * 128, tt * TT + (s + 1) * 128)
                    P.dma(z_out[rows, c0:c0 + nc_], o[:, 0:nc_], reads=[bo], pwrites=[bz])


def merge_phase(P, C, xres, bxres, yretT, yrwT, gaT, gbT, w_a, w_b, w_out, ntiles):
    with P.scope():
        alloc_psum(P, C)
        alloc_wstream(P, C)
        yst = [(P.sbuf("yst%d" % i, [128, 8, TT], F32), P.buf("yst%d" % i)) for i in range(2)]
        yb = [(P.sbuf("yb%d" % i, [128, 8, TT], BF16), P.buf("yb%d" % i)) for i in range(2)]
        gt = [(P.sbuf("gt%d" % i, [128, TT], F32), P.buf("gt%d" % i)) for i in range(4)]
        t12 = [(P.sbuf("t12_%d" % i, [128, TT], F32), P.buf("t12_%d" % i)) for i in range(4)]
        mT = P.sbuf("mT", [128, KC, TT], BF16)
        bmT = [P.buf("mT%d" % i) for i in range(KC)]
        xo = [(P.sbuf("xo%d" % i, [128, 512], F32), P.buf("xo%d" % i)) for i in range(2)]
        gi = 0
        oi = 0
        for tt in range(ntiles):
            tok = slice(tt * TT, (tt + 1) * TT)
            for i, src in enumerate((yretT, yrwT)):
                st, bst = yst[i]
                P.dma(st[:], src.rearrange("(kc p) t -> p kc t", p=128)[:, :, tok], writes=[bst])
                eng = "pool" if i == 0 else "dve"
                P.op(eng, lambda e, st=st, i=i: e.tensor_copy(out=yb[i][0][:], in_=st[:]), reads=[bst], writes=[yb[i][1]])
            for db in range(D // 512):
                wa, bwa = load_w_block(P, C, w_a[:, db * 512:(db + 1) * 512], nkc=8)
                wbb, bwbb = load_w_block(P, C, w_b[:, db * 512:(db + 1) * 512], nkc=8)
                for j in range(4):
                    dc = db * 4 + j
                    res = []
                    for i, (w_, bw_, gT) in enumerate(((wa, bwa, gaT), (wbb, bwbb, gbT))):
                        ps, bps = C.acc[(2 * j + i) % 6]
                        for kc in range(8):
                            kw = dict(writes=[bps]) if kc == 0 else dict(pwrites=[bps])
                            P.op("pe", lambda e, ps=ps, w_=w_, kc=kc, j=j, i=i: e.matmul(
                                ps[:], lhsT=w_[:, kc, j * 128:(j + 1) * 128], rhs=yb[i][0][:, kc, :],
                                start=(kc == 0), stop=(kc == 7)), reads=[bw_, yb[i][1]], **kw)
                        g, bg = gt[gi % 4]
                        t, bt = t12[gi % 4]
                        gi += 1
                        P.dma(g[:], gT[dc * 128:(dc + 1) * 128, tok], writes=[bg])
                        P.op("act", lambda e, g=g: e.activation(out=g[:], in_=g[:], func=AF.Sigmoid), reads=[bg], writes=[bg])
                        P.op("dve", lambda e, t=t, g=g, ps=ps: e.tensor_tensor(out=t[:], in0=g[:], in1=ps[:], op=ALU.mult),
                             reads=[bg, bps], writes=[bt])
                        res.append((t, bt))
                    P.op("pool", lambda e, dc=dc, a=res[0][0], b=res[1][0]: e.tensor_tensor(out=mT[:, dc, :], in0=a[:], in1=b[:], op=ALU.add),
                         reads=[res[0][1], res[1][1]], writes=[bmT[dc]])
            bx = bxres[tt]
            for cb in range(D // 512):
                wb, bwb = load_w_block(P, C, w_out[:, cb * 512:(cb + 1) * 512])
                for s in range(NSUB):
                    ps, bps = C.acc[s]
                    for kc in range(KC):
                        kw = dict(writes=[bps]) if kc == 0 else dict(pwrites=[bps])
                        P.op("pe", lambda e, ps=ps, wb=wb, kc=kc, s=s: e.matmul(
                            ps[:], lhsT=mT[:, kc, s * 128:(s + 1) * 128], rhs=wb[:, kc, :],
                            start=(kc == 0), stop=(kc == KC - 1)), reads=[bwb, bmT[kc]], **kw)
                    o, bo = xo[oi % 2]
                    oi += 1
                    rows = slice(tt * TT + s * 128, tt * TT + (s + 1) * 128)
                    P.dma(o[:], xres[rows, cb * 512:(cb + 1) * 512], reads=[bx], writes=[bo])
                    P.op("dve", lambda e, o=o, ps=ps: e.tensor_tensor(out=o[:], in0=o[:], in1=ps[:], op=ALU.add),
                         reads=[bps, bo], writes=[bo])
                    P.dma(xres[rows, cb * 512:(cb + 1) * 512], o[:], reads=[bo], pwrites=[bx])


def final_norm_phase(P, C, xres, bxres, grep, out, bout, ntiles):
    with P.scope():
        alloc_psum(P, C)
        alloc_norm(P, C)
        gsb, bg = C.gsb
        P.dma(gsb[:], grep, writes=[bg])
        junk, bjunk = C.junk
        ob = [(P.sbuf("fo%d" % i, [128, D], F32), P.buf("fo%d" % i)) for i in range(2)]
        for r in range(ntiles * NSUB):
            xt, bxt = C.xt[r % 2]
            ss, bss = C.ss[r % 2]
            o, bo = ob[r % 2]
            rows = slice(r * 128, (r + 1) * 128)
            P.dma(xt[:], xres[rows, :], reads=[bxres[r // NSUB]], writes=[bxt])
            P.op("act", lambda e, xt=xt, ss=ss: e.activation(out=junk[:], in_=xt[:], func=AF.Square, accum_out=ss[:, 0:1]),
                 reads=[bxt], writes=[bjunk, bss])
            P.op("act", lambda e, ss=ss: e.activation(out=ss[:, 1:2], in_=ss[:, 0:1], func=AF.Sqrt, scale=1.0 / D, bias=C.epsb[:, 0:1]),
                 reads=[bss, C.bconst], pwrites=[bss])
            P.op("dve", lambda e, ss=ss: e.reciprocal(out=ss[:, 1:2], in_=ss[:, 1:2]), reads=[bss], pwrites=[bss])
            P.op("dve", lambda e, xt=xt, ss=ss, o=o: e.scalar_tensor_tensor(out=o[:], in0=xt[:], scalar=ss[:, 1:2], in1=gsb[:],
                                                                             op0=ALU.mult, op1=ALU.mult),
                 reads=[bxt, bss, bg], writes=[bo])
            P.dma(out[rows, :], o[:], reads=[bo], pwrites=[bout])


def build_C(N, final):
    nc = bass.Bass("TRN2", target_bir_lowering=False)
    dt = lambda n, s, k="ExternalInput": nc.dram_tensor(n, s, F32, kind=k).ap()
    x = dt("x", [N, D])
    yretT = dt("yretT", [1024, N]); yrwT = dt("yrwT", [1024, N])
    gaT = dt("gaT", [D, N]); gbT = dt("gbT", [D, N])
    w_a = dt("w_a", [1024, D]); w_b = dt("w_b", [1024, D]); w_out = dt("w_out", [D, D])
    w_up = dt("w_up", [D, DFF]); w_down = dt("w_down", [DFF, D])
    g2 = dt("g2", [128, D]); gf = dt("gf", [128, D]); identf = dt("identf", [128, 128])
    y = dt("y", [N, D], "ExternalOutput")
    xres = nc.dram_tensor("xres", [N, D], F32).ap()
    P = Prog(nc); C = Ctx()
    setup_common(P, C, {"identf": identf})
    nt = N // TT
    bxres = [P.buf() for _ in range(nt)]
    for tt in range(nt):
        P.dma(xres[tt * TT:(tt + 1) * TT, :], x[tt * TT:(tt + 1) * TT, :], writes=[bxres[tt]])
    merge_phase(P, C, xres, bxres, yretT, yrwT, gaT, gbT, w_a, w_b, w_out, nt)
    ffn_phase(P, C, xres, bxres, w_up, w_down, g2, nt)
    bout = P.buf()
    if final:
        final_norm_phase(P, C, xres, bxres, gf, y, bout, nt)
    else:
        for tt in range(nt):
            P.dma(y[tt * TT:(tt + 1) * TT, :], xres[tt * TT:(tt + 1) * TT, :], reads=[bxres[tt]], pwrites=[bout])
    P.wait_all("sp", [bout])
    P.emit(); P.close()
    return nc


def build_A(N, ncols=11520):
    nc = bass.Bass("TRN2", target_bir_lowering=False)
    dt = lambda n, s, k="ExternalInput": nc.dram_tensor(n, s, F32, kind=k).ap()
    x = dt("x", [N, D]); w = dt("w_in", [D, ncols]); g = dt("g1", [128, D]); identf = dt("identf", [128, 128])
    z = dt("z", [N, ncols], "ExternalOutput")
    P = Prog(nc); C = Ctx()
    setup_common(P, C, {"identf": identf})
    bz = P.buf()
    proj_phase(P, C, x, None, w, g, z, bz, N // TT, ncols)
    P.wait_all("sp", [bz])
    P.emit(); P.close()
    return nc


TWO_PI = 6.283185307179586
LN_EPS = 64e-5
GN_EPS = 1e-5


def scan_phase(P, S, A):
    with P.scope():
        ptm = A.ptm
        mu_l = A.mu_l
        mu_vT = A.mu_vT
        wa_up = A.wa_up
        g_up = A.g_up
        sel = A.sel
        identf = A.identf
        invf = A.invf
        gam = A.gam
        gn = A.gn
        selr = A.selr
        y_rw = A.y_rw
        y_ret = A.y_ret
        pos = A.pos
        sb = P.sbuf
        bc = P.buf("const")
        c_ptm = sb("ptm", [128, 10, 256], F32); c_mul = sb("mul", [128, 2], F32); c_muv = sb("muv", [64, 4], F32)
        c_wa = sb("waup", [128, 256], F32); c_gu = sb("gup", [128, 256], F32)
        c_sel = sb("sel", [128, 128, 64], F32); c_id = sb("id", [128, 128], F32)
        c_invf = sb("invf", [128, 64], F32); c_gam = sb("gam", [128, 2, 128], F32); c_gn = sb("gn", [128, 2, 256], F32)
        c_selr = sb("selr", [128, 128, 128], F32); c_kq = sb("kq", [128, 4], F32)
        c_omka = sb("omka", [128, 256], F32); c_bias = sb("cbias", [128, 4], F32)
        for t_, a_ in ((c_ptm, ptm), (c_mul, mu_l), (c_muv, mu_vT), (c_wa, wa_up), (c_gu, g_up), (c_sel, sel), (c_id, identf),
                       (c_invf, invf), (c_gam, gam), (c_gn, gn), (c_selr, selr), (c_kq, A.kq)):
            P.dma(t_[:], a_, pwrites=[bc])
        P.op("dve", lambda e: e.tensor_scalar(out=c_omka[:], in0=c_ptm[:, 6, :], scalar1=-1.0, scalar2=1.0, op0=ALU.mult, op1=ALU.add),
             reads=[bc], pwrites=[bc])
        P.op("dve", lambda e: e.memset(c_bias[:, 0:1], -3.141592653589793), pwrites=[bc])
        P.op("dve", lambda e: e.memset(c_bias[:, 1:2], LN_EPS), pwrites=[bc])
        P.op("dve", lambda e: e.memset(c_bias[:, 2:3], GN_EPS), pwrites=[bc])

        Srw = sb("Srw", [64, 4, 64], F32); bS = P.buf("Srw")
        Rr = sb("Rr", [128, 2, 128], F32); bR = P.buf("Rr")
        bSg = [P.buf("Sg0"), P.buf("Sg1")]
        bTS = [[P.buf(), P.buf()] for _ in range(2)]; bSA = [[P.buf(), P.buf()] for _ in range(2)]; bTP = [[P.buf(), P.buf()] for _ in range(2)]
        P.op("dve", lambda e: e.memset(Srw[:], 0.0), writes=[bS, bSg[0], bSg[1]])
        bRg = [P.buf("Rg0"), P.buf("Rg1")]
        bT1 = [[P.buf(), P.buf()] for _ in range(2)]; bT2 = [[P.buf(), P.buf()] for _ in range(2)]
        P.op("pool", lambda e: e.memset(Rr[:], 0.0), writes=[bR, bRg[0], bRg[1]])

        psRow = [[(P.psum("prow%d_%d" % (i, j), [64, 512], F32), P.buf()) for j in range(3)] for i in range(2)]
        psMisc = (P.psum("pmisc", [128, 512], F32), P.buf("pmisc"))
        psRet = (P.psum("pret", [128, 512], F32), P.buf("pret"))

        def T(name, shape, n=1, dtype=F32):
            return [(sb(name + str(i), shape, dtype), P.buf(name + str(i))) for i in range(n)]
        zt = T("zt", [128, 3, 256], 2); zp = T("zp", [128, 3, 256], 2)
        vT = T("vT", [64, 4, 128], 2); vTp = T("vTp", [64, 4, 128], 2)
        lT = T("lT", [128, 2, 128], 2); lTp = T("lTp", [128, 2, 128], 2)
        rows = T("rows", [128, 5, 256], 2)
        yT = T("yT", [64, 4, 128], 2)
        w1 = T("w1", [128, 256], 6)
        sm = T("sm", [128, 16], 4)
        vtm = T("vtm", [128, 256], 2)
        gtm = T("gtm", [128, 256], 2)
        bon = T("bon", [128, 4], 2)
        tmpS = T("tmpS", [64, 4, 64], 2); saS = T("saS", [64, 4], 2)
        tmpP = T("tmpP", [64, 4, 64], 2)
        orw = T("orw", [128, 256], 2)
        st6 = T("st6", [128, 4, 6], 2); mv = T("mv", [128, 4, 2], 2)
        rqt = T("rqt", [128, 256], 2); rkt = T("rkt", [128, 256], 2); rgt = T("rgt", [128, 256], 2)
        rvt = T("rvt", [128, 2, 128], 2); post = T("post", [128, 2], 2, I32); posf = T("posf", [128, 2], 2)
        cs = T("cs", [128, 2, 64], 2)
        rrow = T("rrow", [128, 2, 256], 2)
        rrs = T("rrs", [128, 512], 2)
        ryT = T("ryT", [128, 2, 128], 2)
        tmpR = T("tmpR", [128, 2, 128], 2); tmpR2 = T("tmpR2", [128, 2, 128], 2)
        oret = T("oret", [128, 256], 2)
        w2 = T("w2", [128, 256], 4)
        kit = T("kit", [128, 64], 2, I32)

        NCH = S // 128
        for c in range(NCH):
            i2 = c % 2
            tok = slice(c * 128, (c + 1) * 128)
            z, bz = zt[i2]; zpp, bzp = zp[i2]
            P.dma(z[:], A.ztm(c), writes=[bz]); P.dma(zpp[:], A.ztm_p(c), writes=[bzp])
            v_, bv_ = vT[i2]; vp_, bvp_ = vTp[i2]
            P.dma(v_[:], A.zvT(c), writes=[bv_]); P.dma(vp_[:], A.zvT_p(c), writes=[bvp_])
            l_, bl_ = lT[i2]; lp_, blp_ = lTp[i2]
            P.dma(l_[:], A.zlT(c), writes=[bl_]); P.dma(lp_[:], A.zlT_p(c), writes=[blp_])
            P.op("dve", lambda e, z=z, zpp=zpp: e.tensor_tensor(out=zpp[:], in0=zpp[:], in1=z[:], op=ALU.subtract), reads=[bz, bzp], writes=[bzp])
            P.op("dve", lambda e, z=z, zpp=zpp: e.tensor_tensor(out=zpp[:], in0=zpp[:], in1=c_ptm[:, 0:3, :], op=ALU.mult), reads=[bzp, bc], writes=[bzp])
            P.op("dve", lambda e, z=z, zpp=zpp: e.tensor_tensor(out=z[:], in0=z[:], in1=zpp[:], op=ALU.add), reads=[bz, bzp], writes=[bz])
            P.op("pool", lambda e, v_=v_, vp_=vp_: e.tensor_tensor(out=vp_[:], in0=vp_[:], in1=v_[:], op=ALU.subtract), reads=[bv_, bvp_], writes=[bvp_])
            P.op("pool", lambda e, v_=v_, vp_=vp_: e.tensor_tensor(out=vp_[:], in0=vp_[:], in1=c_muv[:].unsqueeze(2).to_broadcast([64, 4, 128]), op=ALU.mult), reads=[bvp_, bc], writes=[bvp_])
            P.op("pool", lambda e, v_=v_, vp_=vp_: e.tensor_tensor(out=v_[:], in0=v_[:], in1=vp_[:], op=ALU.add), reads=[bv_, bvp_], writes=[bv_])
            P.op("pool", lambda e, l_=l_, lp_=lp_: e.tensor_tensor(out=lp_[:], in0=lp_[:], in1=l_[:], op=ALU.subtract), reads=[bl_, blp_], writes=[blp_])
            P.op("pool", lambda e, l_=l_, lp_=lp_: e.tensor_tensor(out=lp_[:], in0=lp_[:], in1=c_mul[:].unsqueeze(2).to_broadcast([128, 2, 128]), op=ALU.mult), reads=[blp_, bc], writes=[blp_])
            P.op("pool", lambda e, l_=l_, lp_=lp_: e.tensor_tensor(out=l_[:], in0=l_[:], in1=lp_[:], op=ALU.add), reads=[bl_, blp_], writes=[bl_])
            P.op("act", lambda e, l_=l_: e.activation(out=l_[0:64, 0, :], in_=l_[0:64, 0, :], func=AF.Tanh), reads=[bl_], writes=[bl_])
            P.op("act", lambda e, l_=l_: e.activation(out=l_[:, 1, :], in_=l_[:, 1, :], func=AF.Sigmoid), reads=[bl_], writes=[bl_])
            pm, bpm = psMisc
            rw_, brw_ = rows[i2]
            P.op("pe", lambda e, l_=l_: e.matmul(pm[:, 0:256], lhsT=l_[0:64, 0, :], rhs=c_wa[0:64, :], start=True, stop=True), reads=[bl_, bc], writes=[bpm])
            a1, ba1 = w1[0]
            P.op("dve", lambda e: e.tensor_tensor(out=a1[:], in0=pm[:, 0:256], in1=c_ptm[:, 3, :], op=ALU.add), reads=[bpm, bc], writes=[ba1])
            P.op("act", lambda e: e.activation(out=a1[:], in_=a1[:], func=AF.Sigmoid), reads=[ba1], writes=[ba1])
            P.op("act", lambda e, rw_=rw_: e.activation(out=rw_[:, 0, :], in_=a1[:], func=AF.Exp, scale=-0.6065306597126334), reads=[ba1], writes=[brw_])
            P.op("pe", lambda e, l_=l_: e.matmul(pm[:, 0:256], lhsT=l_[64:128, 0, :], rhs=c_wa[64:128, :], start=True, stop=True), reads=[bl_, bc, ba1], writes=[bpm])
            al, bal = w1[1]
            P.op("dve", lambda e: e.tensor_tensor(out=al[:], in0=pm[:, 0:256], in1=c_ptm[:, 4, :], op=ALU.add), reads=[bpm, bc], writes=[bal])
            P.op("act", lambda e: e.activation(out=al[:], in_=al[:], func=AF.Sigmoid), reads=[bal], writes=[bal])
            g_, bg_ = gtm[i2]
            P.op("pe", lambda e, l_=l_: e.matmul(pm[:, 0:256], lhsT=l_[:, 1, :], rhs=c_gu[:], start=True, stop=True), reads=[bl_, bc, bal], writes=[bpm])
            P.op("act", lambda e, g_=g_: e.activation(out=g_[:], in_=pm[:, 0:256], func=AF.Copy), reads=[bpm], writes=[bg_])
            kk, bkk = w1[2]; k2, bk2 = w1[3]
            s_, bs_ = sm[i2]
            P.op("dve", lambda e, z=z: e.tensor_tensor(out=kk[:], in0=z[:, 1, :], in1=c_ptm[:, 5, :], op=ALU.mult), reads=[bz, bc], writes=[bkk])
            P.op("dve", lambda e: e.tensor_tensor(out=k2[:], in0=kk[:], in1=kk[:], op=ALU.mult), reads=[bkk], writes=[bk2])
            P.op("dve", lambda e, s_=s_: e.tensor_reduce(out=s_[:, 0:4], in_=k2[:].rearrange("p (h k) -> p h k", h=4), axis=AX.X, op=ALU.add), reads=[bk2], writes=[bs_])
            P.op("act", lambda e, s_=s_: e.activation(out=s_[:, 0:4], in_=s_[:, 0:4], func=AF.Sqrt), reads=[bs_], writes=[bs_])
            P.op("dve", lambda e, s_=s_: e.tensor_scalar(out=s_[:, 0:4], in0=s_[:, 0:4], scalar1=1e-12, scalar2=None, op0=ALU.max), reads=[bs_], writes=[bs_])
            P.op("dve", lambda e, s_=s_: e.reciprocal(out=s_[:, 0:4], in_=s_[:, 0:4]), reads=[bs_], writes=[bs_])
            P.op("dve", lambda e, s_=s_: e.tensor_tensor(out=kk[:].rearrange("p (h k) -> p h k", h=4), in0=kk[:].rearrange("p (h k) -> p h k", h=4),
                                                         in1=s_[:, 0:4].unsqueeze(2).to_broadcast([128, 4, 64]), op=ALU.mult), reads=[bkk, bs_], writes=[bkk])
            P.op("dve", lambda e, rw_=rw_: e.tensor_scalar(out=rw_[:, 1, :], in0=kk[:], scalar1=-1.0, scalar2=None, op0=ALU.mult), reads=[bkk], pwrites=[brw_])
            P.op("dve", lambda e, rw_=rw_: e.tensor_tensor(out=rw_[:, 2, :], in0=kk[:], in1=al[:], op=ALU.mult), reads=[bkk, bal], pwrites=[brw_])
            P.op("dve", lambda e: e.tensor_tensor(out=k2[:], in0=al[:], in1=c_ptm[:, 6, :], op=ALU.mult), reads=[bal, bc], writes=[bk2])
            P.op("dve", lambda e: e.tensor_tensor(out=k2[:], in0=k2[:], in1=c_omka[:], op=ALU.add), reads=[bk2, bc], writes=[bk2])
            P.op("dve", lambda e, rw_=rw_, z=z: e.tensor_tensor(out=rw_[:, 3, :], in0=z[:, 1, :], in1=k2[:], op=ALU.mult), reads=[bz, bk2], pwrites=[brw_])
            P.op("dve", lambda e, rw_=rw_, z=z: e.tensor_copy(out=rw_[:, 4, :], in_=z[:, 0, :]), reads=[bz], pwrites=[brw_])
            bn_, bbn_ = bon[i2]
            P.op("dve", lambda e, z=z: e.tensor_tensor(out=k2[:], in0=z[:, 0, :], in1=c_ptm[:, 7, :], op=ALU.mult), reads=[bz, bc], writes=[bk2])
            P.op("dve", lambda e, rw_=rw_: e.tensor_tensor(out=k2[:], in0=k2[:], in1=rw_[:, 3, :], op=ALU.mult), reads=[bk2, brw_], writes=[bk2])
            P.op("dve", lambda e, bn_=bn_: e.tensor_reduce(out=bn_[:], in_=k2[:].rearrange("p (h k) -> p h k", h=4), axis=AX.X, op=ALU.add), reads=[bk2], writes=[bbn_])
            y_, by_ = yT[i2]
            for t in range(128):
                pr = psRow[t % 2]
                for j, (c0, n) in enumerate(((0, 512), (512, 512), (1024, 256))):
                    P.op("pe", lambda e, pr=pr, j=j, c0=c0, n=n, rw_=rw_, t=t: e.matmul(
                        pr[j][0][:, 0:n], lhsT=c_sel[:, t, :], rhs=rw_[:].rearrange("p a b -> p (a b)")[:, c0:c0 + n], start=True, stop=True),
                        reads=[brw_, bc], writes=[pr[j][1]])
                row = lambda r: (pr[(r * 256) // 512][0][:, (r * 256) % 512:(r * 256) % 512 + 256].rearrange("p (h k) -> p h k", h=4), pr[(r * 256) // 512][1])
                wr, bwr = row(0); ar, bar = row(1); br_, bbr = row(2); kr, bkr = row(3); rr, brr = row(4)
                tS, _ = tmpS[t % 2]; sa, _ = saS[t % 2]; tP, _ = tmpP[t % 2]
                G2 = (slice(0, 2), slice(2, 4))
                bts = bTS[t % 2]; bsa2 = bSA[t % 2]; btp = bTP[t % 2]
                def both(fn):
                    for g in range(2):
                        fn(g, G2[g])
                both(lambda g, hs: P.op("dve", lambda e, hs=hs, tS=tS, sa=sa, tP=tP, ar=ar, wr=wr, br_=br_, kr=kr, rr=rr, v_=v_, y_=y_: e.tensor_tensor(out=tS[:, hs, :], in0=Srw[:, hs, :], in1=ar[:, hs, :], op=ALU.mult),
                                        reads=[bSg[g], bar], writes=[bts[g]]))
                both(lambda g, hs: P.op("dve", lambda e, hs=hs, tS=tS, sa=sa, tP=tP, ar=ar, wr=wr, br_=br_, kr=kr, rr=rr, v_=v_, y_=y_: e.tensor_reduce(out=sa[:, hs], in_=tS[:, hs, :], axis=AX.X, op=ALU.add),
                                        reads=[bts[g]], writes=[bsa2[g]]))
                both(lambda g, hs: P.op("dve", lambda e, hs=hs, tS=tS, sa=sa, tP=tP, ar=ar, wr=wr, br_=br_, kr=kr, rr=rr, v_=v_, y_=y_: e.tensor_tensor(out=Srw[:, hs, :], in0=Srw[:, hs, :], in1=wr[:, hs, :], op=ALU.mult),
                                        reads=[bSg[g], bwr], writes=[bSg[g]]))
                both(lambda g, hs: P.op("dve", lambda e, hs=hs, tS=tS, sa=sa, tP=tP, ar=ar, wr=wr, br_=br_, kr=kr, rr=rr, v_=v_, y_=y_: e.tensor_tensor(out=tS[:, hs, :], in0=br_[:, hs, :], in1=sa[:, hs].unsqueeze(2).to_broadcast([64, 2, 64]), op=ALU.mult),
                                        reads=[bbr, bsa2[g]], writes=[bts[g]]))
                both(lambda g, hs: P.op("dve", lambda e, hs=hs, tS=tS, sa=sa, tP=tP, ar=ar, wr=wr, br_=br_, kr=kr, rr=rr, v_=v_, y_=y_: e.tensor_tensor(out=Srw[:, hs, :], in0=Srw[:, hs, :], in1=tS[:, hs, :], op=ALU.add),
                                        reads=[bSg[g], bts[g]], writes=[bSg[g]]))
                both(lambda g, hs: P.op("dve", lambda e, hs=hs, t=t, tS=tS, sa=sa, tP=tP, ar=ar, wr=wr, br_=br_, kr=kr, rr=rr, v_=v_, y_=y_: e.tensor_tensor(out=tP[:, hs, :], in0=kr[:, hs, :], in1=v_[:, hs, t:t + 1].to_broadcast([64, 2, 64]), op=ALU.mult),
                                        reads=[bkr, bv_], writes=[btp[g]]))
                both(lambda g, hs: P.op("dve", lambda e, hs=hs, tS=tS, sa=sa, tP=tP, ar=ar, wr=wr, br_=br_, kr=kr, rr=rr, v_=v_, y_=y_: e.tensor_tensor(out=Srw[:, hs, :], in0=Srw[:, hs, :], in1=tP[:, hs, :], op=ALU.add),
                                        reads=[bSg[g], btp[g]], writes=[bSg[g]]))
                both(lambda g, hs: P.op("dve", lambda e, hs=hs, tS=tS, sa=sa, tP=tP, ar=ar, wr=wr, br_=br_, kr=kr, rr=rr, v_=v_, y_=y_: e.tensor_tensor(out=tS[:, hs, :], in0=Srw[:, hs, :], in1=rr[:, hs, :], op=ALU.mult),
                                        reads=[bSg[g], brr], writes=[bts[g]]))
                both(lambda g, hs: P.op("dve", lambda e, hs=hs, t=t, tS=tS, sa=sa, tP=tP, ar=ar, wr=wr, br_=br_, kr=kr, rr=rr, v_=v_, y_=y_: e.tensor_reduce(out=y_[:, hs, t], in_=tS[:, hs, :], axis=AX.X, op=ALU.add),
                                        reads=[bts[g]], **(dict(writes=[by_]) if (t == 0 and g == 0) else dict(pwrites=[by_]))))
            vt_, bvt_ = vtm[i2]
            o_, bo_ = orw[i2]
            for h in range(4):
                P.op("pe", lambda e, h=h, y_=y_: e.transpose(out=pm[:, h * 64:(h + 1) * 64], in_=y_[:, h, :], identity=c_id[0:64, 0:64]),
                     reads=[by_, bc] + ([bg_] if h == 0 else []), **(dict(writes=[bpm]) if h == 0 else dict(pwrites=[bpm])))
            s6, bs6 = st6[i2]; m_, bm_ = mv[i2]
            for h in range(4):
                P.op("dve", lambda e, h=h, s6=s6: e.bn_stats(out=s6[:, h, :], in_=pm[:, h * 64:(h + 1) * 64]), reads=[bpm], **(dict(writes=[bs6]) if h == 0 else dict(pwrites=[bs6])))
            for h in range(4):
                P.op("dve", lambda e, h=h, s6=s6, m_=m_: e.bn_aggr(out=m_[:, h, :], in_=s6[:, h, :]), reads=[bs6], **(dict(writes=[bm_]) if h == 0 else dict(pwrites=[bm_])))
            P.op("act", lambda e, m_=m_: e.activation(out=m_[:, :, 1], in_=m_[:, :, 1], func=AF.Sqrt, bias=c_bias[:, 1:2]), reads=[bm_, bc], writes=[bm_])
            P.op("dve", lambda e, m_=m_: e.reciprocal(out=m_[:, :, 1], in_=m_[:, :, 1]), reads=[bm_], writes=[bm_])
            for h in range(4):
                P.op("dve", lambda e, h=h, m_=m_, o_=o_: e.tensor_scalar(out=o_[:, h * 64:(h + 1) * 64], in0=pm[:, h * 64:(h + 1) * 64],
                                                                         scalar1=m_[:, h, 0:1], scalar2=m_[:, h, 1:2], op0=ALU.subtract, op1=ALU.mult),
                     reads=[bpm, bm_], **(dict(writes=[bo_]) if h == 0 else dict(pwrites=[bo_])))
            P.op("dve", lambda e, o_=o_: e.tensor_tensor(out=o_[:], in0=o_[:], in1=c_ptm[:, 8, :], op=ALU.mult), reads=[bo_, bc], writes=[bo_])
            P.op("dve", lambda e, o_=o_: e.tensor_tensor(out=o_[:], in0=o_[:], in1=c_ptm[:, 9, :], op=ALU.add), reads=[bo_, bc], writes=[bo_])
            P.op("dve", lambda e, z=z, bn_=bn_: e.tensor_tensor(out=k2[:].rearrange("p (h k) -> p h k", h=4), in0=z[:, 2, :].rearrange("p (h k) -> p h k", h=4),
                                                                in1=bn_[:].unsqueeze(2).to_broadcast([128, 4, 64]), op=ALU.mult), reads=[bz, bbn_], writes=[bk2])
            P.op("dve", lambda e, o_=o_: e.tensor_tensor(out=o_[:], in0=o_[:], in1=k2[:], op=ALU.add), reads=[bo_, bk2], writes=[bo_])
            P.op("dve", lambda e, o_=o_, g_=g_: e.tensor_tensor(out=o_[:], in0=o_[:], in1=g_[:], op=ALU.mult), reads=[bo_, bg_], writes=[bo_])
            P.dma(y_rw[tok, :], o_[:], reads=[bo_])

            q_, bq_ = rqt[i2]; k_, bk_ = rkt[i2]; gr_, bgr_ = rgt[i2]; rv_, brv_ = rvt[i2]
            pt_, bpt_ = post[i2]; pf_, bpf_ = posf[i2]; cs_, bcs_ = cs[i2]; rr_, brr_ = rrow[i2]
            P.dma(q_[:], A.rq(c), writes=[bq_]); P.dma(k_[:], A.rk(c), writes=[bk_]); P.dma(gr_[:], A.rgr(c), writes=[bgr_])
            P.dma(rv_[:], A.rvT(c), writes=[brv_]); P.dma(pt_[:, 0:1], pos[tok, :], writes=[bpt_])
            P.op("pool", lambda e, pt_=pt_, pf_=pf_: e.tensor_copy(out=pf_[:, 0:1], in_=pt_[:, 0:1]), reads=[bpt_], writes=[bpf_])
            ang, bang = w2[0]; ang2, bang2 = w2[1]
            P.op("dve", lambda e, pf_=pf_: e.tensor_scalar(out=ang[:, 0:64], in0=c_invf[:], scalar1=pf_[:, 0:1], scalar2=None, op0=ALU.mult), reads=[bpf_, bc], writes=[bang])
            P.op("dve", lambda e: e.tensor_scalar(out=ang2[:, 0:64], in0=ang[:, 0:64], scalar1=1.5707963267948966, scalar2=None, op0=ALU.add), reads=[bang], writes=[bang2])
            ki_, bki_ = kit[i2]
            for (a_, ba_, col) in ((ang2, bang2, 0), (ang, bang, 1)):
                P.op("dve", lambda e, a_=a_: e.tensor_scalar(out=a_[:, 64:128], in0=a_[:, 0:64], scalar1=1.0 / TWO_PI, scalar2=None, op0=ALU.mult), reads=[ba_], writes=[ba_])
                P.op("dve", lambda e, a_=a_, ki_=ki_: e.tensor_copy(out=ki_[:], in_=a_[:, 64:128]), reads=[ba_], writes=[bki_])
                P.op("dve", lambda e, a_=a_, ki_=ki_: e.tensor_copy(out=a_[:, 64:128], in_=ki_[:]), reads=[bki_], writes=[ba_])
                P.op("dve", lambda e, a_=a_: e.scalar_tensor_tensor(out=a_[:, 0:64], in0=a_[:, 64:128], scalar=-TWO_PI, in1=a_[:, 0:64], op0=ALU.mult, op1=ALU.add), reads=[ba_], writes=[ba_])
                P.op("dve", lambda e, a_=a_: e.tensor_scalar(out=a_[:, 64:128], in0=a_[:, 0:64], scalar1=3.141592653589793, scalar2=-TWO_PI, op0=ALU.is_gt, op1=ALU.mult), reads=[ba_], writes=[ba_])
                P.op("dve", lambda e, a_=a_: e.tensor_tensor(out=a_[:, 0:64], in0=a_[:, 0:64], in1=a_[:, 64:128], op=ALU.add), reads=[ba_], writes=[ba_])
                kw = dict(writes=[bcs_]) if col == 0 else dict(pwrites=[bcs_])
                P.op("act", lambda e, cs_=cs_, a_=a_, col=col: e.activation(out=cs_[:, col, :], in_=a_[:, 0:64], func=AF.Sin), reads=[ba_], **kw)
            ta, bta = w2[2]; tb, btb = w2[3]
            for xi, (x_, bx_) in enumerate(((k_, bk_), (q_, bq_))):
                xv = x_[:].rearrange("p (h two d) -> p h two d", h=2, two=2)
                ov = rr_[:, xi, :].rearrange("p (h two d) -> p h two d", h=2, two=2)
                tav = ta[:, 0:128].rearrange("p (h d) -> p h d", h=2); tbv = tb[:, 0:128].rearrange("p (h d) -> p h d", h=2)
                ncb = lambda cs_=cs_: cs_[:, 0:1, :].to_broadcast([128, 2, 64])
                nsb = lambda cs_=cs_: cs_[:, 1:2, :].to_broadcast([128, 2, 64])
                kw0 = dict(writes=[brr_]) if xi == 0 else dict(pwrites=[brr_])
                P.op("pool", lambda e, xv=xv, tav=tav, ncb=ncb: e.tensor_tensor(out=tav, in0=xv[:, :, 0, :], in1=ncb(), op=ALU.mult), reads=[bx_, bcs_], writes=[bta])
                P.op("pool", lambda e, xv=xv, tbv=tbv, nsb=nsb: e.tensor_tensor(out=tbv, in0=xv[:, :, 1, :], in1=nsb(), op=ALU.mult), reads=[bx_, bcs_], writes=[btb])
                P.op("pool", lambda e, ov=ov, tav=tav, tbv=tbv: e.tensor_tensor(out=ov[:, :, 0, :], in0=tav, in1=tbv, op=ALU.subtract), reads=[bta, btb], **kw0)
                P.op("pool", lambda e, xv=xv, tav=tav, ncb=ncb: e.tensor_tensor(out=tav, in0=xv[:, :, 1, :], in1=ncb(), op=ALU.mult), reads=[bx_, bcs_], writes=[bta])
                P.op("pool", lambda e, xv=xv, tbv=tbv, nsb=nsb: e.tensor_tensor(out=tbv, in0=xv[:, :, 0, :], in1=nsb(), op=ALU.mult), reads=[bx_, bcs_], writes=[btb])
                P.op("pool", lambda e, ov=ov, tav=tav, tbv=tbv: e.tensor_tensor(out=ov[:, :, 1, :], in0=tav, in1=tbv, op=ALU.add), reads=[bta, btb], pwrites=[brr_])
            for xi in range(2):
                for h in range(2):
                    P.op("pool", lambda e, rr_=rr_, xi=xi, h=h: e.tensor_scalar(out=rr_[:, xi, h * 128:(h + 1) * 128], in0=rr_[:, xi, h * 128:(h + 1) * 128],
                                                                                scalar1=c_kq[:, 2 * xi + h:2 * xi + h + 1], scalar2=None, op0=ALU.mult),
                         reads=[brr_, bc], pwrites=[brr_])
            pr_, bpr_ = psRet
            ry_, bry_ = ryT[i2]
            for t in range(128):
                P.op("pe", lambda e, rr_=rr_, t=t: e.matmul(pr_[:], lhsT=c_selr[:, t, :], rhs=rr_[:].rearrange("p a b -> p (a b)"), start=True, stop=True),
                     reads=[brr_, bc], writes=[bpr_])
                rs_, brs_ = rrs[t % 2]
                P.op("act", lambda e, rs_=rs_: e.activation(out=rs_[:], in_=pr_[:], func=AF.Copy), reads=[bpr_], writes=[brs_])
                t1, bt1 = tmpR[t % 2]; t2, bt2 = tmpR2[t % 2]
                krow = rs_[:, 0:256].rearrange("p (h d) -> p h d", h=2); qrow = rs_[:, 256:512].rearrange("p (h d) -> p h d", h=2)
                for h in range(2):
                    P.op("pool", lambda e, t1=t1, krow=krow, rv_=rv_, t=t, h=h: e.tensor_tensor(out=t1[:, h, :], in0=krow[:, h, :], in1=rv_[:, h, t:t + 1].to_broadcast([128, 128]), op=ALU.mult),
                         reads=[brs_, brv_], writes=[bT1[t % 2][h]])
                for h in range(2):
                    P.op("pool", lambda e, t1=t1, h=h: e.tensor_tensor(out=Rr[:, h, :], in0=Rr[:, h, :], in1=t1[:, h, :], op=ALU.add), reads=[bRg[h], bT1[t % 2][h]], writes=[bRg[h]])
                for h in range(2):
                    P.op("pool", lambda e, t2=t2, qrow=qrow, h=h: e.tensor_tensor(out=t2[:, h, :], in0=Rr[:, h, :], in1=qrow[:, h, :], op=ALU.mult), reads=[bRg[h], brs_], writes=[bT2[t % 2][h]])
                kw = dict(writes=[bry_]) if t == 0 else dict(pwrites=[bry_])
                P.op("dve", lambda e, t2=t2, ry_=ry_, t=t: e.tensor_reduce(out=ry_[:, :, t], in_=t2[:], axis=AX.X, op=ALU.add), reads=[bT2[t % 2][0], bT2[t % 2][1]], **kw)
            for h in range(2):
                P.op("pool", lambda e, h=h: e.tensor_tensor(out=Rr[:, h, :], in0=Rr[:, h, :], in1=c_gam[:, h, :], op=ALU.mult), reads=[bRg[h], bc], writes=[bRg[h]])
            for h in range(2):
                P.op("pe", lambda e, h=h, ry_=ry_: e.transpose(out=pm[:, h * 128:(h + 1) * 128], in_=ry_[:, h, :], identity=c_id[:]),
                     reads=[bry_, bc], **(dict(writes=[bpm]) if h == 0 else dict(pwrites=[bpm])))
            s6, bs6 = st6[i2]; m_, bm_ = mv[i2]
            orr, borr = oret[i2]
            for h in range(2):
                P.op("dve", lambda e, h=h, s6=s6: e.bn_stats(out=s6[:, h, :], in_=pm[:, h * 128:(h + 1) * 128]), reads=[bpm], **(dict(writes=[bs6]) if h == 0 else dict(pwrites=[bs6])))
            for h in range(2):
                P.op("dve", lambda e, h=h, s6=s6, m_=m_: e.bn_aggr(out=m_[:, h, :], in_=s6[:, h, :]), reads=[bs6], **(dict(writes=[bm_]) if h == 0 else dict(pwrites=[bm_])))
            P.op("act", lambda e, m_=m_: e.activation(out=m_[:, 0:2, 1], in_=m_[:, 0:2, 1], func=AF.Sqrt, bias=c_bias[:, 2:3]), reads=[bm_, bc], writes=[bm_])
            P.op("dve", lambda e, m_=m_: e.reciprocal(out=m_[:, 0:2, 1], in_=m_[:, 0:2, 1]), reads=[bm_], writes=[bm_])
            for h in range(2):
                P.op("dve", lambda e, h=h, m_=m_, orr=orr: e.tensor_scalar(out=orr[:, h * 128:(h + 1) * 128], in0=pm[:, h * 128:(h + 1) * 128],
                                                                           scalar1=m_[:, h, 0:1], scalar2=m_[:, h, 1:2], op0=ALU.subtract, op1=ALU.mult),
                     reads=[bpm, bm_], **(dict(writes=[borr]) if h == 0 else dict(pwrites=[borr])))
            P.op("dve", lambda e, orr=orr: e.tensor_tensor(out=orr[:], in0=orr[:], in1=c_gn[:, 0, :], op=ALU.mult), reads=[borr, bc], writes=[borr])
            P.op("dve", lambda e, orr=orr: e.tensor_tensor(out=orr[:], in0=orr[:], in1=c_gn[:, 1, :], op=ALU.add), reads=[borr, bc], writes=[borr])
            P.op("act", lambda e, gr_=gr_: e.activation(out=gr_[:], in_=gr_[:], func=AF.Silu), reads=[bgr_], writes=[bgr_])
            P.op("dve", lambda e, orr=orr, gr_=gr_: e.tensor_tensor(out=orr[:], in0=orr[:], in1=gr_[:], op=ALU.mult), reads=[borr, bgr_], writes=[borr])
            P.dma(y_ret[tok, :], orr[:], reads=[borr])


def build_B(S):
    nc = bass.Bass("TRN2", target_bir_lowering=False)
    dt = lambda n, s, k="ExternalInput", d=F32: nc.dram_tensor(n, s, d, kind=k).ap()
    ztm = dt("ztm", [S, 3, 256]); ztm_p = dt("ztm_p", [S, 3, 256])
    zvT = dt("zvT", [64, 4, S]); zvT_p = dt("zvT_p", [64, 4, S])
    zlT = dt("zlT", [128, 2, S]); zlT_p = dt("zlT_p", [128, 2, S])
    ptm = dt("ptm", [128, 10, 256]); mu_l = dt("mu_l", [128, 2]); mu_vT = dt("mu_vT", [64, 4])
    wa_up = dt("wa_up", [128, 256]); g_up = dt("g_up", [128, 256])
    sel = dt("sel", [128, 128, 64]); identf = dt("identf", [128, 128])
    rq = dt("rq", [S, 256]); rk = dt("rk", [S, 256]); rvT = dt("rvT", [128, 2, S]); rgr = dt("rgr", [S, 256])
    pos = dt("pos", [S, 1], d=I32); invf = dt("invf", [128, 64]); gam = dt("gam", [128, 2, 128])
    gn = dt("gn", [128, 2, 256]); selr = dt("selr", [128, 128, 128]); kq = dt("kq", [128, 4])
    y_rw = dt("y_rw", [S, 256], "ExternalOutput"); y_ret = dt("y_ret", [S, 256], "ExternalOutput")

    P = Prog(nc)
    class A: pass
    A.ptm, A.mu_l, A.mu_vT, A.wa_up, A.g_up, A.sel, A.identf = ptm, mu_l, mu_vT, wa_up, g_up, sel, identf
    A.invf, A.gam, A.gn, A.selr, A.y_rw, A.y_ret, A.pos = invf, gam, gn, selr, y_rw, y_ret, pos
    A.kq = kq
    ck = lambda c: slice(c * 128, (c + 1) * 128)
    A.ztm = lambda c: ztm[ck(c)]; A.ztm_p = lambda c: ztm_p[ck(c)]
    A.zvT = lambda c: zvT[:, :, ck(c)]; A.zvT_p = lambda c: zvT_p[:, :, ck(c)]
    A.zlT = lambda c: zlT[:, :, ck(c)]; A.zlT_p = lambda c: zlT_p[:, :, ck(c)]
    A.rq = lambda c: rq[ck(c), :]; A.rk = lambda c: rk[ck(c), :]; A.rgr = lambda c: rgr[ck(c), :]
    A.rvT = lambda c: rvT[:, :, ck(c)]
    scan_phase(P, S, A)
    P.barrier()
    P.emit(); P.close()
    return nc


def shift_prev(a, axis=0):
    out = np.zeros_like(a)
    sl_dst = [slice(None)] * a.ndim; sl_src = [slice(None)] * a.ndim
    sl_dst[axis] = slice(1, None); sl_src[axis] = slice(0, -1)
    out[tuple(sl_dst)] = a[tuple(sl_src)]
    return out


def rep128(v):
    return np.ascontiguousarray(np.broadcast_to(np.asarray(v, np.float32).reshape(1, -1), (128, v.size)))


def prep_B_rwkv(zr, zk, zv, zw, za, zg, p):
    S = zr.shape[0]
    d = {}
    ztm = np.stack([zr, zk, zv], 1).astype(np.float32)
    d["ztm"] = ztm; d["ztm_p"] = shift_prev(ztm, 0)
    zvT = np.ascontiguousarray(zv.reshape(S, 4, 64).transpose(2, 1, 0))
    d["zvT"] = zvT; d["zvT_p"] = shift_prev(zvT, 2)
    zl = np.stack([np.concatenate([zw, za], 1).T, zg.T], 1)
    zl = np.ascontiguousarray(zl.astype(np.float32))
    d["zlT"] = zl; d["zlT_p"] = shift_prev(zl, 2)
    ptm = np.stack([rep128(p[k]) for k in ("mu_r", "mu_k", "mu_v", "w0", "a0", "k_k", "k_a", "r_k", "ln_g", "ln_b")], 1)
    d["ptm"] = np.ascontiguousarray(ptm)
    d["mu_l"] = np.ascontiguousarray(np.stack([np.concatenate([p["mu_w"], p["mu_a"]]), p["mu_g"]], 1).astype(np.float32))
    d["mu_vT"] = np.ascontiguousarray(p["mu_v"].reshape(4, 64).T.astype(np.float32))
    d["wa_up"] = np.ascontiguousarray(np.concatenate([p["w_up"], p["a_up"]], 0).astype(np.float32))
    d["g_up"] = np.ascontiguousarray(p["g_up"].astype(np.float32))
    sel = np.zeros((128, 128, 64), np.float32)
    sel[np.arange(128), np.arange(128), :] = 1.0
    d["sel"] = sel
    d["identf"] = np.eye(128, dtype=np.float32)
    return d


def ret_consts(heads):
    gamma = 1.0 - 2.0 ** (-5.0 - np.asarray(heads, np.float64))
    t1 = np.arange(1, 129, dtype=np.float64)[:, None]
    kq = np.concatenate([gamma[None, :] ** (-t1) * 128.0 ** -0.5, gamma[None, :] ** t1], 1).astype(np.float32)
    gam = np.ascontiguousarray(np.broadcast_to((gamma ** 128).astype(np.float32).reshape(1, 2, 1), (128, 2, 128)))
    return gam, np.ascontiguousarray(kq)


def prep_B_ret(zq, zk, zv, zgr, pos, gn_g, gn_b, heads):
    S = zq.shape[0]
    d = {}
    d["rq"] = np.ascontiguousarray(zq, np.float32); d["rk"] = np.ascontiguousarray(zk, np.float32)
    d["rgr"] = np.ascontiguousarray(zgr, np.float32)
    d["rvT"] = np.ascontiguousarray(zv.reshape(S, 2, 128).transpose(2, 1, 0).astype(np.float32))
    d["pos"] = np.ascontiguousarray(pos.reshape(S, 1).astype(np.int32))
    inv = (10000.0 ** (-np.arange(64, dtype=np.float32) / np.float32(64))).astype(np.float32)
    d["invf"] = rep128(inv)
    d["gam"], d["kq"] = ret_consts(heads)
    d["gn"] = np.ascontiguousarray(np.stack([rep128(gn_g), rep128(gn_b)], 1))
    selr = np.zeros((128, 128, 128), np.float32)
    selr[np.arange(128), np.arange(128), :] = 1.0
    d["selr"] = selr
    return d


RG4 = [[0, 1, 2, 3], [4, 5, 6, 7]]
NTM = 1536
NFM = 768


def load_w_resident(P, C, w_ap, dst, bdst, ncols, nkc=16):
    wv = w_ap.rearrange("(kc p) c -> p kc c", p=128)
    for c0 in range(0, ncols, 512):
        n_c = min(512, ncols - c0)
        for q in range(0, nkc, 4):
            n = min(4, nkc - q)
            st, bst = C.wst[C.wst_i % 4]
            C.wst_i += 1
            P.dma(st[:, 0:n, 0:n_c], wv[:, q:q + n, c0:c0 + n_c], writes=[bst])
            eng = "pool" if (C.wst_i % 2) else "dve"
            P.op(eng, lambda e, st=st, q=q, n=n, c0=c0, n_c=n_c: e.tensor_copy(out=dst[:, q:q + n, c0:c0 + n_c], in_=st[:, 0:n, 0:n_c]),
                 reads=[bst], pwrites=[bdst])


def scanproj_phase(P, C, xb, w_tm, w_fm, g1rep, ZT, ZF, S):
    with P.scope():
        alloc_psum(P, C)
        alloc_norm(P, C)
        C.wst = [(P.sbuf("wst%d" % i, [128, 4, 512], F32), P.buf("wst%d" % i)) for i in range(4)]
        C.wst_i = 0
        Wr = P.sbuf("Wres", [128, KC, NTM + NFM], BF16)
        bWr = P.buf("Wres")
        load_w_resident(P, C, w_tm, Wr[:, :, 0:NTM], bWr, NTM)
        load_w_resident(P, C, w_fm, Wr[:, :, NTM:NTM + NFM], bWr, NFM)
        zo = [(P.sbuf("zo%d" % i, [128, 512], F32), P.buf("zo%d" % i)) for i in range(4)]
        P.dma(C.gsb[0][:], g1rep, writes=[C.gsb[1]])
        oi = 0
        ai = 0
        for tt in range(S // TT):
            def xload(s_, xt, bxt, tt=tt):
                t0 = tt * TT + s_ * 128
                q, i0 = t0 // (S // 4), t0 % (S // 4)
                r0 = i0 // 64
                P.dma(xt[0:64, :], xb[r0, q * 64:(q + 1) * 64, :], writes=[bxt])
                P.dma(xt[64:128, :], xb[r0 + 1, q * 64:(q + 1) * 64, :], pwrites=[bxt])
            norm_T(P, C, None, None, xload=xload)
            hT, bhT = C.hT
            for cb in range(NTM // 512):
                for s in range(NSUB):
                    ps, bps = C.acc[ai % 6]
                    ai += 1
                    for kc in range(KC):
                        kw = dict(writes=[bps]) if kc == 0 else dict(pwrites=[bps])
                        P.op("pe", lambda e, ps=ps, kc=kc, s=s, cb=cb: e.matmul(
                            ps[:], lhsT=hT[:, kc, s * 128:(s + 1) * 128], rhs=Wr[:, kc, cb * 512:(cb + 1) * 512],
                            start=(kc == 0), stop=(kc == KC - 1)), reads=[bWr, bhT], **kw)
                    o, bo = zo[oi % 4]
                    oi += 1
                    eng = "act" if oi % 2 else "dve"
                    if eng == "act":
                        P.op("act", lambda e, o=o, ps=ps: e.activation(out=o[:], in_=ps[:], func=AF.Copy), reads=[bps], writes=[bo])
                    else:
                        P.op("dve", lambda e, o=o, ps=ps: e.tensor_copy(out=o[:], in_=ps[:]), reads=[bps], writes=[bo])
                    r0 = 1 + tt * TT + s * 128
                    P.dma(ZT[r0:r0 + 128, cb * 512:(cb + 1) * 512], o[:], reads=[bo])
            for r in range(NFM // 128):
                ps, bps = C.acc[ai % 6]
                ai += 1
                for kc in range(KC):
                    kw = dict(writes=[bps]) if kc == 0 else dict(pwrites=[bps])
                    P.op("pe", lambda e, ps=ps, kc=kc, r=r: e.matmul(
                        ps[:], lhsT=Wr[:, kc, NTM + r * 128:NTM + (r + 1) * 128], rhs=hT[:, kc, :],
                        start=(kc == 0), stop=(kc == KC - 1)), reads=[bWr, bhT], **kw)
                o, bo = zo[oi % 4]
                oi += 1
                if oi % 2:
                    P.op("act", lambda e, o=o, ps=ps: e.activation(out=o[:], in_=ps[:], func=AF.Copy), reads=[bps], writes=[bo])
                else:
                    P.op("dve", lambda e, o=o, ps=ps: e.tensor_copy(out=o[:], in_=ps[:]), reads=[bps], writes=[bo])
                P.dma(ZF[r * 128:(r + 1) * 128, 1 + tt * TT:1 + (tt + 1) * TT], o[:], reads=[bo])


def partial_phase(P, C, Y, w_ab, PA, PB, S):
    with P.scope():
        alloc_psum(P, C)
        C.wst = [(P.sbuf("wst%d" % i, [128, 4, 512], F32), P.buf("wst%d" % i)) for i in range(4)]
        C.wst_i = 0
        wab = P.sbuf("wab", [128, 4, D], BF16)
        bwab = P.buf("wab")
        load_w_resident(P, C, w_ab.rearrange("a r c -> (a r) c"), wab, bwab, D, nkc=4)
        yt = [(P.sbuf("yt%d" % i, [128, 512], F32), P.buf("yt%d" % i)) for i in range(2)]
        ybf = [(P.sbuf("ybf%d" % i, [128, 512], BF16), P.buf("ybf%d" % i)) for i in range(2)]
        yT = [(P.sbuf("yTp%d" % i, [128, 4, 128], BF16), P.buf("yTp%d" % i)) for i in range(2)]
        ot = [(P.sbuf("pot%d" % i, [128, D], F32), P.buf("pot%d" % i)) for i in range(2)]
        oi = 0
        ai = 0
        for r in range(S // 128):
            rows = slice(r * 128, (r + 1) * 128)
            y_, by_ = yt[r % 2]; yb_, byb_ = ybf[r % 2]; yT_, byT_ = yT[r % 2]
            P.dma(y_[:], Y[rows, :], writes=[by_])
            P.op("pool", lambda e, y_=y_, yb_=yb_: e.tensor_copy(out=yb_[:], in_=y_[:]), reads=[by_], writes=[byb_])
            pt, bpt = C.ptr[r % 2]
            for i in range(4):
                kw = dict(writes=[bpt]) if i == 0 else dict(pwrites=[bpt])
                P.op("pe", lambda e, pt=pt, i=i, yb_=yb_: e.transpose(out=pt[:, i * 128:(i + 1) * 128], in_=yb_[:, i * 128:(i + 1) * 128],
                                                                      identity=C.identb[:]), reads=[byb_, C.bconst], **kw)
            P.op("act", lambda e, pt=pt, yT_=yT_: e.activation(out=yT_[:], in_=pt[:, 0:512].rearrange("p (a b) -> p a b", a=4), func=AF.Copy),
                 reads=[bpt], writes=[byT_])
            for br, dst in ((0, PA), (1, PB)):
                o, bo = ot[oi % 2]
                oi += 1
                for cb in range(4):
                    ps, bps = C.acc[ai % 6]
                    ai += 1
                    for kc in range(2):
                        kw = dict(writes=[bps]) if kc == 0 else dict(pwrites=[bps])
                        P.op("pe", lambda e, ps=ps, kc=kc, br=br, cb=cb, yT_=yT_: e.matmul(
                            ps[:], lhsT=yT_[:, 2 * br + kc, :], rhs=wab[:, 2 * br + kc, cb * 512:(cb + 1) * 512],
                            start=(kc == 0), stop=(kc == 1)), reads=[bwab, byT_], **kw)
                    kw = dict(writes=[bo]) if cb == 0 else dict(pwrites=[bo])
                    if cb % 2:
                        P.op("act", lambda e, o=o, ps=ps, cb=cb: e.activation(out=o[:, cb * 512:(cb + 1) * 512], in_=ps[:], func=AF.Copy), reads=[bps], **kw)
                    else:
                        P.op("dve", lambda e, o=o, ps=ps, cb=cb: e.tensor_copy(out=o[:, cb * 512:(cb + 1) * 512], in_=ps[:]), reads=[bps], **kw)
                P.dma(dst[0][rows, :], o[:, 0:D // 2], reads=[bo])
                P.dma(dst[1][rows, :], o[:, D // 2:D], reads=[bo])


def merge2_phase(P, C, xres, bxres, AO, BO, G, w_out, ntiles):
    with P.scope():
        alloc_psum(P, C)
        alloc_wstream(P, C)
        tl = [[(P.sbuf("mg%d_%d" % (i, j), [128, D], F32), P.buf()) for j in range(4)] for i in range(2)]
        mb = (P.sbuf("mb", [128, D], BF16), P.buf("mb"))
        mT = P.sbuf("mT", [128, KC, TT], BF16)
        bmT = P.buf("mT")
        xo = [(P.sbuf("xo%d" % i, [128, 512], F32), P.buf("xo%d" % i)) for i in range(2)]
        oi = 0
        si = 0
        for tt in range(ntiles):
            for s in range(NSUB):
                rows = slice(tt * TT + s * 128, tt * TT + (s + 1) * 128)
                (a_, ba_), (b_, bb_), (ga_, bga_), (gb_, bgb_) = tl[si % 2]
                si += 1
                P.dma(a_[:, 0:D // 2], AO[0][rows, :], writes=[ba_]); P.dma(a_[:, D // 2:D], AO[1][rows, :], pwrites=[ba_])
                P.dma(b_[:, 0:D // 2], BO[0][rows, :], writes=[bb_]); P.dma(b_[:, D // 2:D], BO[1][rows, :], pwrites=[bb_])
                P.dma(ga_[:], G[rows, 0:D], writes=[bga_]); P.dma(gb_[:], G[rows, D:2 * D], writes=[bgb_])
                P.op("act", lambda e, ga_=ga_: e.activation(out=ga_[:], in_=ga_[:], func=AF.Sigmoid), reads=[bga_], writes=[bga_])
                P.op("act", lambda e, gb_=gb_: e.activation(out=gb_[:], in_=gb_[:], func=AF.Sigmoid), reads=[bgb_], writes=[bgb_])
                P.op("dve", lambda e, a_=a_, ga_=ga_: e.tensor_tensor(out=a_[:], in0=a_[:], in1=ga_[:], op=ALU.mult), reads=[ba_, bga_], writes=[ba_])
                P.op("pool", lambda e, b_=b_, gb_=gb_: e.tensor_tensor(out=b_[:], in0=b_[:], in1=gb_[:], op=ALU.mult), reads=[bb_, bgb_], writes=[bb_])
                m_, bm_ = mb
                P.op("dve", lambda e, a_=a_, b_=b_: e.tensor_tensor(out=m_[:], in0=a_[:], in1=b_[:], op=ALU.add), reads=[ba_, bb_], writes=[bm_])
                for j in range(0, KC, 8):
                    pt, bpt = C.ptr[(j // 8) % 2]
                    for i in range(8):
                        kw = dict(writes=[bpt]) if i == 0 else dict(pwrites=[bpt])
                        P.op("pe", lambda e, pt=pt, i=i, j=j: e.transpose(out=pt[:, i * 128:(i + 1) * 128], in_=m_[:, (j + i) * 128:(j + i + 1) * 128],
                                                                           identity=C.identb[:]), reads=[bm_, C.bconst], **kw)
                    kw = dict(writes=[bmT]) if (s == 0 and j == 0) else dict(pwrites=[bmT])
                    P.op("act", lambda e, pt=pt, j=j, s=s: e.activation(out=mT[:, j:j + 8, s * 128:(s + 1) * 128],
                                                                         in_=pt[:].rearrange("p (a b) -> p a b", a=8), func=AF.Copy), reads=[bpt], **kw)
            bx = bxres[tt]
            for cb in range(D // 512):
                wb, bwb = load_w_block(P, C, w_out[:, cb * 512:(cb + 1) * 512])
                for s in range(NSUB):
                    ps, bps = C.acc[s]
                    for kc in range(KC):
                        kw = dict(writes=[bps]) if kc == 0 else dict(pwrites=[bps])
                        P.op("pe", lambda e, ps=ps, wb=wb, kc=kc, s=s: e.matmul(
                            ps[:], lhsT=mT[:, kc, s * 128:(s + 1) * 128], rhs=wb[:, kc, :],
                            start=(kc == 0), stop=(kc == KC - 1)), reads=[bwb, bmT], **kw)
                    o, bo = xo[oi % 2]
                    oi += 1
                    rows = slice(tt * TT + s * 128, tt * TT + (s + 1) * 128)
                    P.dma(o[:], xres[rows, cb * 512:(cb + 1) * 512], reads=[bx], writes=[bo])
                    P.op("dve", lambda e, o=o, ps=ps: e.tensor_tensor(out=o[:], in0=o[:], in1=ps[:], op=ALU.add), reads=[bps, bo], writes=[bo])
                    P.dma(xres[rows, cb * 512:(cb + 1) * 512], o[:], reads=[bo], pwrites=[bx])


def build_fused(S, L, stop=None):
    N = S // 4
    nc = bass.Bass("TRN2", target_bir_lowering=False)
    dt = lambda n, s, k="ExternalInput", d=F32: nc.dram_tensor(n, s, d, kind=k).ap()
    x = dt("x", [N, D]); pos = dt("pos", [S, 1], d=I32)
    w_tm = dt("w_tm", [L, D, NTM]); w_fm = dt("w_fm", [L, D, NFM]); w_gate = dt("w_gate", [L, D, 2 * D])
    w_ab = dt("w_ab", [L, 2, 256, D]); w_out = dt("w_out", [L, D, D]); w_up = dt("w_up", [L, D, DFF]); w_down = dt("w_down", [L, DFF, D])
    g1 = dt("g1", [L, 128, D]); g2 = dt("g2", [L, 128, D]); gf = dt("gf", [128, D])
    ptm = dt("ptm", [L, 128, 10, 256]); mu_l = dt("mu_l", [L, 128, 2]); mu_vT = dt("mu_vT", [L, 64, 4])
    wa_up = dt("wa_up", [L, 128, 256]); g_up = dt("g_up", [L, 128, 256]); gn = dt("gn", [L, 128, 2, 256])
    sel = dt("sel", [128, 128, 64]); selr = dt("selr", [128, 128, 128]); identf = dt("identf", [128, 128])
    invf = dt("invf", [128, 64]); gam = dt("gam", [128, 2, 128]); kq = dt("kq", [128, 4])
    y = dt("y", [N, D], "ExternalOutput")
    it = lambda n, s: nc.dram_tensor(n, s, F32).ap()
    xres = it("xres", [N, D]); XB = it("XB", [N // 64, 256, D]); ZT = it("ZT", [1 + S, NTM]); ZF = it("ZF", [NFM, 1 + S])
    G = it("G", [N, 2 * D]); Y = it("Y", [S, 512]); PA = [it("PA%d" % i, [S, D // 2]) for i in range(2)]; PB = [it("PB%d" % i, [S, D // 2]) for i in range(2)]
    AO = [it("AO%d" % i, [N, D // 2]) for i in range(2)]; BO = [it("BO%d" % i, [N, D // 2]) for i in range(2)]

    P = Prog(nc); C = Ctx()
    setup_common(P, C, {"identf": identf})
    nt = N // TT
    bxres = [P.buf() for _ in range(nt)]
    for tt in range(nt):
        P.dma(xres[tt * TT:(tt + 1) * TT, :], x[tt * TT:(tt + 1) * TT, :], writes=[bxres[tt]])
    with P.scope():
        zt_ = P.sbuf("zeros", [128, NTM], F32); bz_ = P.buf()
        P.op("dve", lambda e: e.memset(zt_[:], 0.0), writes=[bz_])
        P.dma(ZT[0:1, :], zt_[0:1, :], reads=[bz_])
        for r in range(NFM // 128):
            P.dma(ZF[r * 128:(r + 1) * 128, 0:1], zt_[:, 0:1], reads=[bz_], allow_slow_non_contiguous=True)
    ck1 = lambda c: slice(1 + c * 128, 1 + (c + 1) * 128)
    ck0 = lambda c: slice(c * 128, (c + 1) * 128)
    for l in range(L):
        P.barrier()
        for r_ in range(N // 64):
            P.cc(lambda e, r_=r_: e.collective_compute("AllGather", ALU.bypass, replica_groups=RG4,
                                                       ins=[xres[r_ * 64:(r_ + 1) * 64, :].opt()], outs=[XB[r_].opt()]))
        P.barrier()
        if stop == "ag":
            break
        scanproj_phase(P, C, XB, w_tm[l], w_fm[l], g1[l], ZT, ZF, S)
        if stop == "scanproj":
            break
        bG = P.buf()
        proj_phase(P, C, xres, None, w_gate[l], g1[l], G, bG, nt, 2 * D)
        if stop == "gates":
            break

        class A:
            pass
        A.ptm, A.mu_l, A.mu_vT, A.wa_up, A.g_up, A.gn = ptm[l], mu_l[l], mu_vT[l], wa_up[l], g_up[l], gn[l]
        A.sel, A.selr, A.identf, A.invf, A.gam, A.pos = sel, selr, identf, invf, gam, pos
        A.kq = kq
        A.y_ret, A.y_rw = Y[:, 0:256], Y[:, 256:512]
        A.rq = lambda c: ZT[ck1(c), 0:256]; A.rk = lambda c: ZT[ck1(c), 256:512]; A.rgr = lambda c: ZT[ck1(c), 512:768]
        A.ztm = lambda c: ZT[ck1(c), 768:1536].rearrange("t (a n) -> t a n", a=3)
        A.ztm_p = lambda c: ZT[ck0(c), 768:1536].rearrange("t (a n) -> t a n", a=3)
        A.rvT = lambda c: ZF[0:256, ck1(c)].rearrange("(h e) t -> e h t", h=2)
        A.zvT = lambda c: ZF[256:512, ck1(c)].rearrange("(h d) t -> d h t", h=4)
        A.zvT_p = lambda c: ZF[256:512, ck0(c)].rearrange("(h d) t -> d h t", h=4)
        A.zlT = lambda c: ZF[512:768, ck1(c)].rearrange("(a p) t -> p a t", a=2)
        A.zlT_p = lambda c: ZF[512:768, ck0(c)].rearrange("(a p) t -> p a t", a=2)
        scan_phase(P, S, A)
        if stop == "scan":
            break
        partial_phase(P, C, Y, w_ab[l], PA, PB, S)
        if stop == "partial":
            break
        P.barrier()
        for src_, dst_ in ((PA[0], AO[0]), (PA[1], AO[1]), (PB[0], BO[0]), (PB[1], BO[1])):
            P.cc(lambda e, src_=src_, dst_=dst_: e.collective_compute("ReduceScatter", ALU.add, replica_groups=RG4, ins=[src_.opt()], outs=[dst_.opt()]))
        P.barrier()
        if stop == "rs":
            break
        merge2_phase(P, C, xres, bxres, AO, BO, G, w_out[l], nt)
        if stop == "merge":
            break
        ffn_phase(P, C, xres, bxres, w_up[l], w_down[l], g2[l], nt)
    bout = P.buf()
    final_norm_phase(P, C, xres, bxres, gf, y, bout, nt)
    P.barrier()
    P.emit(); P.close()
    return nc


def host_inputs_fused(inp, S, L):
    N = S // 4
    f32 = lambda a: np.ascontiguousarray(np.asarray(a), dtype=np.float32)
    x = np.asarray(inp["x"]); positions = np.asarray(inp["positions"]).astype(np.int32)
    w_in = np.asarray(inp["w_in"])
    common = dict(
        w_gate=f32(w_in[:L, :, 7424:11520]), w_out=f32(inp["w_out"][:L]), w_up=f32(inp["mlp_up"][:L]), w_down=f32(inp["mlp_down"][:L]),
        g1=np.stack([rep128(np.asarray(inp["norm1_g"][l], np.float32)) for l in range(L)]),
        g2=np.stack([rep128(np.asarray(inp["norm2_g"][l], np.float32)) for l in range(L)]),
        gf=rep128(np.asarray(inp["final_g"], np.float32)), identf=np.eye(128, dtype=np.float32))
    sel = np.zeros((128, 128, 64), np.float32); sel[np.arange(128), np.arange(128), :] = 1.0
    selr = np.zeros((128, 128, 128), np.float32); selr[np.arange(128), np.arange(128), :] = 1.0
    inv = (10000.0 ** (-np.arange(64, dtype=np.float32) / np.float32(64))).astype(np.float32)
    common.update(sel=sel, selr=selr, invf=rep128(inv))
    mu_all = np.asarray(inp["rwkv_mu"], np.float32)
    per_hg = {}
    for hg in range(4):
        cs = slice(hg * 256, (hg + 1) * 256)
        R0 = 4096
        col = lambda base: np.arange(base + hg * 256, base + (hg + 1) * 256)
        tm_cols = np.concatenate([col(0), col(1024), col(3072), col(R0), col(R0 + 1024), col(R0 + 2048)])
        fm_cols = np.concatenate([col(2048), col(R0 + 2048), np.arange(R0 + 3072, R0 + 3328)])
        d = dict(w_tm=f32(w_in[:L][:, :, tm_cols]), w_fm=f32(w_in[:L][:, :, fm_cols]),
                 w_ab=f32(np.stack([np.asarray(inp["w_branch_a"])[:L, cs, :], np.asarray(inp["w_branch_b"])[:L, cs, :]], 1)))
        pv = lambda name, l: np.asarray(inp[name][l], np.float32)
        ptm, mul, muv, waup, gup, gn = [], [], [], [], [], []
        for l in range(L):
            mu = mu_all[l]
            p = dict(mu_r=mu[0:1024][cs], mu_k=mu[1024:2048][cs], mu_v=mu[2048:3072][cs], mu_w=mu[3072:3136], mu_a=mu[3136:3200], mu_g=mu[3200:3328],
                     w0=pv("rwkv_w0", l)[cs], a0=pv("rwkv_a0", l)[cs], k_k=pv("rwkv_k_k", l)[cs], k_a=pv("rwkv_k_a", l)[cs], r_k=pv("rwkv_r_k", l)[cs],
                     ln_g=pv("rwkv_ln_g", l)[cs], ln_b=pv("rwkv_ln_b", l)[cs])
            ptm.append(np.stack([rep128(p[k]) for k in ("mu_r", "mu_k", "mu_v", "w0", "a0", "k_k", "k_a", "r_k", "ln_g", "ln_b")], 1))
            mul.append(np.stack([np.concatenate([p["mu_w"], p["mu_a"]]), p["mu_g"]], 1))
            muv.append(p["mu_v"].reshape(4, 64).T)
            waup.append(np.concatenate([pv("rwkv_w_up", l)[:, cs], pv("rwkv_a_up", l)[:, cs]], 0))
            gup.append(pv("rwkv_g_up", l)[:, cs])
            gn.append(np.stack([rep128(pv("ret_gn_g", l)[cs]), rep128(pv("ret_gn_b", l)[cs])], 1))
        d.update(ptm=f32(np.stack(ptm)), mu_l=f32(np.stack(mul)), mu_vT=f32(np.stack(muv)), wa_up=f32(np.stack(waup)), g_up=f32(np.stack(gup)), gn=f32(np.stack(gn)))
        d["gam"], d["kq"] = ret_consts([2 * hg, 2 * hg + 1])
        per_hg[hg] = d
    ins = []
    for c in range(8):
        b, j = c // 4, c % 4
        d = dict(common); d.update(per_hg[j])
        d["x"] = f32(x[b, j * N:(j + 1) * N]); d["pos"] = np.ascontiguousarray(positions[b, :S].reshape(S, 1))
        ins.append(d)
    return ins


from concourse.bass_utils import run_bass_kernel_spmd

_PROG = {}


def kernel(**inp):
    S, L = 8192, 4
    if "nc" not in _PROG:
        _PROG["nc"] = build_fused(S, L)
    ins = host_inputs_fused(inp, S, L)
    res = run_bass_kernel_spmd(_PROG["nc"], ins, core_ids=list(range(8)))
    out = np.stack([np.concatenate([res.results[4 * b + j]["y"] for j in range(4)], 0) for b in range(2)], 0)
    return np.ascontiguousarray(out, dtype=np.float32)
```
